# Optimizing a Trainium2 kernel written in Bass

```python
import jax, jax.numpy as jnp
from jax import lax
import numpy as np

D_MODEL = 1024
BATCH = 16
SEQ = 4096
DEPTH = 1

HEAD_DIM = 64
D_MIX = D_MODEL
RWKV_WIDTH = D_MIX // 2
FOX_WIDTH = D_MIX - RWKV_WIDTH
RWKV_HEADS = RWKV_WIDTH // HEAD_DIM
FOX_HEADS = FOX_WIDTH // HEAD_DIM
DECAY_LORA = 64
AAA_LORA = 64
GATE_LORA = 128
RWKV_COLS = 3 * RWKV_WIDTH + DECAY_LORA + AAA_LORA + GATE_LORA
FOX_COLS = 3 * FOX_WIDTH + FOX_HEADS
IN_COLS = RWKV_COLS + FOX_COLS
Q_BLOCK = 128
N_GROUPS = 4
EXPERTS_PER_GROUP = 8
N_EXPERTS = N_GROUPS * EXPERTS_PER_GROUP
TOP_K = 2
D_EXPERT = D_MODEL // 2
MOE_BLOCK = 128
NORM_EPS = 1e-6
GN_EPS = 64e-5

kernel_name = "hymba_rwkv7_fox_hier_moe_adaln"


def rms_norm(x, g):
    xf = x.astype(jnp.float32)
    y = xf * lax.rsqrt(jnp.mean(xf * xf, axis=-1, keepdims=True) + NORM_EPS)
    return (y * g.astype(jnp.float32)).astype(x.dtype)


def wkv7_scan(r, decay, k, v, kk, a):
    B, S, H, N = r.shape

    def step(state, inp):
        r_t, w_t, k_t, v_t, kk_t, a_t = inp
        sa = jnp.einsum("bhvk,bhk->bhv", state, -kk_t)
        state = (state * w_t[:, :, None, :]
                 + sa[..., None] * (kk_t * a_t)[:, :, None, :]
                 + v_t[..., :, None] * k_t[:, :, None, :])
        y = jnp.einsum("bhvk,bhk->bhv", state, r_t)
        return state, y

    xs = tuple(jnp.moveaxis(t, 1, 0) for t in (r, decay, k, v, kk, a))
    init = jnp.zeros((B, H, N, N), jnp.float32)
    _, ys = lax.scan(step, init, xs)
    return jnp.moveaxis(ys, 0, 1)


def rwkv7_time_mix(p, mu, w0, w_up, a0, a_up, g_up, k_k, k_a, r_k, lnx_g, lnx_b):
    B, S, _ = p.shape
    H, N, C = RWKV_HEADS, HEAD_DIM, RWKV_WIDTH
    f32 = jnp.float32
    p_prev = jnp.pad(p, ((0, 0), (1, 0), (0, 0)))[:, :-1]
    p = p + (p_prev - p) * mu
    r, k, v, wd, ad, gd = jnp.split(
        p, [C, 2 * C, 3 * C, 3 * C + DECAY_LORA, 3 * C + DECAY_LORA + AAA_LORA], axis=-1)
    w_log = -jax.nn.softplus(-(w0 + jnp.tanh(wd) @ w_up).astype(f32)) - 0.5
    decay = jnp.exp(-jnp.exp(w_log))
    a = jax.nn.sigmoid((a0 + ad @ a_up).astype(f32))
    g = (jax.nn.sigmoid(gd) @ g_up).astype(f32)
    heads = lambda t: t.astype(f32).reshape(B, S, H, N)
    r, k, v, decay, a = heads(r), heads(k), heads(v), heads(decay), heads(a)
    kk = k * k_k.astype(f32).reshape(H, N)
    kk = kk / jnp.maximum(jnp.sqrt(jnp.sum(kk * kk, axis=-1, keepdims=True)), 1e-12)
    k = k * (1.0 + (a - 1.0) * k_a.astype(f32).reshape(H, N))
    y = wkv7_scan(r, decay, k, v, kk, a)
    mean = jnp.mean(y, axis=-1, keepdims=True)
    var = jnp.mean(jnp.square(y - mean), axis=-1, keepdims=True)
    y = ((y - mean) * lax.rsqrt(var + GN_EPS)).reshape(B, S, C) * lnx_g + lnx_b
    bonus = jnp.sum(r * k * r_k.astype(f32), axis=-1, keepdims=True) * v
    y = y + bonus.reshape(B, S, C)
    return (y * g).astype(p.dtype)


def forgetting_attention(p, f_bias):
    B, S, _ = p.shape
    H, N, C = FOX_HEADS, HEAD_DIM, FOX_WIDTH
    f32 = jnp.float32
    q, k, v, f = jnp.split(p, [C, 2 * C, 3 * C], axis=-1)
    to_heads = lambda t: t.reshape(B, S, H, N).transpose(0, 2, 1, 3)
    q = to_heads(q).astype(f32) * (N ** -0.5)
    k = to_heads(k).astype(f32)
    v = to_heads(v)
    log_f = jax.nn.log_sigmoid((f + f_bias).astype(f32))
    cum = jnp.cumsum(log_f, axis=1).transpose(0, 2, 1)
    outs = []
    for i in range(S // Q_BLOCK):
        q0, q1 = i * Q_BLOCK, (i + 1) * Q_BLOCK
        s = jnp.einsum("bhqd,bhkd->bhqk", q[:, :, q0:q1], k[:, :, :q1])
        s = s + cum[:, :, q0:q1, None] - cum[:, :, None, :q1]
        causal = jnp.arange(q0, q1)[:, None] >= jnp.arange(q1)[None, :]
        s = jnp.where(causal, s, -jnp.inf)
        prob = jax.nn.softmax(s, axis=-1)
        outs.append(jnp.einsum("bhqk,bhkd->bhqd", prob.astype(v.dtype), v[:, :, :q1]))
    o = jnp.concatenate(outs, axis=2)
    return o.transpose(0, 2, 1, 3).reshape(B, S, C)


def hier_moe(h, w_grp, b_grp, w_rt, b_rt, w_gate, w_up, w_down):
    T, D = h.shape
    f32 = jnp.float32
    grp_prob = jax.nn.softmax((h @ w_grp).astype(f32) + b_grp, axis=-1)
    g_sel = jnp.argmax(grp_prob, axis=-1)
    p_g = jnp.take_along_axis(grp_prob, g_sel[:, None], axis=-1)
    e_logits = ((h @ w_rt).astype(f32) + b_rt).reshape(T, N_GROUPS, EXPERTS_PER_GROUP)
    e_logits = jnp.take_along_axis(e_logits, g_sel[:, None, None], axis=1)[:, 0]
    top_p, top_i = lax.top_k(jax.nn.softmax(e_logits, axis=-1), TOP_K)
    weights = p_g * top_p / jnp.sum(top_p, axis=-1, keepdims=True)
    expert_id = g_sel[:, None].astype(jnp.int32) * EXPERTS_PER_GROUP + top_i.astype(jnp.int32)

    A = T * TOP_K
    flat_e = expert_id.reshape(A)
    flat_w = weights.reshape(A)
    flat_tok = jnp.repeat(jnp.arange(T, dtype=jnp.int32), TOP_K)
    order = jnp.argsort(flat_e)
    se = flat_e[order]
    counts = jnp.bincount(flat_e, length=N_EXPERTS)
    starts = jnp.cumsum(counts) - counts
    padded = (counts + MOE_BLOCK - 1) // MOE_BLOCK * MOE_BLOCK
    pends = jnp.cumsum(padded)
    pstarts = pends - padded
    dest = pstarts[se] + jnp.arange(A, dtype=jnp.int32) - starts[se]
    P = (A + N_EXPERTS * (MOE_BLOCK - 1) + MOE_BLOCK - 1) // MOE_BLOCK * MOE_BLOCK
    n_blocks = P // MOE_BLOCK
    buf_tok = jnp.zeros((P,), jnp.int32).at[dest].set(flat_tok[order])
    buf_w = jnp.zeros((P,), f32).at[dest].set(flat_w[order])
    blk_e = jnp.searchsorted(pends, jnp.arange(n_blocks, dtype=pends.dtype) * MOE_BLOCK, side="right")
    blk_e = jnp.minimum(blk_e, N_EXPERTS - 1).astype(jnp.int32)

    def expert_block(args):
        tok, wgt, e = args
        xb = h[tok]
        hid = jax.nn.silu(xb @ w_gate[e]) * (xb @ w_up[e])
        y = hid @ w_down[e]
        return y * wgt[:, None].astype(y.dtype)

    ys = lax.map(expert_block, (buf_tok.reshape(n_blocks, MOE_BLOCK),
                                buf_w.reshape(n_blocks, MOE_BLOCK), blk_e))
    return jax.ops.segment_sum(ys.reshape(P, D), buf_tok, num_segments=T)


def setup_inputs(seed: int = 0) -> dict:
    key = jax.random.key(seed)
    ks = jax.random.split(key, 28)
    nrm = lambda i, shape, scale: scale * jax.random.normal(ks[i], shape, jnp.float32)
    L, D, C = DEPTH, D_MODEL, RWKV_WIDTH
    chan = jnp.arange(C, dtype=jnp.float32) / (C - 1)
    w0_base = -7.0 + 5.0 * chan ** 0.85 + 0.5
    return {
        "x": nrm(0, (BATCH, SEQ, D), 1.0),
        "c": nrm(1, (BATCH, D), 1.0),
        "w_ada": nrm(2, (L, D, 6 * D), 0.5 * D ** -0.5),
        "b_ada": nrm(3, (L, 6 * D), 0.02),
        "norm1_g": 1.0 + nrm(4, (L, D), 0.05),
        "w_in": nrm(5, (L, D, IN_COLS), D ** -0.5),
        "rwkv_mu": jax.random.uniform(ks[6], (L, RWKV_COLS), jnp.float32),
        "rwkv_w0": w0_base + nrm(7, (L, C), 0.1),
        "rwkv_w_up": nrm(8, (L, DECAY_LORA, C), 0.5 * DECAY_LORA ** -0.5),
        "rwkv_a0": nrm(9, (L, C), 0.1),
        "rwkv_a_up": nrm(10, (L, AAA_LORA, C), AAA_LORA ** -0.5),
        "rwkv_g_up": nrm(11, (L, GATE_LORA, C), GATE_LORA ** -0.5),
        "rwkv_k_k": 0.85 + nrm(12, (L, C), 0.05),
        "rwkv_k_a": 1.0 + nrm(13, (L, C), 0.05),
        "rwkv_r_k": nrm(14, (L, RWKV_HEADS, HEAD_DIM), 0.1),
        "rwkv_lnx_g": 1.0 + nrm(15, (L, C), 0.05),
        "rwkv_lnx_b": nrm(16, (L, C), 0.02),
        "fox_f_bias": jnp.linspace(1.0, 5.0, FOX_HEADS, dtype=jnp.float32) + nrm(17, (L, FOX_HEADS), 0.1),
        "w_out": nrm(18, (L, D_MIX, D), D_MIX ** -0.5),
        "norm2_g": 1.0 + nrm(19, (L, D), 0.05),
        "moe_w_grp": nrm(20, (L, D, N_GROUPS), D ** -0.5),
        "moe_b_grp": nrm(21, (L, N_GROUPS), 0.01),
        "moe_w_rt": nrm(22, (L, D, N_EXPERTS), D ** -0.5),
        "moe_b_rt": nrm(23, (L, N_EXPERTS), 0.01),
        "moe_w_gate": nrm(24, (L, N_EXPERTS, D, D_EXPERT), D ** -0.5),
        "moe_w_up": nrm(25, (L, N_EXPERTS, D, D_EXPERT), D ** -0.5),
        "moe_w_down": nrm(26, (L, N_EXPERTS, D_EXPERT, D), D_EXPERT ** -0.5),
        "norm_f_g": 1.0 + nrm(27, (D,), 0.05),
    }


def reference(x, c, w_ada, b_ada, norm1_g, w_in, rwkv_mu, rwkv_w0, rwkv_w_up, rwkv_a0, rwkv_a_up,
              rwkv_g_up, rwkv_k_k, rwkv_k_a, rwkv_r_k, rwkv_lnx_g, rwkv_lnx_b, fox_f_bias, w_out,
              norm2_g, moe_w_grp, moe_b_grp, moe_w_rt, moe_b_rt, moe_w_gate, moe_w_up, moe_w_down,
              norm_f_g):
    B, S, D = x.shape
    cond = jax.nn.silu(c)
    for l in range(DEPTH):
        mod = (cond @ w_ada[l] + b_ada[l])[:, None, :]
        sh1, sc1, gt1, sh2, sc2, gt2 = jnp.split(mod, 6, axis=-1)
        h = rms_norm(x, norm1_g[l]) * (1.0 + sc1) + sh1
        proj = h @ w_in[l]
        y_rwkv = rwkv7_time_mix(proj[..., :RWKV_COLS], rwkv_mu[l], rwkv_w0[l], rwkv_w_up[l],
                                rwkv_a0[l], rwkv_a_up[l], rwkv_g_up[l], rwkv_k_k[l], rwkv_k_a[l],
                                rwkv_r_k[l], rwkv_lnx_g[l], rwkv_lnx_b[l])
        y_fox = forgetting_attention(proj[..., RWKV_COLS:], fox_f_bias[l])
        mix = jnp.concatenate([y_rwkv, y_fox], axis=-1) @ w_out[l]
        x = x + gt1 * mix
        h = rms_norm(x, norm2_g[l]) * (1.0 + sc2) + sh2
        ff = hier_moe(h.reshape(B * S, D), moe_w_grp[l], moe_b_grp[l], moe_w_rt[l], moe_b_rt[l],
                      moe_w_gate[l], moe_w_up[l], moe_w_down[l]).reshape(B, S, D)
        x = x + gt2 * ff
    return rms_norm(x, norm_f_g)
```

```python
import contextlib
import itertools
import numpy as np
import concourse.bass as bass
import concourse.mybir as mybir
from concourse.bass_utils import run_bass_kernel_spmd

F32 = mybir.dt.float32
BF16 = mybir.dt.bfloat16
I32 = mybir.dt.uint32
AF = mybir.ActivationFunctionType
ALU = mybir.AluOpType
AX = mybir.AxisListType

NCORES = 8
S = 4096
D = 1024
NB = 2
T = NB * S
NT = T // 128
NBLK = 64
BSZ = 512
NTHR = T // BSZ
CH = 32

STOP_AFTER = None
DEBUG = False


MULTI = []


class Buf:
    __slots__ = ("name", "w", "r", "const", "multi", "ws")

    def __init__(self, name="", const=False, multi=False):
        self.name = name
        self.w = None
        self.r = []
        self.const = const
        self.multi = multi
        self.ws = []
        if multi:
            MULTI.append(self)


class Op:
    __slots__ = ("eng", "fn", "deps", "needs", "ev", "dma", "grp")


class Grp:
    __slots__ = ("key", "last")

    def __init__(self):
        self.key = None
        self.last = None


JOIN = "join"


class Sched:
    COMPUTE = ("pe", "dve", "act", "pool")

    def __init__(self, nc, es):
        self.nc = nc
        self.engobj = dict(pe=nc.tensor, dve=nc.vector, act=nc.scalar, pool=nc.gpsimd, sp=nc.sync)
        self.sems = []
        self.semid = {}
        for e in self.COMPUTE + ("sp",):
            self.semid[e] = len(self.sems)
            self.sems.append(es.enter_context(nc.semaphore("s_" + e)))
        self.dsem = {}
        for q, k in (("sp", 32), ("pool", 16)):
            ids = []
            for i in range(k):
                ids.append(len(self.sems))
                self.sems.append(es.enter_context(nc.semaphore(f"d_{q}{i}")))
            self.dsem[q] = ids
        self.ops = []
        self.dcount = {"sp": 0, "pool": 0}
        self.dlast = {}
        self.last = {}

    def _mk(self, eng, fn, reads, writes, dma, grp=None, first=True):
        o = Op()
        o.eng = eng
        o.fn = fn
        o.needs = False
        o.ev = None
        o.dma = dma
        o.grp = grp
        deps = {}
        for b in reads:
            if b.multi:
                for p in b.ws:
                    deps[id(p)] = (p, True)
            elif b.w is not None:
                deps[id(b.w)] = (b.w, True)
        for b in writes:
            if not b.multi and b.w is not None and id(b.w) not in deps:
                deps[id(b.w)] = (b.w, False)
            for r in b.r:
                if id(r) not in deps:
                    deps[id(r)] = (r, False)
        out = []
        for p, raw in deps.values():
            if p is o:
                continue
            if dma is None and p.dma is None and p.eng == eng:
                if eng == "pe" or not raw:
                    continue
            out.append(p)
        if dma is not None:
            prev = self.dlast.get(dma)
            if prev is not None and first:
                out.append(prev)
            self.dlast[dma] = o
            if grp is not None:
                grp.last = o
        for p in out:
            p.needs = True
        o.deps = out
        for b in reads:
            if not b.const and not (fn is JOIN):
                b.r.append(o)
        for b in writes:
            if b.multi:
                if b.r or fn is JOIN:
                    b.ws = [o]
                else:
                    b.ws.append(o)
            b.w = o
            b.r = []
        self.ops.append(o)
        if dma is None:
            self.last[eng] = o
        return o

    def op(self, eng, fn, reads=(), writes=()):
        return self._mk(eng, fn, reads, writes, None)

    def dma(self, fn, reads=(), writes=(), q="sp", grp=None):
        if grp is not None and grp.key is not None:
            assert grp.key[0] == q
            return self._mk(q, fn, reads, writes, grp.key, grp, first=False)
        k = self.dcount[q]
        self.dcount[q] = k + 1
        ids = self.dsem[q]
        key = (q, k % len(ids))
        if grp is not None:
            grp.key = key
        return self._mk(q, fn, reads, writes, key, grp)

    def join(self, bufs):
        return self._mk("sp", JOIN, bufs, bufs, None)

    def barrier(self):
        for b in MULTI:
            b.ws = []
            b.r = []
            b.w = None
        lastops = [o for o in self.last.values()] + list(self.dlast.values())
        for e in ("pe", "dve", "act", "pool", "sp"):
            o = Op()
            o.eng = e
            o.fn = None
            o.needs = False
            o.ev = None
            o.dma = None
            o.grp = None
            o.deps = [p for p in lastops if not (p.dma is None and p.eng == e and e == "pe")]
            for p in o.deps:
                p.needs = True
            self.ops.append(o)

    def emit(self):
        cnt = {}
        for o in self.ops:
            if o.fn is None:
                continue
            if o.dma is not None:
                sid = self.dsem[o.dma[0]][o.dma[1]]
                cnt[sid] = cnt.get(sid, 0) + 16
                o.ev = (sid, cnt[sid])
            elif o.needs:
                sid = self.semid[o.eng]
                cnt[sid] = cnt.get(sid, 0) + 1
                o.ev = (sid, cnt[sid])
        waited = {e: {} for e in self.engobj}
        nwait = 0
        for o in self.ops:
            E = self.engobj[o.eng]
            w = waited[o.eng]
            need = {}
            for p in o.deps:
                sid, v = p.ev if p.grp is None else p.grp.last.ev
                if w.get(sid, 0) < v and need.get(sid, 0) < v:
                    need[sid] = v
            for sid, v in need.items():
                E.wait_ge(self.sems[sid], v)
                w[sid] = v
                nwait += 1
            if o.fn is JOIN:
                if o.needs:
                    E.sem_inc(self.sems[o.ev[0]], 1)
            elif o.fn is not None:
                inst = o.fn()
                if o.dma is not None:
                    inst.then_inc(self.sems[o.ev[0]], 16)
                elif o.needs:
                    inst.then_inc(self.sems[o.ev[0]], 1)
        return nwait


class K:
    def __init__(self):
        self.nc = bass.Bass("TRN2", target_bir_lowering=False)
        self.es = contextlib.ExitStack()
        self.s = Sched(self.nc, self.es)
        self.nbuf = 0

    def din(self, name, shape, dt=F32):
        return self.nc.dram_tensor(name, list(shape), dt, kind="ExternalInput").ap()

    def dscr(self, name, shape, dt=F32):
        kind = "ExternalOutput" if (DEBUG and name in DEBUG) else "Internal"
        return self.nc.dram_tensor(name, list(shape), dt, kind=kind).ap()

    def sb(self, es, name, shape, dt=F32):
        self.nbuf += 1
        return es.enter_context(self.nc.sbuf_tensor(f"{name}_{self.nbuf}", list(shape), dt))

    def ps(self, es, name, shape, dt=F32):
        self.nbuf += 1
        return es.enter_context(self.nc.psum_tensor(f"{name}_{self.nbuf}", list(shape), dt))

    def v(self, name, R, W, *a, **kw):
        f = getattr(self.nc.vector, name)
        return self.s.op("dve", lambda: f(*a, **kw), R, W)

    def g(self, name, R, W, *a, **kw):
        f = getattr(self.nc.gpsimd, name)
        return self.s.op("pool", lambda: f(*a, **kw), R, W)

    def a(self, R, W, out, in_, func, bias=None, scale=None):
        kw = {}
        if bias is not None:
            kw["bias"] = bias
        if scale is not None:
            kw["scale"] = scale
        f = self.nc.scalar.activation
        return self.s.op("act", lambda: f(out, in_, func, **kw), R, W)

    def mm(self, R, W, out, lhsT, rhs, start=True, stop=True):
        f = self.nc.tensor.matmul
        return self.s.op("pe", lambda: f(out, lhsT, rhs, start=start, stop=stop), R, W)

    def tr(self, R, W, out, in_, ident):
        f = self.nc.tensor.transpose
        return self.s.op("pe", lambda: f(out, in_, ident), R, W)

    def dma(self, R, W, out, in_, q="sp", grp=None, **kw):
        f = self.engobj(q).dma_start
        return self.s.dma(lambda: f(out=out, in_=in_, **kw), R, W, q=q)

    def engobj(self, q):
        return self.s.engobj[q]

    def idma(self, R, W, out, out_off, in_, in_off, grp=None):
        f = self.nc.gpsimd.indirect_dma_start
        return self.s.dma(lambda: f(out, out_off, in_, in_off), R, W, q="pool")

    def build(self):
        nc = self.nc
        es = self.es
        I = {}
        I["x"] = self.din("x", [NB, S, D])
        I["cT"] = self.din("cT", [128, 8, NB])
        I["w_ada"] = self.din("w_ada", [D, 6 * D])
        I["b_ada"] = self.din("b_ada", [1, 6 * D])
        I["g1"] = self.din("g1", [1, D])
        I["g2"] = self.din("g2", [1, D])
        I["gf"] = self.din("gf", [1, D])
        I["w_in"] = self.din("w_in", [D, 3336])
        I["mu"] = self.din("mu", [1, 1792])
        I["rwv"] = self.din("rwv", [7, 512])
        I["w_up"] = self.din("w_up", [64, 512])
        I["a_up"] = self.din("a_up", [64, 512])
        I["g_up"] = self.din("g_up", [128, 512])
        I["fbias"] = self.din("fbias", [8, 1])
        I["w_out"] = self.din("w_out", [D, D])
        I["w_r"] = self.din("w_r", [D, 36])
        I["b_r"] = self.din("b_r", [1, 36])
        I["wg"] = self.din("wg", [32 * 128, 8 * 512])
        I["wu"] = self.din("wu", [32 * 128, 8 * 512])
        I["wd"] = self.din("wd", [32 * 128, 4 * 1024])
        I["ident"] = self.din("ident", [128, 128])
        I["maskneg"] = self.din("maskneg", [128, 128])
        I["lstrict"] = self.din("lstrict", [128, 128])
        I["sel2"] = self.din("sel2", [2, 256])
        I["thr64"] = self.din("thr64", [1, NTHR])
        I["thr160"] = self.din("thr160", [1, NBLK])
        I["iota_pk"] = self.din("iota_pk", [128, 8])
        I["iota_pf"] = self.din("iota_pf", [128, 4])
        self.I = I
        self.out = nc.dram_tensor("out", [NB, S, D], F32, kind="ExternalOutput").ap()
        Sc = {}
        for n in ("s_r", "s_w", "s_k", "s_kk", "s_nk"):
            Sc[n] = self.dscr(n, [NB, 8, S, 64])
        Sc["s_v"] = self.dscr("s_v", [NB * 64, S, 8])
        Sc["s_y"] = self.dscr("s_y", [NB * 64, S, 8])
        Sc["s_g"] = self.dscr("s_g", [NB, S, 512])
        Sc["s_vt"] = self.dscr("s_vt", [NB, S, 512])
        Sc["s_bs"] = self.dscr("s_bs", [NB, S, 8])
        Sc["s_qa"] = self.dscr("s_qa", [NB, 8, 70, S], BF16)
        Sc["s_ka"] = self.dscr("s_ka", [NB, 8, 70, S], BF16)
        Sc["s_fv"] = self.dscr("s_fv", [NB, S, 512], BF16)
        Sc["s_yf"] = self.dscr("s_yf", [NB, S, 512])
        Sc["s_hT"] = self.dscr("s_hT", [NB, 8, 128, 8, 512], BF16)
        Sc["s_x1"] = self.dscr("s_x1", [NB, S, D])
        Sc["s_h2"] = self.dscr("s_h2", [T, D], BF16)
        Sc["s_xs"] = self.dscr("s_xs", [NBLK * BSZ, D], BF16)
        Sc["s_ys"] = self.dscr("s_ys", [NBLK * BSZ, D])
        self.Sc = Sc
        self.Bd = {n: Buf(n, multi=True) for n in Sc}
        self.Bout = Buf("out", multi=True)

        self.modrow = self.sb(es, "modrow", [2, 6 * D])
        self.Bmod = Buf("modrow")
        self.ident_f = self.sb(es, "ident_f", [128, 128])
        self.ident_b = self.sb(es, "ident_b", [128, 128], BF16)
        self.sel2 = self.sb(es, "sel2", [2, 256])
        self.Bconst = Buf("const")
        self.dma([], [self.Bconst], self.ident_f[:], I["ident"])
        self.dma([], [self.Bconst], self.sel2[:], I["sel2"])
        self.v("tensor_copy", [self.Bconst], [self.Bconst], self.ident_b[:], self.ident_f[:])

        phases = ["A", "BC1", "EH", "I", "K", "L"]
        for ph in phases:
            getattr(self, "phase_" + ph)()
            self.s.barrier()
            if STOP_AFTER == ph:
                break
        nw = self.s.emit()
        self.es.close()
        return nc

    def bcast_row(self, dst, b, j, Bdst, pp, Bpp, mode, gb=None, Bg=None):
        for half in range(2):
            p = pp[half]
            self.mm([self.Bmod, self.Bconst], [Bpp[half]], p[:, :], self.sel2[0:2, b * 128:(b + 1) * 128],
                    self.modrow[0:2, j * D + half * 512: j * D + half * 512 + 512])
            if mode == "copy":
                self.a([Bpp[half]], [Bdst], dst[:, half * 512:(half + 1) * 512], p[:, :], AF.Copy)
            else:
                self.v("scalar_tensor_tensor", [Bpp[half], Bg], [Bdst], dst[:, half * 512:(half + 1) * 512],
                       p[:, :], 1.0, gb[:, half * 512:(half + 1) * 512], ALU.add, ALU.mult)

    def rms_rstd(self, xt, Bx, junk, Bj, ss, Bs):
        self.g("tensor_tensor", [Bx], [Bj], junk[:], xt[:], xt[:], ALU.mult)
        self.v("tensor_reduce", [Bj], [Bs], ss[:, 0:1], junk[:], AX.X, ALU.add)
        self.a([Bs], [Bs], ss[:, 1:2], ss[:, 0:1], AF.Sqrt, bias=self.eps6[:, 0:1], scale=1.0 / D)
        self.v("reciprocal", [Bs], [Bs], ss[:, 2:3], ss[:, 1:2])
        return ss[:, 2:3]

    def phase_A(self):
        I = self.I
        with contextlib.ExitStack() as es:
            condT = self.sb(es, "condT", [128, 8, NB])
            Bc = Buf()
            self.dma([], [Bc], condT[:], I["cT"])
            self.a([Bc], [Bc], condT[:], condT[:], AF.Silu)
            b2 = self.sb(es, "b_ada2", [2, 6 * D])
            Bb2 = Buf()
            self.dma([], [Bb2], b2[:], I["b_ada"][0].partition_broadcast(2))
            wv = I["w_ada"].rearrange("(kc p) n -> p kc n", p=128)
            wt = [self.sb(es, "wada", [128, 8, 512]) for _ in range(2)]
            Bw = [Buf(), Buf()]
            pp = [self.ps(es, "psA", [2, 512]) for _ in range(2)]
            Bp = [Buf(), Buf()]
            for j in range(12):
                k = j % 2
                self.dma([], [Bw[k]], wt[k][:], wv[:, :, j * 512:(j + 1) * 512])
                for kc in range(8):
                    self.mm([Bc, Bw[k]], [Bp[k]], pp[k][:, :], condT[:, kc, :], wt[k][:, kc, :],
                            start=(kc == 0), stop=(kc == 7))
                self.v("tensor_tensor", [Bp[k], Bb2], [self.Bmod], self.modrow[0:2, j * 512:(j + 1) * 512],
                       pp[k][:, :], b2[:, j * 512:(j + 1) * 512], ALU.add)
        self.eps6 = self.sb(self.es, "eps6", [128, 2])
        self.v("memset", [], [self.Bconst], self.eps6[:, 0:1], 1e-6)
        self.v("memset", [], [self.Bconst], self.eps6[:, 1:2], 64e-5)

    def make_hT(self, es_bufs, b, blk):
        Bf = es_bufs
        I = self.I
        k = (b * 8 + blk) % 2
        hT, BhT = Bf["hT"][k], Bf["BhT"][k]
        if blk == 0:
            self.v("memset", [], [BhT], hT[:, :, 0:4], 0.0)
        else:
            hp, Bhp = Bf["hT"][1 - k], Bf["BhT"][1 - k]
            self.v("tensor_copy", [Bhp], [BhT], hT[:, :, 3:4], hp[:, :, 515:516])
        for j in range(4):
            n = blk * 4 + j
            t0 = n * 128
            kk = n % 2
            xt, Bx = Bf["xt"][kk], Bf["Bxt"][kk]
            self.dma([], [Bx], xt[:], I["x"][b, t0:t0 + 128, :])
            rstd = self.rms_rstd(xt, Bx, Bf["junk"], Bf["Bjunk"], Bf["ss"][kk], Bf["Bss"][kk])
            self.v("scalar_tensor_tensor", [Bx, Bf["Bss"][kk], Bf["BA1"]], [Bf["Bjunk"]], Bf["junk"][:], xt[:], rstd,
                   Bf["A1"][:], ALU.mult, ALU.mult)
            hb, Bhb = Bf["hb"][kk], Bf["Bhb"][kk]
            self.v("tensor_tensor", [Bf["Bjunk"], Bf["BB1"]], [Bhb], hb[:], Bf["junk"][:], Bf["B1"][:], ALU.add)
            pT, BpT = Bf["pT"][kk], Bf["BpT"][kk]
            for kc in range(8):
                self.tr([Bhb, self.Bconst], [BpT], pT[:, kc, :], hb[:, kc * 128:(kc + 1) * 128], self.ident_b[:])
            self.a([BpT], [BhT], hT[:, :, 4 + j * 128: 4 + (j + 1) * 128], pT[:, :, :], AF.Copy)
        return hT, BhT

    def alloc_hT_bufs(self, es):
        Bf = {}
        Bf["hT"] = [self.sb(es, "hT", [128, 8, 516], BF16) for _ in range(2)]
        Bf["BhT"] = [Buf(), Buf()]
        Bf["xt"] = [self.sb(es, "xt", [128, D]) for _ in range(2)]
        Bf["Bxt"] = [Buf(), Buf()]
        Bf["junk"] = self.sb(es, "junk", [128, D])
        Bf["Bjunk"] = Buf()
        Bf["ss"] = [self.sb(es, "ss", [128, 4]) for _ in range(2)]
        Bf["Bss"] = [Buf(), Buf()]
        Bf["hb"] = [self.sb(es, "hb", [128, D], BF16) for _ in range(2)]
        Bf["Bhb"] = [Buf(), Buf()]
        Bf["pT"] = [self.ps(es, "pT", [128, 8, 128], BF16) for _ in range(2)]
        Bf["BpT"] = [Buf(), Buf()]
        Bf["A1"] = self.sb(es, "A1", [128, D])
        Bf["B1"] = self.sb(es, "B1", [128, D])
        Bf["BA1"] = Buf()
        Bf["BB1"] = Buf()
        Bf["g1b"] = self.sb(es, "g1b", [128, D])
        Bf["Bg1b"] = Buf()
        self.dma([], [Bf["Bg1b"]], Bf["g1b"][:], self.I["g1"][0].partition_broadcast(128))
        return Bf

    def set_AB1(self, Bf, b, pp, Bpp):
        self.bcast_row(Bf["B1"], b, 0, Bf["BB1"], pp, Bpp, "copy")
        self.bcast_row(Bf["A1"], b, 1, Bf["BA1"], pp, Bpp, "scale", Bf["g1b"], Bf["Bg1b"])

    def phase_BC1(self):
        I, Sc, Bd = self.I, self.Sc, self.Bd
        with contextlib.ExitStack() as es:
            Bf = self.alloc_hT_bufs(es)
            W1 = self.sb(es, "W1", [128, 8, 1792], BF16)
            W2 = self.sb(es, "W2", [128, 8, 1792], BF16)
            BW = Buf()
            P = {}
            BP = Buf()
            for i, n in enumerate(["w0", "a0", "kk", "ka", "rk"]):
                P[n] = self.sb(es, "P" + n, [128, 512])
                self.dma([], [BP], P[n][:], I["rwv"][i].partition_broadcast(128))
            P["omka"] = self.sb(es, "Pomka", [128, 512])
            self.v("tensor_scalar", [BP], [BP], P["omka"][:], P["ka"][:], -1.0, 1.0, ALU.mult, ALU.add)
            lora = self.sb(es, "lora", [128, 3, 512], BF16)
            BL = Buf()
            es1 = contextlib.ExitStack()
            es_main = es
            es = es1
            mub = self.sb(es, "mub", [128, 1792])
            omub = self.sb(es, "omub", [128, 1792])
            Bmu = Buf()
            self.dma([], [Bmu], mub[:], I["mu"][0].partition_broadcast(128))
            self.v("tensor_scalar", [Bmu], [Bmu], omub[:], mub[:], -1.0, 1.0, ALU.mult, ALU.add)
            wv = I["w_in"].rearrange("(kc p) n -> p kc n", p=128)
            wtmp = [self.sb(es, "wtmp", [128, 8, 256]) for _ in range(2)]
            Bwt = [Buf(), Buf()]
            for c in range(7):
                k = c % 2
                c0 = c * 256
                self.dma([], [Bwt[k]], wtmp[k][:], wv[:, :, c0:c0 + 256])
                self.v("tensor_tensor", [Bwt[k], Bmu], [BW], W1[:, :, c0:c0 + 256], wtmp[k][:],
                       omub[:, c0:c0 + 256].unsqueeze(1).broadcast_to([128, 8, 256]), ALU.mult)
                self.g("tensor_tensor", [Bwt[k], Bmu], [BW], W2[:, :, c0:c0 + 256], wtmp[k][:],
                       mub[:, c0:c0 + 256].unsqueeze(1).broadcast_to([128, 8, 256]), ALU.mult)
            stage = self.sb(es, "lstage", [128, 3, 512])
            self.v("memset", [], [BL], stage[:], 0.0)
            self.dma([], [BL], stage[0:64, 0, :], I["w_up"])
            self.dma([], [BL], stage[64:128, 1, :], I["a_up"])
            self.dma([], [BL], stage[:, 2, :], I["g_up"])
            self.v("tensor_copy", [BL], [BL], lora[:], stage[:])
            self.s.barrier()
            es1.close()
            es = es_main
            NR = 5
            rot = [self.ps(es, "rot", [128, 512]) for _ in range(NR)]
            Brot = [Buf() for _ in range(NR)]
            rc = [0]

            def nxt():
                i = rc[0] % NR
                rc[0] += 1
                return rot[i], Brot[i]

            twd = self.sb(es, "twd", [128, 512], BF16)
            sgd = self.sb(es, "sgd", [128, 512], BF16)
            Btwd, Bsgd = Buf(), Buf()
            names = ["r", "w", "k", "v", "kk", "nk", "g"]
            O = {n: [self.sb(es, "o_" + n, [128, 512]) for _ in range(2)] for n in names}
            BO = {n: [Buf(), Buf()] for n in names}
            obs = [self.sb(es, "o_bs", [128, 8]) for _ in range(2)]
            Bobs = [Buf(), Buf()]
            tmpsL = [{n: self.sb(es, "t_" + n, [128, 512]) for n in ["zw", "a", "kku", "sq", "t1", "rk"]} for _ in range(2)]
            BtL = [{n: Buf() for n in tmpsL[0]} for _ in range(2)]
            smL = [self.sb(es, "sm", [128, 32]) for _ in range(2)]
            BsmL = [Buf(), Buf()]

            for b in range(NB):
                self.set_AB1(Bf, b, [rot[0], rot[1]], [Brot[0], Brot[1]])
                for blk in range(8):
                    hT, BhT = self.make_hT(Bf, b, blk)
                    self.dma([BhT], [Bd["s_hT"]], Sc["s_hT"][b, blk], hT[:, :, 4:516])
                    for ch in (12, 13):
                        p, Bp = nxt()
                        for kc in range(8):
                            self.mm([BW, BhT], [Bp], p[:, :], W1[:, kc, ch * 128:(ch + 1) * 128], hT[:, kc, 4:516],
                                    start=(kc == 0), stop=False)
                            self.mm([BW, BhT], [Bp], p[:, :], W2[:, kc, ch * 128:(ch + 1) * 128], hT[:, kc, 3:515],
                                    start=False, stop=(kc == 7))
                        if ch == 12:
                            self.a([Bp], [Btwd], twd[0:64, :], p[0:64, :], AF.Tanh)
                            self.a([Bp], [Btwd], twd[64:128, :], p[64:128, :], AF.Copy)
                        else:
                            self.a([Bp], [Bsgd], sgd[:, :], p[:, :], AF.Sigmoid)
                    for j in range(4):
                        n = blk * 4 + j
                        t0 = n * 128
                        q = n % 2
                        tmps, Bt, sm, Bsm = tmpsL[q], BtL[q], smL[q], BsmL[q]
                        cur = lambda kc: hT[:, kc, 4 + j * 128: 4 + (j + 1) * 128]
                        prv = lambda kc: hT[:, kc, 3 + j * 128: 3 + (j + 1) * 128]

                        def proj(c0):
                            p, Bp = nxt()
                            for kc in range(8):
                                self.mm([BW, BhT], [Bp], p[:, :], cur(kc), W1[:, kc, c0:c0 + 512],
                                        start=(kc == 0), stop=False)
                                self.mm([BW, BhT], [Bp], p[:, :], prv(kc), W2[:, kc, c0:c0 + 512],
                                        start=False, stop=(kc == 7))
                            return p, Bp

                        p_r, Bp_r = proj(0)
                        self.a([Bp_r], [BO["r"][q]], O["r"][q][:], p_r[:, :], AF.Copy)
                        p_v, Bp_v = proj(1024)
                        self.a([Bp_v], [BO["v"][q]], O["v"][q][:], p_v[:, :], AF.Copy)
                        p_w, Bp_w = nxt()
                        self.mm([Btwd, BL], [Bp_w], p_w[:, :], twd[0:64, j * 128:(j + 1) * 128], lora[0:64, 0, :])
                        self.v("tensor_tensor", [Bp_w, BP], [Bt["zw"]], tmps["zw"][:], p_w[:, :], P["w0"][:], ALU.add)
                        self.a([Bt["zw"]], [Bt["zw"]], tmps["zw"][:], tmps["zw"][:], AF.Sigmoid)
                        self.a([Bt["zw"]], [BO["w"][q]], O["w"][q][:], tmps["zw"][:], AF.Exp, scale=-0.6065306597126334)
                        p_a, Bp_a = nxt()
                        self.mm([Btwd, BL], [Bp_a], p_a[:, :], twd[64:128, j * 128:(j + 1) * 128], lora[64:128, 1, :])
                        self.v("tensor_tensor", [Bp_a, BP], [Bt["a"]], tmps["a"][:], p_a[:, :], P["a0"][:], ALU.add)
                        self.a([Bt["a"]], [Bt["a"]], tmps["a"][:], tmps["a"][:], AF.Sigmoid)
                        p_k, Bp_k = proj(512)
                        self.v("tensor_tensor", [Bp_k, BP], [Bt["kku"]], tmps["kku"][:], p_k[:, :], P["kk"][:], ALU.mult)
                        self.g("tensor_tensor", [Bt["kku"]], [Bt["sq"]], tmps["sq"][:], tmps["kku"][:], tmps["kku"][:], ALU.mult)
                        self.v("tensor_reduce", [Bt["sq"]], [Bsm], sm[:, 0:8],
                               tmps["sq"][:].rearrange("p (h k) -> p h k", k=64), AX.X, ALU.add)
                        self.a([Bsm], [Bsm], sm[:, 8:16], sm[:, 0:8], AF.Sqrt)
                        self.v("tensor_scalar", [Bsm], [Bsm], sm[:, 8:16], sm[:, 8:16], 1e-12, None, ALU.max)
                        self.v("reciprocal", [Bsm], [Bsm], sm[:, 16:24], sm[:, 8:16])
                        self.v("tensor_tensor", [Bt["kku"], Bsm], [BO["kk"][q]],
                               O["kk"][q][:].rearrange("p (h k) -> p h k", k=64),
                               tmps["kku"][:].rearrange("p (h k) -> p h k", k=64),
                               sm[:, 16:24].unsqueeze(2).broadcast_to([128, 8, 64]), ALU.mult)
                        self.v("tensor_tensor", [Bt["a"], BP], [Bt["t1"]], tmps["t1"][:], tmps["a"][:], P["ka"][:], ALU.mult)
                        self.g("tensor_tensor", [Bt["t1"], BP], [Bt["t1"]], tmps["t1"][:], tmps["t1"][:], P["omka"][:], ALU.add)
                        self.v("tensor_tensor", [Bp_k, Bt["t1"]], [BO["k"][q]], O["k"][q][:], p_k[:, :], tmps["t1"][:], ALU.mult)
                        self.v("scalar_tensor_tensor", [BO["kk"][q], Bt["a"]], [BO["nk"][q]], O["nk"][q][:],
                               O["kk"][q][:], -1.0, tmps["a"][:], ALU.mult, ALU.mult)
                        self.g("tensor_tensor", [BO["r"][q], BO["k"][q]], [Bt["rk"]], tmps["rk"][:], O["r"][q][:], O["k"][q][:], ALU.mult)
                        self.g("tensor_tensor", [Bt["rk"], BP], [Bt["rk"]], tmps["rk"][:], tmps["rk"][:], P["rk"][:], ALU.mult)
                        self.v("tensor_reduce", [Bt["rk"]], [Bobs[q]], obs[q][:, 0:8],
                               tmps["rk"][:].rearrange("p (h k) -> p h k", k=64), AX.X, ALU.add)
                        p_g, Bp_g = nxt()
                        self.mm([Bsgd, BL], [Bp_g], p_g[:, :], sgd[:, j * 128:(j + 1) * 128], lora[:, 2, :])
                        self.a([Bp_g], [BO["g"][q]], O["g"][q][:], p_g[:, :], AF.Copy)
                        for n_, sname in (("r", "s_r"), ("w", "s_w"), ("k", "s_k"), ("kk", "s_kk"), ("nk", "s_nk")):
                            dst = Sc[sname][b].rearrange("h t k -> t h k")[t0:t0 + 128]
                            self.dma([BO[n_][q]], [Bd[sname]], dst, O[n_][q][:].rearrange("p (h k) -> p h k", k=64))
                        dstv = Sc["s_v"][b * 64:(b + 1) * 64].rearrange("hv t l -> t hv l")[t0:t0 + 128]
                        self.dma([BO["v"][q]], [Bd["s_v"]], dstv, O["v"][q][:].rearrange("p (hv l) -> p hv l", l=8))
                        self.dma([BO["v"][q]], [Bd["s_vt"]], Sc["s_vt"][b, t0:t0 + 128, :], O["v"][q][:])
                        self.dma([BO["g"][q]], [Bd["s_g"]], Sc["s_g"][b, t0:t0 + 128, :], O["g"][q][:])
                        self.dma([Bobs[q]], [Bd["s_bs"]], Sc["s_bs"][b, t0:t0 + 128, :], obs[q][:])

    def phase_BC2(self):
        I, Sc, Bd = self.I, self.Sc, self.Bd
        with contextlib.ExitStack() as es:
            hTs = [self.sb(es, "hTf", [128, 8, 512], BF16) for _ in range(2)]
            BhTs = [Buf(), Buf()]
            Wf = self.sb(es, "Wf", [128, 8, 1544], BF16)
            BW = Buf()
            wv = I["w_in"].rearrange("(kc p) n -> p kc n", p=128)
            wtmp1 = self.sb(es, "wtmp", [128, 8, 256])
            wtmp = [wtmp1, wtmp1]
            Bwt1 = Buf()
            Bwt = [Bwt1, Bwt1]
            for c in range(7):
                k = c % 2
                c0 = c * 256
                w_ = min(256, 1544 - c0)
                self.dma([], [Bwt[k]], wtmp[k][:, :, 0:w_], wv[:, :, 1792 + c0:1792 + c0 + w_])
                self.v("tensor_copy", [Bwt[k]], [BW], Wf[:, :, c0:c0 + w_], wtmp[k][:, :, 0:w_])
            NR = 4
            rot = [self.ps(es, "rot", [128, 512]) for _ in range(NR)]
            Brot = [Buf() for _ in range(NR)]
            rc = [0]

            def nxt():
                i = rc[0] % NR
                rc[0] += 1
                return rot[i], Brot[i]

            qsb = [self.sb(es, "qsb", [128, 512], BF16) for _ in range(3)]
            Bq = [Buf() for _ in range(3)]
            qc_ = [0]
            vsb = [self.sb(es, "vsb", [128, 512], BF16) for _ in range(2)]
            Bv = [Buf(), Buf()]
            nfb = self.sb(es, "nfb", [8, 2])
            Bnfb = Buf()
            self.dma([], [Bnfb], nfb[:, 0:1], I["fbias"])
            self.v("tensor_scalar", [Bnfb], [Bnfb], nfb[:, 1:2], nfb[:, 0:1], -1.0, None, ALU.mult)
            ones8 = self.sb(es, "ones8", [8, 512])
            ones8b = self.sb(es, "ones8b", [8, 512], BF16)
            Bon = Buf()
            self.v("memset", [], [Bon], ones8[:], 1.0)
            self.v("tensor_copy", [Bon], [Bon], ones8b[:], ones8[:])
            fe = self.sb(es, "fe", [8, 512])
            Bfe = Buf()
            cum = [self.sb(es, "cum", [8, 512]) for _ in range(2)]
            Bcum = [Buf(), Buf()]
            r1 = self.sb(es, "r1", [8, 512])
            Br1 = Buf()
            parts = [self.sb(es, "parts", [8, 6, 512], BF16) for _ in range(2)]
            Bparts = [Buf(), Buf()]
            def loadh(nb_):
                self.dma([Bd["s_hT"]], [BhTs[nb_ % 2]], hTs[nb_ % 2][:], Sc["s_hT"][nb_ // 8, nb_ % 8])

            loadh(0)
            for b in range(NB):
                for blk in range(8):
                    t0 = blk * 512
                    nb = b * 8 + blk
                    if nb + 1 < NB * 8:
                        loadh(nb + 1)
                    hT, BhT = hTs[nb % 2], BhTs[nb % 2]
                    for which, c_base, sname in ((0, 0, "s_qa"), (1, 512, "s_ka")):
                        for hp in range(4):
                            p, Bp = nxt()
                            for kc in range(8):
                                self.mm([BW, BhT], [Bp], p[:, :], Wf[:, kc, c_base + hp * 128: c_base + (hp + 1) * 128],
                                        hT[:, kc, :], start=(kc == 0), stop=(kc == 7))
                            qi = qc_[0] % 3
                            qc_[0] += 1
                            self.a([Bp], [Bq[qi]], qsb[qi][:], p[:, :], AF.Copy, scale=(0.125 if which == 0 else 1.0))
                            for jj in range(2):
                                self.dma([Bq[qi]], [Bd[sname]], Sc[sname][b, 2 * hp + jj, 0:64, t0:t0 + 512],
                                         qsb[qi][jj * 64:(jj + 1) * 64, :])
                            yield
                    p, Bp = nxt()
                    for kc in range(8):
                        self.mm([BW, BhT], [Bp], p[0:8, :], Wf[:, kc, 1536:1544], hT[:, kc, :],
                                start=(kc == 0), stop=(kc == 7))
                    self.a([Bp, Bnfb], [Bfe], fe[:], p[0:8, :], AF.Exp, bias=nfb[:, 1:2], scale=-1.0)
                    self.a([Bfe], [Bfe], fe[:], fe[:], AF.Ln, bias=1.0)
                    ck = nb % 2
                    init = 0.0 if blk == 0 else cum[1 - ck][:, 511:512]
                    self.v("tensor_tensor_scan", [Bfe, Bon, Bcum[1 - ck]], [Bcum[ck]], cum[ck][:], ones8[:], fe[:], init,
                           ALU.mult, ALU.subtract)
                    pt, Bpt = parts[ck], Bparts[ck]
                    self.v("tensor_copy", [Bcum[ck]], [Bpt], pt[:, 0, :], cum[ck][:])
                    self.v("tensor_tensor", [Bcum[ck], Bpt], [Br1], r1[:], cum[ck][:], pt[:, 0, :], ALU.subtract)
                    self.v("tensor_copy", [Br1], [Bpt], pt[:, 1, :], r1[:])
                    self.v("tensor_tensor", [Br1, Bpt], [Br1], r1[:], r1[:], pt[:, 1, :], ALU.subtract)
                    self.v("tensor_copy", [Br1], [Bpt], pt[:, 2, :], r1[:])
                    self.v("tensor_scalar", [Bpt], [Bpt], pt[:, 3:6, :], pt[:, 0:3, :], -1.0, None, ALU.mult)
                    for i in range(3):
                        self.dma([Bpt], [Bd["s_qa"]], Sc["s_qa"][b, :, 64 + i, t0:t0 + 512], pt[:, i, :])
                        self.dma([Bon], [Bd["s_qa"]], Sc["s_qa"][b, :, 67 + i, t0:t0 + 512], ones8b[:])
                        self.dma([Bon], [Bd["s_ka"]], Sc["s_ka"][b, :, 64 + i, t0:t0 + 512], ones8b[:])
                        self.dma([Bpt], [Bd["s_ka"]], Sc["s_ka"][b, :, 67 + i, t0:t0 + 512], pt[:, 3 + i, :])
                    yield
                    for j in range(4):
                        n = blk * 4 + j
                        p, Bp = nxt()
                        for kc in range(8):
                            self.mm([BW, BhT], [Bp], p[:, :], hT[:, kc, j * 128:(j + 1) * 128],
                                    Wf[:, kc, 1024:1536], start=(kc == 0), stop=(kc == 7))
                        self.a([Bp], [Bv[n % 2]], vsb[n % 2][:], p[:, :], AF.Copy)
                        self.dma([Bv[n % 2]], [Bd["s_fv"]], Sc["s_fv"][b, n * 128:(n + 1) * 128, :], vsb[n % 2][:])
                        yield

    def phase_EH(self):
        gE = self.phase_E()
        gH = itertools.chain(self.phase_BC2(), self.phase_H())
        doneH = False
        for _ in gE:
            if not doneH:
                try:
                    next(gH)
                except StopIteration:
                    doneH = True
        if not doneH:
            for _ in gH:
                pass

    def phase_E(self):
        Sc, Bd = self.Sc, self.Bd
        with contextlib.ExitStack() as es:
            St = [self.sb(es, "St", [128, 8, 64]) for _ in range(2)]
            BS = [Buf(), Buf()]
            self.v("memset", [], [BS[0]], St[0][:], 0.0)
            self.v("memset", [], [BS[1]], St[1][:], 0.0)
            Sp = self.sb(es, "Sp", [128, 8, 64])
            BSp = Buf()
            tmp = self.sb(es, "tmp", [128, 8, 64])
            tmp2 = self.sb(es, "tmp2", [128, 8, 64])
            Btmp, Btmp2 = Buf(), Buf()
            tmp3 = [self.sb(es, "tmp3", [128, 8, 64]) for _ in range(2)]
            tmp4 = [self.sb(es, "tmp4", [128, 8, 64]) for _ in range(3)]
            Bt3 = [Buf(), Buf()]
            Bt4 = [Buf(), Buf(), Buf()]
            sa = [self.sb(es, "sa", [128, 8]) for _ in range(2)]
            Bsa = [Buf(), Buf()]
            ops = ["s_kk", "s_w", "s_nk", "s_k", "s_r"]
            OB = {n: [self.sb(es, "ob" + n, [128, CH, 64]) for _ in range(2)] for n in ops}
            BOB = {n: [Buf(multi=True), Buf(multi=True)] for n in ops}
            vB = [self.sb(es, "vB", [128, CH, 8]) for _ in range(2)]
            BvB = [Buf(), Buf()]
            yB = [self.sb(es, "yB", [128, CH, 8]) for _ in range(2)]
            ByB = [Buf(), Buf()]
            nch = S // CH

            def load(c):
                k = c % 2
                t0 = c * CH
                gr = Grp()
                for n in ops:
                    for bh in range(16):
                        b, h = bh // 8, bh % 8
                        src = Sc[n][b, h, t0:t0 + CH, :].partition_broadcast(8)
                        self.dma([Bd[n]], [BOB[n][k]], OB[n][k][bh * 8:(bh + 1) * 8, :, :], src, grp=gr)
                self.dma([Bd["s_v"]], [BvB[k]], vB[k][:], Sc["s_v"][:, t0:t0 + CH, :], grp=gr)
                self.s.join([BOB[n][k] for n in ops] + [BvB[k]])

            def bc(ap):
                return ap.unsqueeze(1).broadcast_to([128, 8, 64])

            def poolC(t):
                c, i = t // CH, t % CH
                k = c % 2
                self.g("tensor_tensor", [BS[t % 2], BOB["s_r"][k]], [Bt4[t % 3]], tmp4[t % 3][:], St[t % 2][:],
                       bc(OB["s_r"][k][:, i, :]), ALU.mult)

            def dveY(t):
                c, i = t // CH, t % CH
                k = c % 2
                self.v("tensor_reduce", [Bt4[t % 3]], [ByB[k]], yB[k][:, i, :], tmp4[t % 3][:], AX.X, ALU.add)
                if i == CH - 1:
                    self.dma([ByB[k]], [Bd["s_y"]], Sc["s_y"][:, c * CH:(c + 1) * CH, :], yB[k][:])

            load(0)
            for c in range(nch):
                k = c % 2
                for i in range(CH):
                    if i == 1 and c + 1 < nch:
                        load(c + 1)
                    t = c * CH + i
                    q = t % 2
                    So, Sn = St[(t + 1) % 2], St[t % 2]
                    BSo, BSn = BS[(t + 1) % 2], BS[t % 2]
                    for vl in range(8):
                        self.a([BOB["s_k"][k], BvB[k]], [Bt3[q]], tmp3[q][:, vl, :], OB["s_k"][k][:, i, :], AF.Copy,
                               scale=vB[k][:, i, vl:vl + 1])
                    if t >= 1:
                        poolC(t - 1)
                    self.v("tensor_tensor", [BSo, BOB["s_kk"][k]], [Btmp], tmp[:], So[:], bc(OB["s_kk"][k][:, i, :]), ALU.mult)
                    self.v("tensor_tensor", [BSo, BOB["s_w"][k]], [BSp], Sp[:], So[:], bc(OB["s_w"][k][:, i, :]), ALU.mult)
                    self.v("tensor_reduce", [Btmp], [Bsa[q]], sa[q][:], tmp[:], AX.X, ALU.add)
                    self.v("tensor_tensor", [BSp, Bt3[q]], [BSp], Sp[:], Sp[:], tmp3[q][:], ALU.add)
                    if t >= 2:
                        dveY(t - 2)
                    self.v("tensor_tensor", [Bsa[q], BOB["s_nk"][k]], [Btmp2], tmp2[:],
                           sa[q][:].unsqueeze(2).broadcast_to([128, 8, 64]), bc(OB["s_nk"][k][:, i, :]), ALU.mult)
                    self.v("tensor_tensor", [BSp, Btmp2], [BSn], Sn[:], Sp[:], tmp2[:], ALU.add)
                    yield
            poolC(S - 1)
            dveY(S - 2)
            dveY(S - 1)

    def phase_H(self):
        I, Sc, Bd = self.I, self.Sc, self.Bd
        with contextlib.ExitStack() as es:
            mstage = self.sb(es, "mstage", [128, 128])
            maskb = self.sb(es, "maskb", [128, 128], BF16)
            Bm = Buf()
            self.dma([], [Bm], mstage[:], I["maskneg"])
            self.v("tensor_copy", [Bm], [Bm], maskb[:], mstage[:])
            qa = [self.sb(es, "qa", [70, S], BF16) for _ in range(2)]
            ka = [self.sb(es, "ka", [70, S], BF16) for _ in range(2)]
            vt = [self.sb(es, "vt", [128, 32, 65], BF16) for _ in range(2)]
            Bqa, Bka, Bvt = [Buf(), Buf()], [Buf(), Buf()], [Buf(), Buf()]
            for k in range(2):
                self.v("memset", [], [Bvt[k]], vt[k][:], 1.0)
            yf = [self.sb(es, "yf", [128, 32, 64]) for _ in range(2)]
            Byf = [Buf(), Buf()]
            NS = 3
            sps = [self.ps(es, "sps", [128, 512]) for _ in range(NS)]
            Bsps = [Buf() for _ in range(NS)]
            acc = [self.ps(es, "acc", [128, 512]) for _ in range(4)]
            Bacc = [Buf() for _ in range(4)]
            NP = 3
            pts = [self.sb(es, "pts", [128, 512], BF16) for _ in range(NP)]
            Bpts = [Buf() for _ in range(NP)]
            rec = self.sb(es, "rec", [128, 8])
            Brec = Buf()
            cnt = 0

            def load(bh):
                b, h = bh // 8, bh % 8
                k = bh % 2
                gr = Grp()
                self.dma([Bd["s_qa"]], [Bqa[k]], qa[k][:], Sc["s_qa"][b, h], grp=gr)
                self.dma([Bd["s_ka"]], [Bka[k]], ka[k][:], Sc["s_ka"][b, h], grp=gr)
                src = Sc["s_fv"][b].rearrange("(n p) c -> p n c", p=128)[:, :, h * 64:(h + 1) * 64]
                self.dma([Bd["s_fv"]], [Bvt[k]], vt[k][:, :, 0:64], src, grp=gr)
                self.s.join([Bqa[k], Bka[k], Bvt[k]])

            load(0)
            for bh in range(16):
                b, h = bh // 8, bh % 8
                k = bh % 2
                if bh + 1 < 16:
                    load(bh + 1)
                for qc in range(8):
                    for kt in range(4 * qc + 4):
                        d = kt - 4 * qc
                        si = cnt % NS
                        pi = cnt % NP
                        cnt += 1
                        sp_, Bsp = sps[si], Bsps[si]
                        lhs = ka[k][:, kt * 128:(kt + 1) * 128]
                        if d < 0:
                            c0 = 0
                            self.mm([Bka[k], Bqa[k]], [Bsp], sp_[:, 0:512], lhs, qa[k][:, qc * 512:(qc + 1) * 512])
                        else:
                            c0 = d * 128
                            q0 = qc * 512 + c0
                            self.mm([Bka[k], Bqa[k]], [Bsp], sp_[:, c0:c0 + 128], lhs, qa[k][:, q0:q0 + 128],
                                    start=True, stop=False)
                            self.mm([Bm, self.Bconst], [Bsp], sp_[:, c0:c0 + 128], self.ident_b[:], maskb[:],
                                    start=False, stop=True)
                            if c0 + 128 < 512:
                                self.mm([Bka[k], Bqa[k]], [Bsp], sp_[:, c0 + 128:512], lhs,
                                        qa[k][:, q0 + 128:qc * 512 + 512])
                        self.a([Bsp], [Bpts[pi]], pts[pi][:, c0:512], sp_[:, c0:512], AF.Exp)
                        for qs in range(max(d, 0), 4):
                            self.mm([Bpts[pi], Bvt[k]], [Bacc[qs]], acc[qs][:, 0:65], pts[pi][:, qs * 128:(qs + 1) * 128],
                                    vt[k][:, kt, :], start=(kt == 0), stop=(kt == 4 * qc + qs))
                        yield
                    for qs in range(4):
                        n = 4 * qc + qs
                        self.v("reciprocal", [Bacc[qs]], [Brec], rec[:, qs:qs + 1], acc[qs][:, 64:65])
                        self.v("tensor_scalar", [Bacc[qs], Brec], [Byf[k]], yf[k][:, n, :], acc[qs][:, 0:64],
                               rec[:, qs:qs + 1], None, ALU.mult)
                    yield
                dst = Sc["s_yf"][b].rearrange("(n p) c -> p n c", p=128)[:, :, h * 64:(h + 1) * 64]
                self.dma([Byf[k]], [Bd["s_yf"]], dst, yf[k][:])

    def phase_I(self):
        I, Sc, Bd = self.I, self.Sc, self.Bd
        es = contextlib.ExitStack()
        with es:
            pes = self.es
            self.Wts = self.sb(pes, "Wts", [128, NT, 2])
            self.OH1 = self.sb(pes, "OH1", [128, NT, 32])
            self.OH2 = self.sb(pes, "OH2", [128, NT, 32])
            self.OHb = self.sb(pes, "OHb", [128, NT, 32], BF16)
            self.BWts, self.BOH = Buf(), Buf()
            wout = self.sb(es, "wout", [128, 8, D], BF16)
            BWo = Buf()
            wv = I["w_out"].rearrange("(kc p) n -> p kc n", p=128)
            wtmp = [self.sb(es, "wtmp", [128, 8, 256]) for _ in range(2)]
            Bwt = [Buf(), Buf()]
            for c in range(4):
                k = c % 2
                self.dma([], [Bwt[k]], wtmp[k][:], wv[:, :, c * 256:(c + 1) * 256])
                self.v("tensor_copy", [Bwt[k]], [BWo], wout[:, :, c * 256:(c + 1) * 256], wtmp[k][:])
            wr = self.sb(es, "wr", [128, 8, 36])
            brb = self.sb(es, "brb", [128, 36])
            BWr = Buf()
            self.dma([], [BWr], wr[:], I["w_r"].rearrange("(kc p) n -> p kc n", p=128))
            self.dma([], [BWr], brb[:], I["b_r"][0].partition_broadcast(128))
            Pl = self.sb(es, "Pl", [128, 2, 512])
            BPl = Buf()
            self.dma([], [BPl], Pl[:, 0, :], I["rwv"][5].partition_broadcast(128))
            self.dma([], [BPl], Pl[:, 1, :], I["rwv"][6].partition_broadcast(128))
            g2b = self.sb(es, "g2b", [128, D])
            Bg2 = Buf()
            self.dma([], [Bg2], g2b[:], I["g2"][0].partition_broadcast(128))
            G1 = self.sb(es, "G1", [128, D])
            A2 = self.sb(es, "A2", [128, D])
            B2 = self.sb(es, "B2", [128, D])
            BG1, BA2, BB2 = Buf(), Buf(), Buf()
            pso = [self.ps(es, "pso", [128, 512]) for _ in range(2)]
            Bpso = [Buf(), Buf()]
            pmT = self.ps(es, "pmT", [128, 8, 128], BF16)
            BpmT = Buf()
            phT = [self.ps(es, "phT", [128, 4, 128]) for _ in range(2)]
            BphT = [Buf(), Buf()]
            pl = self.ps(es, "pl", [128, 512])
            Bpl = Buf()
            yt = [self.sb(es, "yt", [128, 512]) for _ in range(2)]
            gt = [self.sb(es, "gt", [128, 512]) for _ in range(2)]
            vtk = [self.sb(es, "vtk", [128, 512]) for _ in range(2)]
            bst = [self.sb(es, "bst", [128, 8]) for _ in range(2)]
            yft = [self.sb(es, "yft", [128, 512]) for _ in range(2)]
            xt = [self.sb(es, "xt", [128, D]) for _ in range(2)]
            Bin = [Buf(multi=True), Buf(multi=True)]
            ysq = self.sb(es, "ysq", [128, 512])
            yn = self.sb(es, "yn", [128, 512])
            bon = self.sb(es, "bon", [128, 512])
            Bw_ = Buf()
            st = self.sb(es, "st", [128, 64])
            Bst = Buf()
            mix = self.sb(es, "mix", [128, D], BF16)
            Bmix = Buf()
            mixT = self.sb(es, "mixT", [128, 8, 128], BF16)
            BmixT = Buf()
            x1 = [self.sb(es, "x1", [128, D]) for _ in range(2)]
            Bx1 = [Buf(), Buf()]
            junk = self.sb(es, "junk", [128, D])
            Bjunk = Buf()
            ss = self.sb(es, "ss", [128, 4])
            Bss = Buf()
            h2 = [self.sb(es, "h2", [128, D]) for _ in range(2)]
            Bh2 = [Buf(), Buf()]
            h2b = [self.sb(es, "h2b", [128, D], BF16) for _ in range(2)]
            Bh2b = [Buf(), Buf()]
            h2T = self.sb(es, "h2T", [128, 8, 128])
            Bh2T = Buf()
            lg = self.sb(es, "lg", [128, 36])
            rs = self.sb(es, "rs", [128, 96])
            Brs = Buf()

            def v3(ap):
                return ap.rearrange("p (h k) -> p h k", k=64)

            def b8(ap):
                return ap.unsqueeze(2).broadcast_to([128, 8, 64])

            def load(n):
                b, j = n // 32, n % 32
                t0 = j * 128
                k = n % 2
                src = Sc["s_y"][b * 64:(b + 1) * 64].rearrange("hv t l -> t hv l")[t0:t0 + 128]
                gr = Grp()
                self.dma([Bd["s_y"]], [Bin[k]], yt[k][:].rearrange("p (hv l) -> p hv l", l=8), src, grp=gr)
                self.dma([Bd["s_g"]], [Bin[k]], gt[k][:], Sc["s_g"][b, t0:t0 + 128, :], grp=gr)
                self.dma([Bd["s_vt"]], [Bin[k]], vtk[k][:], Sc["s_vt"][b, t0:t0 + 128, :], grp=gr)
                self.dma([Bd["s_bs"]], [Bin[k]], bst[k][:], Sc["s_bs"][b, t0:t0 + 128, :], grp=gr)
                self.dma([Bd["s_yf"]], [Bin[k]], yft[k][:], Sc["s_yf"][b, t0:t0 + 128, :], grp=gr)
                self.dma([], [Bin[k]], xt[k][:], I["x"][b, t0:t0 + 128, :], grp=gr)
                self.s.join([Bin[k]])

            def dup(name, shape, dt=F32):
                return [self.sb(es, name + "2", shape, dt), None]

            L2 = dict(ysq=[ysq, self.sb(es, "ysq2", [128, 512])], yn=[yn, self.sb(es, "yn2", [128, 512])],
                      bon=[bon, self.sb(es, "bon2", [128, 512])], st=[st, self.sb(es, "st2", [128, 64])],
                      mix=[mix, self.sb(es, "mix2", [128, D], BF16)], mixT=[mixT, self.sb(es, "mixT2", [128, 8, 128], BF16)],
                      junk=[junk, self.sb(es, "junk2", [128, D])], ss=[ss, self.sb(es, "ss2", [128, 4])],
                      h2T=[h2T, self.sb(es, "h2T2", [128, 8, 128])], lg=[lg, self.sb(es, "lg2", [128, 36])],
                      rs=[rs, self.sb(es, "rs2", [128, 96])], pmT=[pmT, self.ps(es, "pmT2", [128, 8, 128], BF16)],
                      pl=[pl, self.ps(es, "pl2", [128, 512])])
            B2_ = {nm: [Buf(), Buf()] for nm in ("Bw_", "Bst", "Bmix", "BmixT", "Bjunk", "Bss", "Bh2T", "Brs", "BpmT", "Bpl")}
            load(0)
            for n in range(NT):
                b, j = n // 32, n % 32
                t0 = j * 128
                k = n % 2
                ysq, yn, bon, st, mix, mixT, junk, ss, h2T, lg, rs, pmT, pl = [L2[nm][k] for nm in (
                    "ysq", "yn", "bon", "st", "mix", "mixT", "junk", "ss", "h2T", "lg", "rs", "pmT", "pl")]
                Bw_, Bst, Bmix, BmixT, Bjunk, Bss, Bh2T, Brs, BpmT, Bpl = [B2_[nm][k] for nm in (
                    "Bw_", "Bst", "Bmix", "BmixT", "Bjunk", "Bss", "Bh2T", "Brs", "BpmT", "Bpl")]
                if j == 0:
                    self.bcast_row(G1, b, 2, BG1, pso, Bpso, "copy")
                    self.bcast_row(B2, b, 3, BB2, pso, Bpso, "copy")
                    self.bcast_row(A2, b, 4, BA2, pso, Bpso, "scale", g2b, Bg2)
                if n + 1 < NT:
                    load(n + 1)
                y = yt[k]
                self.v("tensor_reduce", [Bin[k]], [Bst], st[:, 0:8], v3(y[:]), AX.X, ALU.add)
                self.g("tensor_tensor", [Bin[k]], [Bw_], ysq[:], y[:], y[:], ALU.mult)
                self.v("tensor_reduce", [Bw_], [Bst], st[:, 8:16], v3(ysq[:]), AX.X, ALU.add)
                self.v("tensor_scalar", [Bst], [Bst], st[:, 16:24], st[:, 0:8], 1.0 / 64, None, ALU.mult)
                self.v("tensor_tensor", [Bst], [Bst], st[:, 24:32], st[:, 16:24], st[:, 16:24], ALU.mult)
                self.v("scalar_tensor_tensor", [Bst], [Bst], st[:, 32:40], st[:, 8:16], 1.0 / 64, st[:, 24:32],
                       ALU.mult, ALU.subtract)
                self.a([Bst, self.Bconst], [Bst], st[:, 40:48], st[:, 32:40], AF.Sqrt, bias=self.eps6[:, 1:2], scale=1.0)
                self.v("reciprocal", [Bst], [Bst], st[:, 48:56], st[:, 40:48])
                self.v("tensor_tensor", [Bin[k], Bst], [Bw_], v3(yn[:]), v3(y[:]), b8(st[:, 16:24]), ALU.subtract)
                self.v("tensor_tensor", [Bw_, Bst], [Bw_], v3(yn[:]), v3(yn[:]), b8(st[:, 48:56]), ALU.mult)
                self.v("tensor_tensor", [Bw_, BPl], [Bw_], yn[:], yn[:], Pl[:, 0, :], ALU.mult)
                self.g("tensor_tensor", [Bw_, BPl], [Bw_], yn[:], yn[:], Pl[:, 1, :], ALU.add)
                self.g("tensor_tensor", [Bin[k]], [Bw_], v3(bon[:]), v3(vtk[k][:]), b8(bst[k][:, 0:8]), ALU.mult)
                self.v("tensor_tensor", [Bw_], [Bw_], yn[:], yn[:], bon[:], ALU.add)
                self.v("tensor_tensor", [Bw_, Bin[k]], [Bmix], mix[:, 0:512], yn[:], gt[k][:], ALU.mult)
                self.a([Bin[k]], [Bmix], mix[:, 512:1024], yft[k][:], AF.Copy)
                for kc in range(8):
                    self.tr([Bmix, self.Bconst], [BpmT], pmT[:, kc, :], mix[:, kc * 128:(kc + 1) * 128], self.ident_b[:])
                self.a([BpmT], [BmixT], mixT[:], pmT[:], AF.Copy)
                for half in range(2):
                    for kc in range(8):
                        self.mm([BmixT, BWo], [Bpso[half]], pso[half][:, :], mixT[:, kc, :],
                                wout[:, kc, half * 512:(half + 1) * 512], start=(kc == 0), stop=(kc == 7))
                    hs = slice(half * 512, (half + 1) * 512)
                    self.v("tensor_tensor", [Bpso[half], BG1], [Bjunk], junk[:, hs], pso[half][:, :], G1[:, hs], ALU.mult)
                    self.v("tensor_tensor", [Bjunk, Bin[k]], [Bx1[k]], x1[k][:, hs], junk[:, hs], xt[k][:, hs], ALU.add)
                self.dma([Bx1[k]], [Bd["s_x1"]], Sc["s_x1"][b, t0:t0 + 128, :], x1[k][:])
                rstd = self.rms_rstd(x1[k], Bx1[k], junk, Bjunk, ss, Bss)
                self.v("scalar_tensor_tensor", [Bx1[k], Bss, BA2], [Bjunk], junk[:], x1[k][:], rstd, A2[:], ALU.mult, ALU.mult)
                self.v("tensor_tensor", [Bjunk, BB2], [Bh2[k]], h2[k][:], junk[:], B2[:], ALU.add)
                self.a([Bh2[k]], [Bh2b[k]], h2b[k][:], h2[k][:], AF.Copy)
                self.dma([Bh2b[k]], [Bd["s_h2"]], Sc["s_h2"][n * 128:(n + 1) * 128, :], h2b[k][:])
                for hh in range(2):
                    for kc4 in range(4):
                        kc = hh * 4 + kc4
                        self.tr([Bh2[k], self.Bconst], [BphT[hh]], phT[hh][:, kc4, :], h2[k][:, kc * 128:(kc + 1) * 128],
                                self.ident_f[:])
                    self.a([BphT[hh]], [Bh2T], h2T[:, hh * 4:(hh + 1) * 4, :], phT[hh][:], AF.Copy)
                for kc in range(8):
                    self.mm([Bh2T, BWr], [Bpl], pl[:, 0:36], h2T[:, kc, :], wr[:, kc, :], start=(kc == 0), stop=(kc == 7))
                self.v("tensor_tensor", [Bpl, BWr], [Brs], lg[:], pl[:, 0:36], brb[:], ALU.add)
                R = [Brs]
                self.v("tensor_reduce", R, R, rs[:, 0:1], lg[:, 0:4], AX.X, ALU.max)
                self.v("tensor_scalar", R, R, rs[:, 1:2], rs[:, 0:1], -1.0, None, ALU.mult)
                self.a(R, R, rs[:, 4:8], lg[:, 0:4], AF.Exp, bias=rs[:, 1:2], scale=1.0)
                self.v("tensor_reduce", R, R, rs[:, 2:3], rs[:, 4:8], AX.X, ALU.add)
                self.v("reciprocal", R, R, rs[:, 3:4], rs[:, 2:3])
                self.v("tensor_scalar", R, R, rs[:, 8:12], lg[:, 0:4], rs[:, 0:1], None, ALU.is_equal)
                self.v("tensor_tensor", R, R, rs[:, 16:48].rearrange("p (g e) -> p g e", e=8),
                       lg[:, 4:36].rearrange("p (g e) -> p g e", e=8),
                       rs[:, 8:12].unsqueeze(2).broadcast_to([128, 4, 8]), ALU.mult)
                self.v("tensor_reduce", R, R, rs[:, 48:56], rs[:, 16:48].rearrange("p (g e) -> p e g", e=8), AX.X, ALU.add)
                self.v("max", R, R, rs[:, 56:64], rs[:, 48:56])
                self.v("tensor_scalar", R, R, rs[:, 64:72], rs[:, 48:56], rs[:, 56:57], None, ALU.is_equal)
                self.v("tensor_scalar", R, R, rs[:, 72:80], rs[:, 48:56], rs[:, 57:58], None, ALU.is_equal)
                self.v("tensor_scalar", R, R, rs[:, 80:81], rs[:, 56:57], -1.0, None, ALU.mult)
                self.a(R, R, rs[:, 81:82], rs[:, 57:58], AF.Exp, bias=rs[:, 80:81], scale=1.0)
                self.v("tensor_scalar", R, R, rs[:, 82:83], rs[:, 81:82], 1.0, None, ALU.add)
                self.v("reciprocal", R, R, rs[:, 83:84], rs[:, 82:83])
                self.v("tensor_tensor", R, [self.BWts], self.Wts[:, n, 0:1], rs[:, 3:4], rs[:, 83:84], ALU.mult)
                self.v("tensor_tensor", R + [self.BWts], [self.BWts], self.Wts[:, n, 1:2], self.Wts[:, n, 0:1], rs[:, 81:82], ALU.mult)
                gohb = rs[:, 8:12].unsqueeze(2).broadcast_to([128, 4, 8])
                self.v("tensor_tensor", R, [self.BOH], self.OH1[:, n, :].rearrange("p (g e) -> p g e", e=8), gohb,
                       rs[:, 64:72].unsqueeze(1).broadcast_to([128, 4, 8]), ALU.mult)
                self.v("tensor_tensor", R, [self.BOH], self.OH2[:, n, :].rearrange("p (g e) -> p g e", e=8), gohb,
                       rs[:, 72:80].unsqueeze(1).broadcast_to([128, 4, 8]), ALU.mult)
                self.v("tensor_tensor", [self.BOH], [self.BOH], self.OHb[:, n, :], self.OH1[:, n, :], self.OH2[:, n, :], ALU.add)

    def phase_K(self):
        I, Sc, Bd = self.I, self.Sc, self.Bd
        pes = self.es
        self.slotI = [self.sb(pes, "slotI", [128, NT], I32) for _ in range(2)]
        self.Bslot = Buf()
        with contextlib.ExitStack() as es0:
            idxG = self.sb(es0, "idxG", [128, NBLK, 8], I32)
            idxD = self.sb(es0, "idxD", [128, NBLK, 4], I32)
            Bidx = Buf()
            with contextlib.ExitStack() as es:
                lst = self.sb(es, "lst", [128, 128])
                lsb = self.sb(es, "lsb", [128, 128], BF16)
                onb = self.sb(es, "onb", [128, 128], BF16)
                Bc = Buf()
                self.dma([], [Bc], lst[:], I["lstrict"])
                self.v("tensor_copy", [Bc], [Bc], lsb[:], lst[:])
                self.v("memset", [], [Bc], onb[:], 1.0)
                thr64 = self.sb(es, "thr64", [128, NTHR])
                thr160 = self.sb(es, "thr160", [128, NBLK])
                ipk = self.sb(es, "ipk", [128, 8])
                ipf = self.sb(es, "ipf", [128, 4])
                self.dma([], [Bc], thr64[:], I["thr64"][0].partition_broadcast(128))
                self.dma([], [Bc], thr160[:], I["thr160"][0].partition_broadcast(128))
                self.dma([], [Bc], ipk[:], I["iota_pk"])
                self.dma([], [Bc], ipf[:], I["iota_pf"])
                ones64 = self.sb(es, "ones64", [128, 64])
                self.v("memset", [], [Bc], ones64[:], 1.0)
                base = self.sb(es, "base", [128, NT, 32])
                tot = self.sb(es, "tot", [128, NT, 32])
                incl = self.sb(es, "incl", [128, NT, 32])
                Bb = Buf()
                pp = [self.ps(es, "pk", [128, 512]) for _ in range(2)]
                Bpp = [Buf(), Buf()]
                OHf = self.OHb[:].rearrange("p n e -> p (n e)")
                for c in range(4):
                    self.mm([self.BOH, Bc], [Bpp[0]], pp[0][:, :], lsb[:], OHf[:, c * 512:(c + 1) * 512])
                    self.a([Bpp[0]], [Bb], base[:].rearrange("p n e -> p (n e)")[:, c * 512:(c + 1) * 512], pp[0][:, :], AF.Copy)
                    self.mm([self.BOH, Bc], [Bpp[1]], pp[1][:, :], onb[:], OHf[:, c * 512:(c + 1) * 512])
                    self.a([Bpp[1]], [Bb], tot[:].rearrange("p n e -> p (n e)")[:, c * 512:(c + 1) * 512], pp[1][:, :], AF.Copy)
                for e in range(32):
                    self.v("tensor_tensor_scan", [Bb, Bc], [Bb], incl[:, :, e], ones64[:, 0:NT], tot[:, :, e], 0.0,
                           ALU.mult, ALU.add)
                sm = self.sb(es, "smk", [128, 8, 32])
                Bsm = Buf()
                cmp = self.sb(es, "cmp", [128, 32, NTHR])
                self.v("tensor_copy", [Bb], [Bsm], sm[:, 0, :], incl[:, NT - 1, :])
                self.v("tensor_tensor", [Bsm, Bc], [Bsm], cmp[:], sm[:, 0, :].unsqueeze(2).broadcast_to([128, 32, NTHR]),
                       thr64[:].unsqueeze(1).broadcast_to([128, 32, NTHR]), ALU.is_gt)
                self.v("tensor_reduce", [Bsm], [Bsm], sm[:, 1, :], cmp[:], AX.X, ALU.add)
                self.v("tensor_scalar", [Bsm], [Bsm], sm[:, 2, :], sm[:, 1, :], float(BSZ), None, ALU.mult)
                self.v("tensor_tensor_scan", [Bsm, Bc], [Bsm], sm[:, 3, :], ones64[:, 0:32], sm[:, 2, :], 0.0,
                       ALU.mult, ALU.add)
                self.v("tensor_tensor", [Bsm], [Bsm], sm[:, 4, :], sm[:, 3, :], sm[:, 2, :], ALU.subtract)
                self.v("tensor_tensor", [Bb], [Bb], incl[:], incl[:], tot[:], ALU.subtract)
                self.v("tensor_tensor", [Bb], [Bb], base[:], base[:], incl[:], ALU.add)
                self.v("tensor_tensor", [Bb, Bsm], [Bb], base[:], base[:],
                       sm[:, 4, :].unsqueeze(1).broadcast_to([128, NT, 32]), ALU.add)
                slf = self.sb(es, "slf", [128, 2, NT])
                for kx, OHk in enumerate((self.OH1, self.OH2)):
                    self.v("tensor_tensor", [Bb, self.BOH], [Bb], tot[:], base[:], OHk[:], ALU.mult)
                    self.v("tensor_reduce", [Bb], [Bsm], slf[:, kx, :], tot[:], AX.X, ALU.add)
                    self.v("tensor_copy", [Bsm], [self.Bslot], self.slotI[kx][:], slf[:, kx, :])
                cmp2 = self.sb(es, "cmp2", [128, NBLK, 32])
                be = self.sb(es, "be", [128, NBLK])
                self.v("tensor_tensor", [Bsm, Bc], [Bsm], cmp2[:], sm[:, 3, :].unsqueeze(1).broadcast_to([128, NBLK, 32]),
                       thr160[:].unsqueeze(2).broadcast_to([128, NBLK, 32]), ALU.is_le)
                self.v("tensor_reduce", [Bsm], [Bsm], be[:], cmp2[:], AX.X, ALU.add)
                self.v("tensor_scalar", [Bsm], [Bsm], be[:], be[:], 31.0, None, ALU.min)
                fi = self.sb(es, "fi", [128, NBLK])
                self.v("tensor_scalar", [Bsm, Bc], [Bsm], fi[:], be[:], 128.0, ipk[:, 0:1], ALU.mult, ALU.add)
                self.v("tensor_copy", [Bsm], [Bidx], idxG[:, :, 0], fi[:])
                ht = [self.sb(es, "ht", [128, D], BF16) for _ in range(2)]
                Bht = [Buf(), Buf()]
                for n in range(NT):
                    k = n % 2
                    self.dma([Bd["s_h2"]], [Bht[k]], ht[k][:], Sc["s_h2"][n * 128:(n + 1) * 128, :])
                    for kx in range(2):
                        self.idma([Bht[k], self.Bslot], [Bd["s_xs"]], Sc["s_xs"],
                                  bass.IndirectOffsetOnAxis(ap=self.slotI[kx][:, n:n + 1], axis=0), ht[k][:], None)
            self.s.barrier()
            with contextlib.ExitStack() as es:
                wgS = self.sb(es, "wgS", [128, 8, 512])
                wuS = self.sb(es, "wuS", [128, 8, 512])
                wdS = self.sb(es, "wdS", [128, 4, D])
                BwgS, BwuS, BwdS = Buf(multi=True), Buf(multi=True), Buf(multi=True)
                wgB = self.sb(es, "wgB", [128, 8, 512], BF16)
                wuB = self.sb(es, "wuB", [128, 8, 512], BF16)
                wdB = self.sb(es, "wdB", [128, 4, D], BF16)
                BwgB, BwuB, BwdB = Buf(), Buf(), Buf()
                xb = [self.sb(es, "xb", [128, 4, D], BF16) for _ in range(2)]
                Bxb = [Buf(), Buf()]
                XT = self.sb(es, "XT", [128, 8, BSZ], BF16)
                BXT = Buf()
                sg = [self.sb(es, "sg", [128, 512]) for _ in range(2)]
                Bsg = [Buf(), Buf()]
                hidT = self.sb(es, "hidT", [128, 4, BSZ], BF16)
                BhidT = Buf()
                yb = [self.sb(es, "yb", [128, D]) for _ in range(2)]
                Byb = [Buf(), Buf()]
                pX = [self.ps(es, "pX", [128, 8, 128], BF16) for _ in range(2)]
                BpX = [Buf(), Buf()]
                pG = [self.ps(es, "pG", [128, 512]) for _ in range(2)]
                pU = [self.ps(es, "pU", [128, 512]) for _ in range(2)]
                pY = [self.ps(es, "pY", [128, 512]) for _ in range(2)]
                BpG, BpU, BpY = [Buf(), Buf()], [Buf(), Buf()], [Buf(), Buf()]

                def load(i):
                    g1_, g2_, g3_ = Grp(), Grp(), Grp()
                    off = bass.IndirectOffsetOnAxis(ap=idxG[:, i, 0:1], axis=0)
                    self.idma([Bidx], [BwgS], wgS[:].rearrange("p a f -> p (a f)"), None, I["wg"], off)
                    self.idma([Bidx], [BwuS], wuS[:].rearrange("p a f -> p (a f)"), None, I["wu"], off)
                    self.idma([Bidx], [BwdS], wdS[:].rearrange("p a f -> p (a f)"), None, I["wd"], off)
                    self.s.join([BwgS])
                    self.s.join([BwuS])
                    self.s.join([BwdS])
                    src = Sc["s_xs"][i * BSZ:(i + 1) * BSZ, :].rearrange("(s p) d -> p s d", p=128)
                    self.dma([Bd["s_xs"]], [Bxb[i % 2]], xb[i % 2][:], src)

                def cast(i):
                    self.v("tensor_copy", [BwgS], [BwgB], wgB[:], wgS[:])
                    self.a([BwuS], [BwuB], wuB[:], wuS[:], AF.Copy)
                    self.g("tensor_copy", [BwdS], [BwdB], wdB[:], wdS[:])

                load(0)
                cnt = 0
                for i in range(NBLK):
                    k = i % 2
                    cast(i)
                    if i + 1 < NBLK:
                        load(i + 1)
                    for sub in range(4):
                        px, Bpx = pX[sub % 2], BpX[sub % 2]
                        for kc in range(8):
                            self.tr([Bxb[k], self.Bconst], [Bpx], px[:, kc, :], xb[k][:, sub, kc * 128:(kc + 1) * 128],
                                    self.ident_b[:])
                        if sub % 2 == 0:
                            self.a([Bpx], [BXT], XT[:, :, sub * 128:(sub + 1) * 128], px[:], AF.Copy)
                        else:
                            self.v("tensor_copy", [Bpx], [BXT], XT[:, :, sub * 128:(sub + 1) * 128], px[:])
                    for fc in range(4):
                        j = fc % 2
                        for kc in range(8):
                            self.mm([BXT, BwgB], [BpG[j]], pG[j][:, :], wgB[:, kc, fc * 128:(fc + 1) * 128], XT[:, kc, :],
                                    start=(kc == 0), stop=(kc == 7))
                        for kc in range(8):
                            self.mm([BXT, BwuB], [BpU[j]], pU[j][:, :], wuB[:, kc, fc * 128:(fc + 1) * 128], XT[:, kc, :],
                                    start=(kc == 0), stop=(kc == 7))
                        self.a([BpG[j]], [Bsg[j]], sg[j][:], pG[j][:, :], AF.Silu)
                        self.v("tensor_tensor", [Bsg[j], BpU[j]], [BhidT], hidT[:, fc, :], sg[j][:], pU[j][:, :], ALU.mult)
                    for sub in range(4):
                        y_, By_ = yb[sub % 2], Byb[sub % 2]
                        for half in range(2):
                            for fc in range(4):
                                self.mm([BhidT, BwdB], [BpY[half]], pY[half][:, :], hidT[:, fc, sub * 128:(sub + 1) * 128],
                                        wdB[:, fc, half * 512:(half + 1) * 512], start=(fc == 0), stop=(fc == 3))
                            if half == 0:
                                self.a([BpY[half]], [By_], y_[:, 0:512], pY[half][:, :], AF.Copy)
                            else:
                                self.v("tensor_copy", [BpY[half]], [By_], y_[:, 512:1024], pY[half][:, :])
                        r0 = i * BSZ + sub * 128
                        self.dma([By_], [Bd["s_ys"]], Sc["s_ys"][r0:r0 + 128, :], y_[:])

    def phase_L(self):
        I, Sc, Bd = self.I, self.Sc, self.Bd
        with contextlib.ExitStack() as es:
            G2 = self.sb(es, "G2", [128, D])
            BG2 = Buf()
            gfb = self.sb(es, "gfb", [128, D])
            Bgf = Buf()
            self.dma([], [Bgf], gfb[:], I["gf"][0].partition_broadcast(128))
            pp = [self.ps(es, "pL", [128, 512]) for _ in range(2)]
            Bpp = [Buf(), Buf()]
            Y1 = [self.sb(es, "Y1", [128, D]) for _ in range(2)]
            Y2 = [self.sb(es, "Y2", [128, D]) for _ in range(2)]
            x1 = [self.sb(es, "x1", [128, D]) for _ in range(2)]
            Bin = [Buf(multi=True), Buf(multi=True)]
            ff = self.sb(es, "ff", [128, D])
            Bff = Buf()
            junk = self.sb(es, "junk", [128, D])
            Bjunk = Buf()
            ss = self.sb(es, "ss", [128, 4])
            Bss = Buf()
            ot = [self.sb(es, "ot", [128, D]) for _ in range(2)]
            Bot = [Buf(), Buf()]

            def load(n):
                b, j = n // 32, n % 32
                k = n % 2
                gr = Grp()
                self.idma([self.Bslot, Bd["s_ys"]], [Bin[k]], Y1[k][:], None, Sc["s_ys"],
                          bass.IndirectOffsetOnAxis(ap=self.slotI[0][:, n:n + 1], axis=0), grp=gr)
                self.idma([self.Bslot, Bd["s_ys"]], [Bin[k]], Y2[k][:], None, Sc["s_ys"],
                          bass.IndirectOffsetOnAxis(ap=self.slotI[1][:, n:n + 1], axis=0), grp=gr)
                self.dma([Bd["s_x1"]], [Bin[k]], x1[k][:], Sc["s_x1"][b, j * 128:(j + 1) * 128, :])
                self.s.join([Bin[k]])

            ffL = [ff, self.sb(es, "ff2", [128, D])]
            junkL = [junk, self.sb(es, "junkL2", [128, D])]
            ssL = [ss, self.sb(es, "ssL2", [128, 4])]
            BL_ = {nm: [Buf(), Buf()] for nm in ("Bff", "Bjunk", "Bss")}
            load(0)
            for n in range(NT):
                b, j = n // 32, n % 32
                k = n % 2
                ff, junk, ss = ffL[k], junkL[k], ssL[k]
                Bff, Bjunk, Bss = BL_["Bff"][k], BL_["Bjunk"][k], BL_["Bss"][k]
                if j == 0:
                    self.bcast_row(G2, b, 5, BG2, pp, Bpp, "copy")
                if n + 1 < NT:
                    load(n + 1)
                self.v("tensor_scalar", [Bin[k], self.BWts], [Bff], ff[:], Y1[k][:], self.Wts[:, n, 0:1], None, ALU.mult)
                self.v("scalar_tensor_tensor", [Bin[k], self.BWts, Bff], [Bff], ff[:], Y2[k][:], self.Wts[:, n, 1:2], ff[:],
                       ALU.mult, ALU.add)
                self.v("tensor_tensor", [Bff, BG2], [Bff], ff[:], ff[:], G2[:], ALU.mult)
                self.v("tensor_tensor", [Bff, Bin[k]], [Bff], ff[:], ff[:], x1[k][:], ALU.add)
                rstd = self.rms_rstd(ff, Bff, junk, Bjunk, ss, Bss)
                self.v("scalar_tensor_tensor", [Bff, Bss, Bgf], [Bot[k]], ot[k][:], ff[:], rstd, gfb[:], ALU.mult, ALU.mult)
                self.dma([Bot[k]], [self.Bout], self.out[b, j * 128:(j + 1) * 128, :], ot[k][:])


def _host_inputs(inp):
    f = np.float32
    c = np.ascontiguousarray
    p = np.arange(128)
    ident = np.eye(128, dtype=f)
    maskneg = np.where(p[:, None] > p[None, :], f(-30000.0), f(0.0)).astype(f)
    lstrict = (p[:, None] < p[None, :]).astype(f)
    sel2 = np.zeros((2, 256), f)
    sel2[0, 0:128] = 1.0
    sel2[1, 128:256] = 1.0
    thr64 = (float(BSZ) * np.arange(NTHR, dtype=f))[None, :]
    thr160 = (float(BSZ) * np.arange(NBLK, dtype=f))[None, :]
    iota_pk = (p[:, None] + 128 * np.arange(8)[None, :]).astype(f)
    iota_pf = (p[:, None] + 128 * np.arange(4)[None, :]).astype(f)
    rwv = np.stack([inp["rwkv_w0"][0], inp["rwkv_a0"][0], inp["rwkv_k_k"][0], inp["rwkv_k_a"][0],
                    inp["rwkv_r_k"][0].reshape(512), inp["rwkv_lnx_g"][0], inp["rwkv_lnx_b"][0]]).astype(f)
    shared = {
        "w_ada": c(inp["w_ada"][0]), "b_ada": c(inp["b_ada"]), "g1": c(inp["norm1_g"]), "g2": c(inp["norm2_g"]),
        "gf": c(inp["norm_f_g"][None, :]), "w_in": c(inp["w_in"][0]), "mu": c(inp["rwkv_mu"]), "rwv": c(rwv),
        "w_up": c(inp["rwkv_w_up"][0]), "a_up": c(inp["rwkv_a_up"][0]), "g_up": c(inp["rwkv_g_up"][0]),
        "fbias": c(inp["fox_f_bias"][0][:, None]), "w_out": c(inp["w_out"][0]),
        "w_r": c(np.concatenate([inp["moe_w_grp"][0], inp["moe_w_rt"][0]], axis=1)),
        "b_r": c(np.concatenate([inp["moe_b_grp"][0], inp["moe_b_rt"][0]])[None, :]),
        "wg": c(inp["moe_w_gate"][0].reshape(32, 8, 128, 512).transpose(0, 2, 1, 3).reshape(32 * 128, 8 * 512)),
        "wu": c(inp["moe_w_up"][0].reshape(32, 8, 128, 512).transpose(0, 2, 1, 3).reshape(32 * 128, 8 * 512)),
        "wd": c(inp["moe_w_down"][0].reshape(32, 4, 128, 1024).transpose(0, 2, 1, 3).reshape(32 * 128, 4 * 1024)),
        "ident": ident, "maskneg": maskneg, "lstrict": lstrict, "sel2": sel2, "thr64": thr64, "thr160": thr160,
        "iota_pk": iota_pk, "iota_pf": iota_pf,
    }
    maps = []
    for i in range(NCORES):
        m = dict(shared)
        m["x"] = c(inp["x"][NB * i:NB * (i + 1)])
        cc = inp["c"][NB * i:NB * (i + 1)]
        m["cT"] = c(cc.reshape(NB, 8, 128).transpose(2, 1, 0))
        maps.append(m)
    return maps


def kernel(**inputs):
    inp = {k: np.asarray(v, dtype=np.float32) for k, v in inputs.items()}
    maps = _host_inputs(inp)
    nc = K().build()
    res = run_bass_kernel_spmd(nc, maps, core_ids=list(range(NCORES)))
    return np.concatenate([np.asarray(r["out"]) for r in res.results], axis=0).astype(np.float32)
```

```python
import contextlib
import itertools
import numpy as np
import concourse.bass as bass
import concourse.mybir as mybir
from concourse.bass_utils import run_bass_kernel_spmd

F32 = mybir.dt.float32
BF16 = mybir.dt.bfloat16
I32 = mybir.dt.uint32
AF = mybir.ActivationFunctionType
ALU = mybir.AluOpType
AX = mybir.AxisListType

NCORES = 8
S = 4096
D = 1024
NB = 2
T = NB * S
NT = T // 128
NBLK = 64
BSZ = 512
NTHR = T // BSZ
CH = 32

STOP_AFTER = None
DEBUG = False


MULTI = []


class Buf:
    __slots__ = ("name", "w", "r", "const", "multi", "ws")

    def __init__(self, name="", const=False, multi=False):
        self.name = name
        self.w = None
        self.r = []
        self.const = const
        self.multi = multi
        self.ws = []
        if multi:
            MULTI.append(self)


class Op:
    __slots__ = ("eng", "fn", "deps", "needs", "ev", "dma", "grp")


class Grp:
    __slots__ = ("key", "last")

    def __init__(self):
        self.key = None
        self.last = None


JOIN = "join"


class Sched:
    COMPUTE = ("pe", "dve", "act", "pool")

    def __init__(self, nc, es):
        self.nc = nc
        self.engobj = dict(pe=nc.tensor, dve=nc.vector, act=nc.scalar, pool=nc.gpsimd, sp=nc.sync)
        self.sems = []
        self.semid = {}
        for e in self.COMPUTE + ("sp",):
            self.semid[e] = len(self.sems)
            self.sems.append(es.enter_context(nc.semaphore("s_" + e)))
        self.dsem = {}
        for q, k in (("sp", 32), ("pool", 16)):
            ids = []
            for i in range(k):
                ids.append(len(self.sems))
                self.sems.append(es.enter_context(nc.semaphore(f"d_{q}{i}")))
            self.dsem[q] = ids
        self.ops = []
        self.dcount = {"sp": 0, "pool": 0}
        self.dlast = {}
        self.last = {}

    def _mk(self, eng, fn, reads, writes, dma, grp=None, first=True):
        o = Op()
        o.eng = eng
        o.fn = fn
        o.needs = False
        o.ev = None
        o.dma = dma
        o.grp = grp
        deps = {}
        for b in reads:
            if b.multi:
                for p in b.ws:
                    deps[id(p)] = (p, True)
            elif b.w is not None:
                deps[id(b.w)] = (b.w, True)
        for b in writes:
            if not b.multi and b.w is not None and id(b.w) not in deps:
                deps[id(b.w)] = (b.w, False)
            for r in b.r:
                if id(r) not in deps:
                    deps[id(r)] = (r, False)
        out = []
        for p, raw in deps.values():
            if p is o:
                continue
            if dma is None and p.dma is None and p.eng == eng:
                if eng == "pe" or not raw:
                    continue
            out.append(p)
        if dma is not None:
            prev = self.dlast.get(dma)
            if prev is not None and first:
                out.append(prev)
            self.dlast[dma] = o
            if grp is not None:
                grp.last = o
        for p in out:
            p.needs = True
        o.deps = out
        for b in reads:
            if not b.const and not (fn is JOIN):
                b.r.append(o)
        for b in writes:
            if b.multi:
                if b.r or fn is JOIN:
                    b.ws = [o]
                else:
                    b.ws.append(o)
            b.w = o
            b.r = []
        self.ops.append(o)
        if dma is None:
            self.last[eng] = o
        return o

    def op(self, eng, fn, reads=(), writes=()):
        return self._mk(eng, fn, reads, writes, None)

    def dma(self, fn, reads=(), writes=(), q="sp", grp=None):
        if grp is not None and grp.key is not None:
            assert grp.key[0] == q
            return self._mk(q, fn, reads, writes, grp.key, grp, first=False)
        k = self.dcount[q]
        self.dcount[q] = k + 1
        ids = self.dsem[q]
        key = (q, k % len(ids))
        if grp is not None:
            grp.key = key
        return self._mk(q, fn, reads, writes, key, grp)

    def join(self, bufs):
        return self._mk("sp", JOIN, bufs, bufs, None)

    def barrier(self):
        for b in MULTI:
            b.ws = []
            b.r = []
            b.w = None
        lastops = [o for o in self.last.values()] + list(self.dlast.values())
        for e in ("pe", "dve", "act", "pool", "sp"):
            o = Op()
            o.eng = e
            o.fn = None
            o.needs = False
            o.ev = None
            o.dma = None
            o.grp = None
            o.deps = [p for p in lastops if not (p.dma is None and p.eng == e and e == "pe")]
            for p in o.deps:
                p.needs = True
            self.ops.append(o)

    def emit(self):
        cnt = {}
        for o in self.ops:
            if o.fn is None:
                continue
            if o.dma is not None:
                sid = self.dsem[o.dma[0]][o.dma[1]]
                cnt[sid] = cnt.get(sid, 0) + 16
                o.ev = (sid, cnt[sid])
            elif o.needs:
                sid = self.semid[o.eng]
                cnt[sid] = cnt.get(sid, 0) + 1
                o.ev = (sid, cnt[sid])
        waited = {e: {} for e in self.engobj}
        nwait = 0
        for o in self.ops:
            E = self.engobj[o.eng]
            w = waited[o.eng]
            need = {}
            for p in o.deps:
                sid, v = p.ev if p.grp is None else p.grp.last.ev
                if w.get(sid, 0) < v and need.get(sid, 0) < v:
                    need[sid] = v
            for sid, v in need.items():
                E.wait_ge(self.sems[sid], v)
                w[sid] = v
                nwait += 1
            if o.fn is JOIN:
                if o.needs:
                    E.sem_inc(self.sems[o.ev[0]], 1)
            elif o.fn is not None:
                inst = o.fn()
                if o.dma is not None:
                    inst.then_inc(self.sems[o.ev[0]], 16)
                elif o.needs:
                    inst.then_inc(self.sems[o.ev[0]], 1)
        return nwait


class K:
    def __init__(self):
        self.nc = bass.Bass("TRN2", target_bir_lowering=False)
        self.es = contextlib.ExitStack()
        self.s = Sched(self.nc, self.es)
        self.nbuf = 0

    def din(self, name, shape, dt=F32):
        return self.nc.dram_tensor(name, list(shape), dt, kind="ExternalInput").ap()

    def dscr(self, name, shape, dt=F32):
        kind = "ExternalOutput" if (DEBUG and name in DEBUG) else "Internal"
        return self.nc.dram_tensor(name, list(shape), dt, kind=kind).ap()

    def sb(self, es, name, shape, dt=F32):
        self.nbuf += 1
        return es.enter_context(self.nc.sbuf_tensor(f"{name}_{self.nbuf}", list(shape), dt))

    def ps(self, es, name, shape, dt=F32):
        self.nbuf += 1
        return es.enter_context(self.nc.psum_tensor(f"{name}_{self.nbuf}", list(shape), dt))

    def v(self, name, R, W, *a, **kw):
        f = getattr(self.nc.vector, name)
        return self.s.op("dve", lambda: f(*a, **kw), R, W)

    def g(self, name, R, W, *a, **kw):
        f = getattr(self.nc.gpsimd, name)
        return self.s.op("pool", lambda: f(*a, **kw), R, W)

    def a(self, R, W, out, in_, func, bias=None, scale=None):
        kw = {}
        if bias is not None:
            kw["bias"] = bias
        if scale is not None:
            kw["scale"] = scale
        f = self.nc.scalar.activation
        return self.s.op("act", lambda: f(out, in_, func, **kw), R, W)

    def mm(self, R, W, out, lhsT, rhs, start=True, stop=True):
        f = self.nc.tensor.matmul
        return self.s.op("pe", lambda: f(out, lhsT, rhs, start=start, stop=stop), R, W)

    def tr(self, R, W, out, in_, ident):
        f = self.nc.tensor.transpose
        return self.s.op("pe", lambda: f(out, in_, ident), R, W)

    def dma(self, R, W, out, in_, q="sp", grp=None, **kw):
        f = self.engobj(q).dma_start
        return self.s.dma(lambda: f(out=out, in_=in_, **kw), R, W, q=q)

    def engobj(self, q):
        return self.s.engobj[q]

    def idma(self, R, W, out, out_off, in_, in_off, grp=None):
        f = self.nc.gpsimd.indirect_dma_start
        return self.s.dma(lambda: f(out, out_off, in_, in_off), R, W, q="pool")

    def build(self):
        nc = self.nc
        es = self.es
        I = {}
        I["x"] = self.din("x", [NB, S, D])
        I["cT"] = self.din("cT", [128, 8, NB])
        I["w_ada"] = self.din("w_ada", [D, 6 * D])
        I["b_ada"] = self.din("b_ada", [1, 6 * D])
        I["g1"] = self.din("g1", [1, D])
        I["g2"] = self.din("g2", [1, D])
        I["gf"] = self.din("gf", [1, D])
        I["w_in"] = self.din("w_in", [D, 3336])
        I["mu"] = self.din("mu", [1, 1792])
        I["rwv"] = self.din("rwv", [7, 512])
        I["w_up"] = self.din("w_up", [64, 512])
        I["a_up"] = self.din("a_up", [64, 512])
        I["g_up"] = self.din("g_up", [128, 512])
        I["fbias"] = self.din("fbias", [8, 1])
        I["w_out"] = self.din("w_out", [D, D])
        I["w_r"] = self.din("w_r", [D, 36])
        I["b_r"] = self.din("b_r", [1, 36])
        I["wg"] = self.din("wg", [32 * 128, 8 * 512])
        I["wu"] = self.din("wu", [32 * 128, 8 * 512])
        I["wd"] = self.din("wd", [32 * 128, 4 * 1024])
        I["ident"] = self.din("ident", [128, 128])
        I["maskneg"] = self.din("maskneg", [128, 128])
        I["lstrict"] = self.din("lstrict", [128, 128])
        I["sel2"] = self.din("sel2", [2, 256])
        I["thr64"] = self.din("thr64", [1, NTHR])
        I["thr160"] = self.din("thr160", [1, NBLK])
        I["iota_pk"] = self.din("iota_pk", [128, 8])
        I["iota_pf"] = self.din("iota_pf", [128, 4])
        self.I = I
        self.out = nc.dram_tensor("out", [NB, S, D], F32, kind="ExternalOutput").ap()
        Sc = {}
        for n in ("s_r", "s_w", "s_k", "s_kk", "s_nk"):
            Sc[n] = self.dscr(n, [NB, 8, S, 64])
        Sc["s_v"] = self.dscr("s_v", [NB * 64, S, 8])
        Sc["s_y"] = self.dscr("s_y", [NB * 64, S, 8])
        Sc["s_g"] = self.dscr("s_g", [NB, S, 512])
        Sc["s_vt"] = self.dscr("s_vt", [NB, S, 512])
        Sc["s_bs"] = self.dscr("s_bs", [NB, S, 8])
        Sc["s_qa"] = self.dscr("s_qa", [NB, 8, 70, S], BF16)
        Sc["s_ka"] = self.dscr("s_ka", [NB, 8, 70, S], BF16)
        Sc["s_fv"] = self.dscr("s_fv", [NB, S, 512], BF16)
        Sc["s_yf"] = self.dscr("s_yf", [NB, S, 512])
        Sc["s_hT"] = self.dscr("s_hT", [NB, 8, 128, 8, 512], BF16)
        Sc["s_x1"] = self.dscr("s_x1", [NB, S, D])
        Sc["s_h2"] = self.dscr("s_h2", [T, D], BF16)
        Sc["s_xs"] = self.dscr("s_xs", [NBLK * BSZ, D], BF16)
        Sc["s_ys"] = self.dscr("s_ys", [NBLK * BSZ, D])
        self.Sc = Sc
        self.Bd = {n: Buf(n, multi=True) for n in Sc}
        self.Bout = Buf("out", multi=True)

        self.modrow = self.sb(es, "modrow", [2, 6 * D])
        self.Bmod = Buf("modrow")
        self.ident_f = self.sb(es, "ident_f", [128, 128])
        self.ident_b = self.sb(es, "ident_b", [128, 128], BF16)
        self.sel2 = self.sb(es, "sel2", [2, 256])
        self.Bconst = Buf("const")
        self.dma([], [self.Bconst], self.ident_f[:], I["ident"])
        self.dma([], [self.Bconst], self.sel2[:], I["sel2"])
        self.v("tensor_copy", [self.Bconst], [self.Bconst], self.ident_b[:], self.ident_f[:])

        phases = ["A", "BC1", "EH", "I", "K", "L"]
        for ph in phases:
            getattr(self, "phase_" + ph)()
            self.s.barrier()
            if STOP_AFTER == ph:
                break
        nw = self.s.emit()
        self.es.close()
        return nc

    def bcast_row(self, dst, b, j, Bdst, pp, Bpp, mode, gb=None, Bg=None):
        for half in range(2):
            p = pp[half]
            self.mm([self.Bmod, self.Bconst], [Bpp[half]], p[:, :], self.sel2[0:2, b * 128:(b + 1) * 128],
                    self.modrow[0:2, j * D + half * 512: j * D + half * 512 + 512])
            if mode == "copy":
                self.a([Bpp[half]], [Bdst], dst[:, half * 512:(half + 1) * 512], p[:, :], AF.Copy)
            else:
                self.v("scalar_tensor_tensor", [Bpp[half], Bg], [Bdst], dst[:, half * 512:(half + 1) * 512],
                       p[:, :], 1.0, gb[:, half * 512:(half + 1) * 512], ALU.add, ALU.mult)

    def rms_rstd(self, xt, Bx, junk, Bj, ss, Bs):
        self.g("tensor_tensor", [Bx], [Bj], junk[:], xt[:], xt[:], ALU.mult)
        self.v("tensor_reduce", [Bj], [Bs], ss[:, 0:1], junk[:], AX.X, ALU.add)
        self.a([Bs], [Bs], ss[:, 1:2], ss[:, 0:1], AF.Sqrt, bias=self.eps6[:, 0:1], scale=1.0 / D)
        self.v("reciprocal", [Bs], [Bs], ss[:, 2:3], ss[:, 1:2])
        return ss[:, 2:3]

    def phase_A(self):
        I = self.I
        with contextlib.ExitStack() as es:
            condT = self.sb(es, "condT", [128, 8, NB])
            Bc = Buf()
            self.dma([], [Bc], condT[:], I["cT"])
            self.a([Bc], [Bc], condT[:], condT[:], AF.Silu)
            b2 = self.sb(es, "b_ada2", [2, 6 * D])
            Bb2 = Buf()
            self.dma([], [Bb2], b2[:], I["b_ada"][0].partition_broadcast(2))
            wv = I["w_ada"].rearrange("(kc p) n -> p kc n", p=128)
            wt = [self.sb(es, "wada", [128, 8, 512]) for _ in range(2)]
            Bw = [Buf(), Buf()]
            pp = [self.ps(es, "psA", [2, 512]) for _ in range(2)]
            Bp = [Buf(), Buf()]
            for j in range(12):
                k = j % 2
                self.dma([], [Bw[k]], wt[k][:], wv[:, :, j * 512:(j + 1) * 512])
                for kc in range(8):
                    self.mm([Bc, Bw[k]], [Bp[k]], pp[k][:, :], condT[:, kc, :], wt[k][:, kc, :],
                            start=(kc == 0), stop=(kc == 7))
                self.v("tensor_tensor", [Bp[k], Bb2], [self.Bmod], self.modrow[0:2, j * 512:(j + 1) * 512],
                       pp[k][:, :], b2[:, j * 512:(j + 1) * 512], ALU.add)
        self.eps6 = self.sb(self.es, "eps6", [128, 2])
        self.v("memset", [], [self.Bconst], self.eps6[:, 0:1], 1e-6)
        self.v("memset", [], [self.Bconst], self.eps6[:, 1:2], 64e-5)

    def make_hT(self, es_bufs, b, blk):
        Bf = es_bufs
        I = self.I
        k = (b * 8 + blk) % 2
        hT, BhT = Bf["hT"][k], Bf["BhT"][k]
        if blk == 0:
            self.v("memset", [], [BhT], hT[:, :, 0:4], 0.0)
        else:
            hp, Bhp = Bf["hT"][1 - k], Bf["BhT"][1 - k]
            self.v("tensor_copy", [Bhp], [BhT], hT[:, :, 3:4], hp[:, :, 515:516])
        for j in range(4):
            n = blk * 4 + j
            t0 = n * 128
            kk = n % 2
            xt, Bx = Bf["xt"][kk], Bf["Bxt"][kk]
            self.dma([], [Bx], xt[:], I["x"][b, t0:t0 + 128, :])
            rstd = self.rms_rstd(xt, Bx, Bf["junk"], Bf["Bjunk"], Bf["ss"][kk], Bf["Bss"][kk])
            self.v("scalar_tensor_tensor", [Bx, Bf["Bss"][kk], Bf["BA1"]], [Bf["Bjunk"]], Bf["junk"][:], xt[:], rstd,
                   Bf["A1"][:], ALU.mult, ALU.mult)
            hb, Bhb = Bf["hb"][kk], Bf["Bhb"][kk]
            self.v("tensor_tensor", [Bf["Bjunk"], Bf["BB1"]], [Bhb], hb[:], Bf["junk"][:], Bf["B1"][:], ALU.add)
            pT, BpT = Bf["pT"][kk], Bf["BpT"][kk]
            for kc in range(8):
                self.tr([Bhb, self.Bconst], [BpT], pT[:, kc, :], hb[:, kc * 128:(kc + 1) * 128], self.ident_b[:])
            self.a([BpT], [BhT], hT[:, :, 4 + j * 128: 4 + (j + 1) * 128], pT[:, :, :], AF.Copy)
        return hT, BhT

    def alloc_hT_bufs(self, es):
        Bf = {}
        Bf["hT"] = [self.sb(es, "hT", [128, 8, 516], BF16) for _ in range(2)]
        Bf["BhT"] = [Buf(), Buf()]
        Bf["xt"] = [self.sb(es, "xt", [128, D]) for _ in range(2)]
        Bf["Bxt"] = [Buf(), Buf()]
        Bf["junk"] = self.sb(es, "junk", [128, D])
        Bf["Bjunk"] = Buf()
        Bf["ss"] = [self.sb(es, "ss", [128, 4]) for _ in range(2)]
        Bf["Bss"] = [Buf(), Buf()]
        Bf["hb"] = [self.sb(es, "hb", [128, D], BF16) for _ in range(2)]
        Bf["Bhb"] = [Buf(), Buf()]
        Bf["pT"] = [self.ps(es, "pT", [128, 8, 128], BF16) for _ in range(2)]
        Bf["BpT"] = [Buf(), Buf()]
        Bf["A1"] = self.sb(es, "A1", [128, D])
        Bf["B1"] = self.sb(es, "B1", [128, D])
        Bf["BA1"] = Buf()
        Bf["BB1"] = Buf()
        Bf["g1b"] = self.sb(es, "g1b", [128, D])
        Bf["Bg1b"] = Buf()
        self.dma([], [Bf["Bg1b"]], Bf["g1b"][:], self.I["g1"][0].partition_broadcast(128))
        return Bf

    def set_AB1(self, Bf, b, pp, Bpp):
        self.bcast_row(Bf["B1"], b, 0, Bf["BB1"], pp, Bpp, "copy")
        self.bcast_row(Bf["A1"], b, 1, Bf["BA1"], pp, Bpp, "scale", Bf["g1b"], Bf["Bg1b"])

    def phase_BC1(self):
        I, Sc, Bd = self.I, self.Sc, self.Bd
        with contextlib.ExitStack() as es:
            Bf = self.alloc_hT_bufs(es)
            W1 = self.sb(es, "W1", [128, 8, 1792], BF16)
            W2 = self.sb(es, "W2", [128, 8, 1792], BF16)
            BW = Buf()
            P = {}
            BP = Buf()
            for i, n in enumerate(["w0", "a0", "kk", "ka", "rk"]):
                P[n] = self.sb(es, "P" + n, [128, 512])
                self.dma([], [BP], P[n][:], I["rwv"][i].partition_broadcast(128))
            P["omka"] = self.sb(es, "Pomka", [128, 512])
            self.v("tensor_scalar", [BP], [BP], P["omka"][:], P["ka"][:], -1.0, 1.0, ALU.mult, ALU.add)
            lora = self.sb(es, "lora", [128, 3, 512], BF16)
            BL = Buf()
            es1 = contextlib.ExitStack()
            es_main = es
            es = es1
            mub = self.sb(es, "mub", [128, 1792])
            omub = self.sb(es, "omub", [128, 1792])
            Bmu = Buf()
            self.dma([], [Bmu], mub[:], I["mu"][0].partition_broadcast(128))
            self.v("tensor_scalar", [Bmu], [Bmu], omub[:], mub[:], -1.0, 1.0, ALU.mult, ALU.add)
            wv = I["w_in"].rearrange("(kc p) n -> p kc n", p=128)
            wtmp = [self.sb(es, "wtmp", [128, 8, 256]) for _ in range(2)]
            Bwt = [Buf(), Buf()]
            for c in range(7):
                k = c % 2
                c0 = c * 256
                self.dma([], [Bwt[k]], wtmp[k][:], wv[:, :, c0:c0 + 256])
                self.v("tensor_tensor", [Bwt[k], Bmu], [BW], W1[:, :, c0:c0 + 256], wtmp[k][:],
                       omub[:, c0:c0 + 256].unsqueeze(1).broadcast_to([128, 8, 256]), ALU.mult)
                self.g("tensor_tensor", [Bwt[k], Bmu], [BW], W2[:, :, c0:c0 + 256], wtmp[k][:],
                       mub[:, c0:c0 + 256].unsqueeze(1).broadcast_to([128, 8, 256]), ALU.mult)
            stage = self.sb(es, "lstage", [128, 3, 512])
            self.v("memset", [], [BL], stage[:], 0.0)
            self.dma([], [BL], stage[0:64, 0, :], I["w_up"])
            self.dma([], [BL], stage[64:128, 1, :], I["a_up"])
            self.dma([], [BL], stage[:, 2, :], I["g_up"])
            self.v("tensor_copy", [BL], [BL], lora[:], stage[:])
            self.s.barrier()
            es1.close()
            es = es_main
            NR = 5
            rot = [self.ps(es, "rot", [128, 512]) for _ in range(NR)]
            Brot = [Buf() for _ in range(NR)]
            rc = [0]

            def nxt():
                i = rc[0] % NR
                rc[0] += 1
                return rot[i], Brot[i]

            twd = self.sb(es, "twd", [128, 512], BF16)
            sgd = self.sb(es, "sgd", [128, 512], BF16)
            Btwd, Bsgd = Buf(), Buf()
            names = ["r", "w", "k", "v", "kk", "nk", "g"]
            O = {n: [self.sb(es, "o_" + n, [128, 512]) for _ in range(2)] for n in names}
            BO = {n: [Buf(), Buf()] for n in names}
            obs = [self.sb(es, "o_bs", [128, 8]) for _ in range(2)]
            Bobs = [Buf(), Buf()]
            tmpsL = [{n: self.sb(es, "t_" + n, [128, 512]) for n in ["zw", "a", "kku", "sq", "t1", "rk"]} for _ in range(2)]
            BtL = [{n: Buf() for n in tmpsL[0]} for _ in range(2)]
            smL = [self.sb(es, "sm", [128, 32]) for _ in range(2)]
            BsmL = [Buf(), Buf()]

            for b in range(NB):
                self.set_AB1(Bf, b, [rot[0], rot[1]], [Brot[0], Brot[1]])
                for blk in range(8):
                    hT, BhT = self.make_hT(Bf, b, blk)
                    self.dma([BhT], [Bd["s_hT"]], Sc["s_hT"][b, blk], hT[:, :, 4:516])
                    for ch in (12, 13):
                        p, Bp = nxt()
                        for kc in range(8):
                            self.mm([BW, BhT], [Bp], p[:, :], W1[:, kc, ch * 128:(ch + 1) * 128], hT[:, kc, 4:516],
                                    start=(kc == 0), stop=False)
                            self.mm([BW, BhT], [Bp], p[:, :], W2[:, kc, ch * 128:(ch + 1) * 128], hT[:, kc, 3:515],
                                    start=False, stop=(kc == 7))
                        if ch == 12:
                            self.a([Bp], [Btwd], twd[0:64, :], p[0:64, :], AF.Tanh)
                            self.a([Bp], [Btwd], twd[64:128, :], p[64:128, :], AF.Copy)
                        else:
                            self.a([Bp], [Bsgd], sgd[:, :], p[:, :], AF.Sigmoid)
                    for j in range(4):
                        n = blk * 4 + j
                        t0 = n * 128
                        q = n % 2
                        tmps, Bt, sm, Bsm = tmpsL[q], BtL[q], smL[q], BsmL[q]
                        cur = lambda kc: hT[:, kc, 4 + j * 128: 4 + (j + 1) * 128]
                        prv = lambda kc: hT[:, kc, 3 + j * 128: 3 + (j + 1) * 128]

                        def proj(c0):
                            p, Bp = nxt()
                            for kc in range(8):
                                self.mm([BW, BhT], [Bp], p[:, :], cur(kc), W1[:, kc, c0:c0 + 512],
                                        start=(kc == 0), stop=False)
                                self.mm([BW, BhT], [Bp], p[:, :], prv(kc), W2[:, kc, c0:c0 + 512],
                                        start=False, stop=(kc == 7))
                            return p, Bp

                        p_r, Bp_r = proj(0)
                        self.a([Bp_r], [BO["r"][q]], O["r"][q][:], p_r[:, :], AF.Copy)
                        p_v, Bp_v = proj(1024)
                        self.a([Bp_v], [BO["v"][q]], O["v"][q][:], p_v[:, :], AF.Copy)
                        p_w, Bp_w = nxt()
                        self.mm([Btwd, BL], [Bp_w], p_w[:, :], twd[0:64, j * 128:(j + 1) * 128], lora[0:64, 0, :])
                        self.v("tensor_tensor", [Bp_w, BP], [Bt["zw"]], tmps["zw"][:], p_w[:, :], P["w0"][:], ALU.add)
                        self.a([Bt["zw"]], [Bt["zw"]], tmps["zw"][:], tmps["zw"][:], AF.Sigmoid)
                        self.a([Bt["zw"]], [BO["w"][q]], O["w"][q][:], tmps["zw"][:], AF.Exp, scale=-0.6065306597126334)
                        p_a, Bp_a = nxt()
                        self.mm([Btwd, BL], [Bp_a], p_a[:, :], twd[64:128, j * 128:(j + 1) * 128], lora[64:128, 1, :])
                        self.v("tensor_tensor", [Bp_a, BP], [Bt["a"]], tmps["a"][:], p_a[:, :], P["a0"][:], ALU.add)
                        self.a([Bt["a"]], [Bt["a"]], tmps["a"][:], tmps["a"][:], AF.Sigmoid)
                        p_k, Bp_k = proj(512)
                        self.v("tensor_tensor", [Bp_k, BP], [Bt["kku"]], tmps["kku"][:], p_k[:, :], P["kk"][:], ALU.mult)
                        self.g("tensor_tensor", [Bt["kku"]], [Bt["sq"]], tmps["sq"][:], tmps["kku"][:], tmps["kku"][:], ALU.mult)
                        self.v("tensor_reduce", [Bt["sq"]], [Bsm], sm[:, 0:8],
                               tmps["sq"][:].rearrange("p (h k) -> p h k", k=64), AX.X, ALU.add)
                        self.a([Bsm], [Bsm], sm[:, 8:16], sm[:, 0:8], AF.Sqrt)
                        self.v("tensor_scalar", [Bsm], [Bsm], sm[:, 8:16], sm[:, 8:16], 1e-12, None, ALU.max)
                        self.v("reciprocal", [Bsm], [Bsm], sm[:, 16:24], sm[:, 8:16])
                        self.v("tensor_tensor", [Bt["kku"], Bsm], [BO["kk"][q]],
                               O["kk"][q][:].rearrange("p (h k) -> p h k", k=64),
                               tmps["kku"][:].rearrange("p (h k) -> p h k", k=64),
                               sm[:, 16:24].unsqueeze(2).broadcast_to([128, 8, 64]), ALU.mult)
                        self.v("tensor_tensor", [Bt["a"], BP], [Bt["t1"]], tmps["t1"][:], tmps["a"][:], P["ka"][:], ALU.mult)
                        self.g("tensor_tensor", [Bt["t1"], BP], [Bt["t1"]], tmps["t1"][:], tmps["t1"][:], P["omka"][:], ALU.add)
                        self.v("tensor_tensor", [Bp_k, Bt["t1"]], [BO["k"][q]], O["k"][q][:], p_k[:, :], tmps["t1"][:], ALU.mult)
                        self.v("scalar_tensor_tensor", [BO["kk"][q], Bt["a"]], [BO["nk"][q]], O["nk"][q][:],
                               O["kk"][q][:], -1.0, tmps["a"][:], ALU.mult, ALU.mult)
                        self.g("tensor_tensor", [BO["r"][q], BO["k"][q]], [Bt["rk"]], tmps["rk"][:], O["r"][q][:], O["k"][q][:], ALU.mult)
                        self.g("tensor_tensor", [Bt["rk"], BP], [Bt["rk"]], tmps["rk"][:], tmps["rk"][:], P["rk"][:], ALU.mult)
                        self.v("tensor_reduce", [Bt["rk"]], [Bobs[q]], obs[q][:, 0:8],
                               tmps["rk"][:].rearrange("p (h k) -> p h k", k=64), AX.X, ALU.add)
                        p_g, Bp_g = nxt()
                        self.mm([Bsgd, BL], [Bp_g], p_g[:, :], sgd[:, j * 128:(j + 1) * 128], lora[:, 2, :])
                        self.a([Bp_g], [BO["g"][q]], O["g"][q][:], p_g[:, :], AF.Copy)
                        for n_, sname in (("r", "s_r"), ("w", "s_w"), ("k", "s_k"), ("kk", "s_kk"), ("nk", "s_nk")):
                            dst = Sc[sname][b].rearrange("h t k -> t h k")[t0:t0 + 128]
                            self.dma([BO[n_][q]], [Bd[sname]], dst, O[n_][q][:].rearrange("p (h k) -> p h k", k=64))
                        dstv = Sc["s_v"][b * 64:(b + 1) * 64].rearrange("hv t l -> t hv l")[t0:t0 + 128]
                        self.dma([BO["v"][q]], [Bd["s_v"]], dstv, O["v"][q][:].rearrange("p (hv l) -> p hv l", l=8))
                        self.dma([BO["v"][q]], [Bd["s_vt"]], Sc["s_vt"][b, t0:t0 + 128, :], O["v"][q][:])
                        self.dma([BO["g"][q]], [Bd["s_g"]], Sc["s_g"][b, t0:t0 + 128, :], O["g"][q][:])
                        self.dma([Bobs[q]], [Bd["s_bs"]], Sc["s_bs"][b, t0:t0 + 128, :], obs[q][:])

    def phase_BC2(self):
        I, Sc, Bd = self.I, self.Sc, self.Bd
        with contextlib.ExitStack() as es:
            hTs = [self.sb(es, "hTf", [128, 8, 512], BF16) for _ in range(2)]
            BhTs = [Buf(), Buf()]
            Wf = self.sb(es, "Wf", [128, 8, 1544], BF16)
            BW = Buf()
            wv = I["w_in"].rearrange("(kc p) n -> p kc n", p=128)
            wtmp1 = self.sb(es, "wtmp", [128, 8, 256])
            wtmp = [wtmp1, wtmp1]
            Bwt1 = Buf()
            Bwt = [Bwt1, Bwt1]
            for c in range(7):
                k = c % 2
                c0 = c * 256
                w_ = min(256, 1544 - c0)
                self.dma([], [Bwt[k]], wtmp[k][:, :, 0:w_], wv[:, :, 1792 + c0:1792 + c0 + w_])
                self.v("tensor_copy", [Bwt[k]], [BW], Wf[:, :, c0:c0 + w_], wtmp[k][:, :, 0:w_])
            NR = 4
            rot = [self.ps(es, "rot", [128, 512]) for _ in range(NR)]
            Brot = [Buf() for _ in range(NR)]
            rc = [0]

            def nxt():
                i = rc[0] % NR
                rc[0] += 1
                return rot[i], Brot[i]

            qsb = [self.sb(es, "qsb", [128, 512], BF16) for _ in range(3)]
            Bq = [Buf() for _ in range(3)]
            qc_ = [0]
            vsb = [self.sb(es, "vsb", [128, 512], BF16) for _ in range(2)]
            Bv = [Buf(), Buf()]
            nfb = self.sb(es, "nfb", [8, 2])
            Bnfb = Buf()
            self.dma([], [Bnfb], nfb[:, 0:1], I["fbias"])
            self.v("tensor_scalar", [Bnfb], [Bnfb], nfb[:, 1:2], nfb[:, 0:1], -1.0, None, ALU.mult)
            ones8 = self.sb(es, "ones8", [8, 512])
            ones8b = self.sb(es, "ones8b", [8, 512], BF16)
            Bon = Buf()
            self.v("memset", [], [Bon], ones8[:], 1.0)
            self.v("tensor_copy", [Bon], [Bon], ones8b[:], ones8[:])
            fe = self.sb(es, "fe", [8, 512])
            Bfe = Buf()
            cum = [self.sb(es, "cum", [8, 512]) for _ in range(2)]
            Bcum = [Buf(), Buf()]
            r1 = self.sb(es, "r1", [8, 512])
            Br1 = Buf()
            parts = [self.sb(es, "parts", [8, 6, 512], BF16) for _ in range(2)]
            Bparts = [Buf(), Buf()]
            def loadh(nb_):
                self.dma([Bd["s_hT"]], [BhTs[nb_ % 2]], hTs[nb_ % 2][:], Sc["s_hT"][nb_ // 8, nb_ % 8])

            loadh(0)
            for b in range(NB):
                for blk in range(8):
                    t0 = blk * 512
                    nb = b * 8 + blk
                    if nb + 1 < NB * 8:
                        loadh(nb + 1)
                    hT, BhT = hTs[nb % 2], BhTs[nb % 2]
                    for which, c_base, sname in ((0, 0, "s_qa"), (1, 512, "s_ka")):
                        for hp in range(4):
                            p, Bp = nxt()
                            for kc in range(8):
                                self.mm([BW, BhT], [Bp], p[:, :], Wf[:, kc, c_base + hp * 128: c_base + (hp + 1) * 128],
                                        hT[:, kc, :], start=(kc == 0), stop=(kc == 7))
                            qi = qc_[0] % 3
                            qc_[0] += 1
                            self.a([Bp], [Bq[qi]], qsb[qi][:], p[:, :], AF.Copy, scale=(0.125 if which == 0 else 1.0))
                            for jj in range(2):
                                self.dma([Bq[qi]], [Bd[sname]], Sc[sname][b, 2 * hp + jj, 0:64, t0:t0 + 512],
                                         qsb[qi][jj * 64:(jj + 1) * 64, :])
                            yield
                    p, Bp = nxt()
                    for kc in range(8):
                        self.mm([BW, BhT], [Bp], p[0:8, :], Wf[:, kc, 1536:1544], hT[:, kc, :],
                                start=(kc == 0), stop=(kc == 7))
                    self.a([Bp, Bnfb], [Bfe], fe[:], p[0:8, :], AF.Exp, bias=nfb[:, 1:2], scale=-1.0)
                    self.a([Bfe], [Bfe], fe[:], fe[:], AF.Ln, bias=1.0)
                    ck = nb % 2
                    init = 0.0 if blk == 0 else cum[1 - ck][:, 511:512]
                    self.v("tensor_tensor_scan", [Bfe, Bon, Bcum[1 - ck]], [Bcum[ck]], cum[ck][:], ones8[:], fe[:], init,
                           ALU.mult, ALU.subtract)
                    pt, Bpt = parts[ck], Bparts[ck]
                    self.v("tensor_copy", [Bcum[ck]], [Bpt], pt[:, 0, :], cum[ck][:])
                    self.v("tensor_tensor", [Bcum[ck], Bpt], [Br1], r1[:], cum[ck][:], pt[:, 0, :], ALU.subtract)
                    self.v("tensor_copy", [Br1], [Bpt], pt[:, 1, :], r1[:])
                    self.v("tensor_tensor", [Br1, Bpt], [Br1], r1[:], r1[:], pt[:, 1, :], ALU.subtract)
                    self.v("tensor_copy", [Br1], [Bpt], pt[:, 2, :], r1[:])
                    self.v("tensor_scalar", [Bpt], [Bpt], pt[:, 3:6, :], pt[:, 0:3, :], -1.0, None, ALU.mult)
                    for i in range(3):
                        self.dma([Bpt], [Bd["s_qa"]], Sc["s_qa"][b, :, 64 + i, t0:t0 + 512], pt[:, i, :])
                        self.dma([Bon], [Bd["s_qa"]], Sc["s_qa"][b, :, 67 + i, t0:t0 + 512], ones8b[:])
                        self.dma([Bon], [Bd["s_ka"]], Sc["s_ka"][b, :, 64 + i, t0:t0 + 512], ones8b[:])
                        self.dma([Bpt], [Bd["s_ka"]], Sc["s_ka"][b, :, 67 + i, t0:t0 + 512], pt[:, 3 + i, :])
                    yield
                    for j in range(4):
                        n = blk * 4 + j
                        p, Bp = nxt()
                        for kc in range(8):
                            self.mm([BW, BhT], [Bp], p[:, :], hT[:, kc, j * 128:(j + 1) * 128],
                                    Wf[:, kc, 1024:1536], start=(kc == 0), stop=(kc == 7))
                        self.a([Bp], [Bv[n % 2]], vsb[n % 2][:], p[:, :], AF.Copy)
                        self.dma([Bv[n % 2]], [Bd["s_fv"]], Sc["s_fv"][b, n * 128:(n + 1) * 128, :], vsb[n % 2][:])
                        yield

    def phase_EH(self):
        gE = self.phase_E()
        gH = itertools.chain(self.phase_BC2(), self.phase_H())
        doneH = False
        for _ in gE:
            if not doneH:
                try:
                    next(gH)
                except StopIteration:
                    doneH = True
        if not doneH:
            for _ in gH:
                pass

    def phase_E(self):
        Sc, Bd = self.Sc, self.Bd
        with contextlib.ExitStack() as es:
            St = [self.sb(es, "St", [128, 8, 64]) for _ in range(2)]
            BS = [Buf(), Buf()]
            self.v("memset", [], [BS[0]], St[0][:], 0.0)
            self.v("memset", [], [BS[1]], St[1][:], 0.0)
            Sp = self.sb(es, "Sp", [128, 8, 64])
            BSp = Buf()
            tmp = self.sb(es, "tmp", [128, 8, 64])
            tmp2 = self.sb(es, "tmp2", [128, 8, 64])
            Btmp, Btmp2 = Buf(), Buf()
            tmp3 = [self.sb(es, "tmp3", [128, 8, 64]) for _ in range(2)]
            tmp4 = [self.sb(es, "tmp4", [128, 8, 64]) for _ in range(3)]
            Bt3 = [Buf(), Buf()]
            Bt4 = [Buf(), Buf(), Buf()]
            sa = [self.sb(es, "sa", [128, 8]) for _ in range(2)]
            Bsa = [Buf(), Buf()]
            ops = ["s_kk", "s_w", "s_nk", "s_k", "s_r"]
            OB = {n: [self.sb(es, "ob" + n, [128, CH, 64]) for _ in range(2)] for n in ops}
            BOB = {n: [Buf(multi=True), Buf(multi=True)] for n in ops}
            vB = [self.sb(es, "vB", [128, CH, 8]) for _ in range(2)]
            BvB = [Buf(), Buf()]
            yB = [self.sb(es, "yB", [128, CH, 8]) for _ in range(2)]
            ByB = [Buf(), Buf()]
            nch = S // CH

            def load(c):
                k = c % 2
                t0 = c * CH
                gr = Grp()
                for n in ops:
                    for bh in range(16):
                        b, h = bh // 8, bh % 8
                        src = Sc[n][b, h, t0:t0 + CH, :].partition_broadcast(8)
                        self.dma([Bd[n]], [BOB[n][k]], OB[n][k][bh * 8:(bh + 1) * 8, :, :], src, grp=gr)
                self.dma([Bd["s_v"]], [BvB[k]], vB[k][:], Sc["s_v"][:, t0:t0 + CH, :], grp=gr)
                self.s.join([BOB[n][k] for n in ops] + [BvB[k]])

            def bc(ap):
                return ap.unsqueeze(1).broadcast_to([128, 8, 64])

            def poolC(t):
                c, i = t // CH, t % CH
                k = c % 2
                self.g("tensor_tensor", [BS[t % 2], BOB["s_r"][k]], [Bt4[t % 3]], tmp4[t % 3][:], St[t % 2][:],
                       bc(OB["s_r"][k][:, i, :]), ALU.mult)

            def dveY(t):
                c, i = t // CH, t % CH
                k = c % 2
                self.v("tensor_reduce", [Bt4[t % 3]], [ByB[k]], yB[k][:, i, :], tmp4[t % 3][:], AX.X, ALU.add)
                if i == CH - 1:
                    self.dma([ByB[k]], [Bd["s_y"]], Sc["s_y"][:, c * CH:(c + 1) * CH, :], yB[k][:])

            load(0)
            for c in range(nch):
                k = c % 2
                for i in range(CH):
                    if i == 1 and c + 1 < nch:
                        load(c + 1)
                    t = c * CH + i
                    q = t % 2
                    So, Sn = St[(t + 1) % 2], St[t % 2]
                    BSo, BSn = BS[(t + 1) % 2], BS[t % 2]
                    for vl in range(8):
                        self.a([BOB["s_k"][k], BvB[k]], [Bt3[q]], tmp3[q][:, vl, :], OB["s_k"][k][:, i, :], AF.Copy,
                               scale=vB[k][:, i, vl:vl + 1])
                    self.g("tensor_tensor", [BSo, BOB["s_w"][k]], [BSp], Sp[:], So[:], bc(OB["s_w"][k][:, i, :]), ALU.mult)
                    if t >= 1:
                        poolC(t - 1)
                    self.v("tensor_tensor", [BSo, BOB["s_kk"][k]], [Btmp], tmp[:], So[:], bc(OB["s_kk"][k][:, i, :]), ALU.mult)
                    self.v("tensor_reduce", [Btmp], [Bsa[q]], sa[q][:], tmp[:], AX.X, ALU.add)
                    if t >= 2:
                        dveY(t - 2)
                    self.v("tensor_tensor", [Bsa[q], BOB["s_nk"][k]], [Btmp2], tmp2[:],
                           sa[q][:].unsqueeze(2).broadcast_to([128, 8, 64]), bc(OB["s_nk"][k][:, i, :]), ALU.mult)
                    self.v("tensor_tensor", [BSp, Bt3[q]], [BSp], Sp[:], Sp[:], tmp3[q][:], ALU.add)
                    self.v("tensor_tensor", [BSp, Btmp2], [BSn], Sn[:], Sp[:], tmp2[:], ALU.add)
                    yield
            poolC(S - 1)
            dveY(S - 2)
            dveY(S - 1)

    def phase_H(self):
        I, Sc, Bd = self.I, self.Sc, self.Bd
        with contextlib.ExitStack() as es:
            mstage = self.sb(es, "mstage", [128, 128])
            maskb = self.sb(es, "maskb", [128, 128], BF16)
            Bm = Buf()
            self.dma([], [Bm], mstage[:], I["maskneg"])
            self.v("tensor_copy", [Bm], [Bm], maskb[:], mstage[:])
            qa = [self.sb(es, "qa", [70, S], BF16) for _ in range(2)]
            ka = [self.sb(es, "ka", [70, S], BF16) for _ in range(2)]
            vt = [self.sb(es, "vt", [128, 32, 65], BF16) for _ in range(2)]
            Bqa, Bka, Bvt = [Buf(), Buf()], [Buf(), Buf()], [Buf(), Buf()]
            for k in range(2):
                self.v("memset", [], [Bvt[k]], vt[k][:], 1.0)
            yf = [self.sb(es, "yf", [128, 32, 64]) for _ in range(2)]
            Byf = [Buf(), Buf()]
            NS = 3
            sps = [self.ps(es, "sps", [128, 512]) for _ in range(NS)]
            Bsps = [Buf() for _ in range(NS)]
            acc = [self.ps(es, "acc", [128, 512]) for _ in range(4)]
            Bacc = [Buf() for _ in range(4)]
            NP = 3
            pts = [self.sb(es, "pts", [128, 512], BF16) for _ in range(NP)]
            Bpts = [Buf() for _ in range(NP)]
            rec = self.sb(es, "rec", [128, 8])
            Brec = Buf()
            cnt = 0

            def load(bh):
                b, h = bh // 8, bh % 8
                k = bh % 2
                gr = Grp()
                self.dma([Bd["s_qa"]], [Bqa[k]], qa[k][:], Sc["s_qa"][b, h], grp=gr)
                self.dma([Bd["s_ka"]], [Bka[k]], ka[k][:], Sc["s_ka"][b, h], grp=gr)
                src = Sc["s_fv"][b].rearrange("(n p) c -> p n c", p=128)[:, :, h * 64:(h + 1) * 64]
                self.dma([Bd["s_fv"]], [Bvt[k]], vt[k][:, :, 0:64], src, grp=gr)
                self.s.join([Bqa[k], Bka[k], Bvt[k]])

            load(0)
            for bh in range(16):
                b, h = bh // 8, bh % 8
                k = bh % 2
                if bh + 1 < 16:
                    load(bh + 1)
                for qc in range(8):
                    for kt in range(4 * qc + 4):
                        d = kt - 4 * qc
                        si = cnt % NS
                        pi = cnt % NP
                        cnt += 1
                        sp_, Bsp = sps[si], Bsps[si]
                        lhs = ka[k][:, kt * 128:(kt + 1) * 128]
                        if d < 0:
                            c0 = 0
                            self.mm([Bka[k], Bqa[k]], [Bsp], sp_[:, 0:512], lhs, qa[k][:, qc * 512:(qc + 1) * 512])
                        else:
                            c0 = d * 128
                            q0 = qc * 512 + c0
                            self.mm([Bka[k], Bqa[k]], [Bsp], sp_[:, c0:c0 + 128], lhs, qa[k][:, q0:q0 + 128],
                                    start=True, stop=False)
                            self.mm([Bm, self.Bconst], [Bsp], sp_[:, c0:c0 + 128], self.ident_b[:], maskb[:],
                                    start=False, stop=True)
                            if c0 + 128 < 512:
                                self.mm([Bka[k], Bqa[k]], [Bsp], sp_[:, c0 + 128:512], lhs,
                                        qa[k][:, q0 + 128:qc * 512 + 512])
                        self.a([Bsp], [Bpts[pi]], pts[pi][:, c0:512], sp_[:, c0:512], AF.Exp)
                        for qs in range(max(d, 0), 4):
                            self.mm([Bpts[pi], Bvt[k]], [Bacc[qs]], acc[qs][:, 0:65], pts[pi][:, qs * 128:(qs + 1) * 128],
                                    vt[k][:, kt, :], start=(kt == 0), stop=(kt == 4 * qc + qs))
                        yield
                    for qs in range(4):
                        n = 4 * qc + qs
                        self.v("reciprocal", [Bacc[qs]], [Brec], rec[:, qs:qs + 1], acc[qs][:, 64:65])
                        self.v("tensor_scalar", [Bacc[qs], Brec], [Byf[k]], yf[k][:, n, :], acc[qs][:, 0:64],
                               rec[:, qs:qs + 1], None, ALU.mult)
                    yield
                dst = Sc["s_yf"][b].rearrange("(n p) c -> p n c", p=128)[:, :, h * 64:(h + 1) * 64]
                self.dma([Byf[k]], [Bd["s_yf"]], dst, yf[k][:])

    def phase_I(self):
        I, Sc, Bd = self.I, self.Sc, self.Bd
        es = contextlib.ExitStack()
        with es:
            pes = self.es
            self.Wts = self.sb(pes, "Wts", [128, NT, 2])
            self.OH1 = self.sb(pes, "OH1", [128, NT, 32])
            self.OH2 = self.sb(pes, "OH2", [128, NT, 32])
            self.OHb = self.sb(pes, "OHb", [128, NT, 32], BF16)
            self.BWts, self.BOH = Buf(), Buf()
            wout = self.sb(es, "wout", [128, 8, D], BF16)
            BWo = Buf()
            wv = I["w_out"].rearrange("(kc p) n -> p kc n", p=128)
            wtmp = [self.sb(es, "wtmp", [128, 8, 256]) for _ in range(2)]
            Bwt = [Buf(), Buf()]
            for c in range(4):
                k = c % 2
                self.dma([], [Bwt[k]], wtmp[k][:], wv[:, :, c * 256:(c + 1) * 256])
                self.v("tensor_copy", [Bwt[k]], [BWo], wout[:, :, c * 256:(c + 1) * 256], wtmp[k][:])
            wr = self.sb(es, "wr", [128, 8, 36])
            brb = self.sb(es, "brb", [128, 36])
            BWr = Buf()
            self.dma([], [BWr], wr[:], I["w_r"].rearrange("(kc p) n -> p kc n", p=128))
            self.dma([], [BWr], brb[:], I["b_r"][0].partition_broadcast(128))
            Pl = self.sb(es, "Pl", [128, 2, 512])
            BPl = Buf()
            self.dma([], [BPl], Pl[:, 0, :], I["rwv"][5].partition_broadcast(128))
            self.dma([], [BPl], Pl[:, 1, :], I["rwv"][6].partition_broadcast(128))
            g2b = self.sb(es, "g2b", [128, D])
            Bg2 = Buf()
            self.dma([], [Bg2], g2b[:], I["g2"][0].partition_broadcast(128))
            G1 = self.sb(es, "G1", [128, D])
            A2 = self.sb(es, "A2", [128, D])
            B2 = self.sb(es, "B2", [128, D])
            BG1, BA2, BB2 = Buf(), Buf(), Buf()
            pso = [self.ps(es, "pso", [128, 512]) for _ in range(2)]
            Bpso = [Buf(), Buf()]
            pmT = self.ps(es, "pmT", [128, 8, 128], BF16)
            BpmT = Buf()
            phT = [self.ps(es, "phT", [128, 4, 128]) for _ in range(2)]
            BphT = [Buf(), Buf()]
            pl = self.ps(es, "pl", [128, 512])
            Bpl = Buf()
            yt = [self.sb(es, "yt", [128, 512]) for _ in range(2)]
            gt = [self.sb(es, "gt", [128, 512]) for _ in range(2)]
            vtk = [self.sb(es, "vtk", [128, 512]) for _ in range(2)]
            bst = [self.sb(es, "bst", [128, 8]) for _ in range(2)]
            yft = [self.sb(es, "yft", [128, 512]) for _ in range(2)]
            xt = [self.sb(es, "xt", [128, D]) for _ in range(2)]
            Bin = [Buf(multi=True), Buf(multi=True)]
            ysq = self.sb(es, "ysq", [128, 512])
            yn = self.sb(es, "yn", [128, 512])
            bon = self.sb(es, "bon", [128, 512])
            Bw_ = Buf()
            st = self.sb(es, "st", [128, 64])
            Bst = Buf()
            mix = self.sb(es, "mix", [128, D], BF16)
            Bmix = Buf()
            mixT = self.sb(es, "mixT", [128, 8, 128], BF16)
            BmixT = Buf()
            x1 = [self.sb(es, "x1", [128, D]) for _ in range(2)]
            Bx1 = [Buf(), Buf()]
            junk = self.sb(es, "junk", [128, D])
            Bjunk = Buf()
            ss = self.sb(es, "ss", [128, 4])
            Bss = Buf()
            h2 = [self.sb(es, "h2", [128, D]) for _ in range(2)]
            Bh2 = [Buf(), Buf()]
            h2b = [self.sb(es, "h2b", [128, D], BF16) for _ in range(2)]
            Bh2b = [Buf(), Buf()]
            h2T = self.sb(es, "h2T", [128, 8, 128])
            Bh2T = Buf()
            lg = self.sb(es, "lg", [128, 36])
            rs = self.sb(es, "rs", [128, 96])
            Brs = Buf()

            def v3(ap):
                return ap.rearrange("p (h k) -> p h k", k=64)

            def b8(ap):
                return ap.unsqueeze(2).broadcast_to([128, 8, 64])

            def load(n):
                b, j = n // 32, n % 32
                t0 = j * 128
                k = n % 2
                src = Sc["s_y"][b * 64:(b + 1) * 64].rearrange("hv t l -> t hv l")[t0:t0 + 128]
                gr = Grp()
                self.dma([Bd["s_y"]], [Bin[k]], yt[k][:].rearrange("p (hv l) -> p hv l", l=8), src, grp=gr)
                self.dma([Bd["s_g"]], [Bin[k]], gt[k][:], Sc["s_g"][b, t0:t0 + 128, :], grp=gr)
                self.dma([Bd["s_vt"]], [Bin[k]], vtk[k][:], Sc["s_vt"][b, t0:t0 + 128, :], grp=gr)
                self.dma([Bd["s_bs"]], [Bin[k]], bst[k][:], Sc["s_bs"][b, t0:t0 + 128, :], grp=gr)
                self.dma([Bd["s_yf"]], [Bin[k]], yft[k][:], Sc["s_yf"][b, t0:t0 + 128, :], grp=gr)
                self.dma([], [Bin[k]], xt[k][:], I["x"][b, t0:t0 + 128, :], grp=gr)
                self.s.join([Bin[k]])

            def dup(name, shape, dt=F32):
                return [self.sb(es, name + "2", shape, dt), None]

            L2 = dict(ysq=[ysq, self.sb(es, "ysq2", [128, 512])], yn=[yn, self.sb(es, "yn2", [128, 512])],
                      bon=[bon, self.sb(es, "bon2", [128, 512])], st=[st, self.sb(es, "st2", [128, 64])],
                      mix=[mix, self.sb(es, "mix2", [128, D], BF16)], mixT=[mixT, self.sb(es, "mixT2", [128, 8, 128], BF16)],
                      junk=[junk, self.sb(es, "junk2", [128, D])], ss=[ss, self.sb(es, "ss2", [128, 4])],
                      h2T=[h2T, self.sb(es, "h2T2", [128, 8, 128])], lg=[lg, self.sb(es, "lg2", [128, 36])],
                      rs=[rs, self.sb(es, "rs2", [128, 96])], pmT=[pmT, self.ps(es, "pmT2", [128, 8, 128], BF16)],
                      pl=[pl, self.ps(es, "pl2", [128, 512])])
            B2_ = {nm: [Buf(), Buf()] for nm in ("Bw_", "Bst", "Bmix", "BmixT", "Bjunk", "Bss", "Bh2T", "Brs", "BpmT", "Bpl")}
            load(0)
            for n in range(NT):
                b, j = n // 32, n % 32
                t0 = j * 128
                k = n % 2
                ysq, yn, bon, st, mix, mixT, junk, ss, h2T, lg, rs, pmT, pl = [L2[nm][k] for nm in (
                    "ysq", "yn", "bon", "st", "mix", "mixT", "junk", "ss", "h2T", "lg", "rs", "pmT", "pl")]
                Bw_, Bst, Bmix, BmixT, Bjunk, Bss, Bh2T, Brs, BpmT, Bpl = [B2_[nm][k] for nm in (
                    "Bw_", "Bst", "Bmix", "BmixT", "Bjunk", "Bss", "Bh2T", "Brs", "BpmT", "Bpl")]
                if j == 0:
                    self.bcast_row(G1, b, 2, BG1, pso, Bpso, "copy")
                    self.bcast_row(B2, b, 3, BB2, pso, Bpso, "copy")
                    self.bcast_row(A2, b, 4, BA2, pso, Bpso, "scale", g2b, Bg2)
                if n + 1 < NT:
                    load(n + 1)
                y = yt[k]
                self.v("tensor_reduce", [Bin[k]], [Bst], st[:, 0:8], v3(y[:]), AX.X, ALU.add)
                self.g("tensor_tensor", [Bin[k]], [Bw_], ysq[:], y[:], y[:], ALU.mult)
                self.v("tensor_reduce", [Bw_], [Bst], st[:, 8:16], v3(ysq[:]), AX.X, ALU.add)
                self.v("tensor_scalar", [Bst], [Bst], st[:, 16:24], st[:, 0:8], 1.0 / 64, None, ALU.mult)
                self.v("tensor_tensor", [Bst], [Bst], st[:, 24:32], st[:, 16:24], st[:, 16:24], ALU.mult)
                self.v("scalar_tensor_tensor", [Bst], [Bst], st[:, 32:40], st[:, 8:16], 1.0 / 64, st[:, 24:32],
                       ALU.mult, ALU.subtract)
                self.a([Bst, self.Bconst], [Bst], st[:, 40:48], st[:, 32:40], AF.Sqrt, bias=self.eps6[:, 1:2], scale=1.0)
                self.v("reciprocal", [Bst], [Bst], st[:, 48:56], st[:, 40:48])
                self.v("tensor_tensor", [Bin[k], Bst], [Bw_], v3(yn[:]), v3(y[:]), b8(st[:, 16:24]), ALU.subtract)
                self.v("tensor_tensor", [Bw_, Bst], [Bw_], v3(yn[:]), v3(yn[:]), b8(st[:, 48:56]), ALU.mult)
                self.v("tensor_tensor", [Bw_, BPl], [Bw_], yn[:], yn[:], Pl[:, 0, :], ALU.mult)
                self.g("tensor_tensor", [Bw_, BPl], [Bw_], yn[:], yn[:], Pl[:, 1, :], ALU.add)
                self.g("tensor_tensor", [Bin[k]], [Bw_], v3(bon[:]), v3(vtk[k][:]), b8(bst[k][:, 0:8]), ALU.mult)
                self.v("tensor_tensor", [Bw_], [Bw_], yn[:], yn[:], bon[:], ALU.add)
                self.v("tensor_tensor", [Bw_, Bin[k]], [Bmix], mix[:, 0:512], yn[:], gt[k][:], ALU.mult)
                self.a([Bin[k]], [Bmix], mix[:, 512:1024], yft[k][:], AF.Copy)
                for kc in range(8):
                    self.tr([Bmix, self.Bconst], [BpmT], pmT[:, kc, :], mix[:, kc * 128:(kc + 1) * 128], self.ident_b[:])
                self.a([BpmT], [BmixT], mixT[:], pmT[:], AF.Copy)
                for half in range(2):
                    for kc in range(8):
                        self.mm([BmixT, BWo], [Bpso[half]], pso[half][:, :], mixT[:, kc, :],
                                wout[:, kc, half * 512:(half + 1) * 512], start=(kc == 0), stop=(kc == 7))
                    hs = slice(half * 512, (half + 1) * 512)
                    self.v("tensor_tensor", [Bpso[half], BG1], [Bjunk], junk[:, hs], pso[half][:, :], G1[:, hs], ALU.mult)
                    self.v("tensor_tensor", [Bjunk, Bin[k]], [Bx1[k]], x1[k][:, hs], junk[:, hs], xt[k][:, hs], ALU.add)
                self.dma([Bx1[k]], [Bd["s_x1"]], Sc["s_x1"][b, t0:t0 + 128, :], x1[k][:])
                rstd = self.rms_rstd(x1[k], Bx1[k], junk, Bjunk, ss, Bss)
                self.v("scalar_tensor_tensor", [Bx1[k], Bss, BA2], [Bjunk], junk[:], x1[k][:], rstd, A2[:], ALU.mult, ALU.mult)
                self.v("tensor_tensor", [Bjunk, BB2], [Bh2[k]], h2[k][:], junk[:], B2[:], ALU.add)
                self.a([Bh2[k]], [Bh2b[k]], h2b[k][:], h2[k][:], AF.Copy)
                self.dma([Bh2b[k]], [Bd["s_h2"]], Sc["s_h2"][n * 128:(n + 1) * 128, :], h2b[k][:])
                for hh in range(2):
                    for kc4 in range(4):
                        kc = hh * 4 + kc4
                        self.tr([Bh2[k], self.Bconst], [BphT[hh]], phT[hh][:, kc4, :], h2[k][:, kc * 128:(kc + 1) * 128],
                                self.ident_f[:])
                    self.a([BphT[hh]], [Bh2T], h2T[:, hh * 4:(hh + 1) * 4, :], phT[hh][:], AF.Copy)
                for kc in range(8):
                    self.mm([Bh2T, BWr], [Bpl], pl[:, 0:36], h2T[:, kc, :], wr[:, kc, :], start=(kc == 0), stop=(kc == 7))
                self.v("tensor_tensor", [Bpl, BWr], [Brs], lg[:], pl[:, 0:36], brb[:], ALU.add)
                R = [Brs]
                self.v("tensor_reduce", R, R, rs[:, 0:1], lg[:, 0:4], AX.X, ALU.max)
                self.v("tensor_scalar", R, R, rs[:, 1:2], rs[:, 0:1], -1.0, None, ALU.mult)
                self.a(R, R, rs[:, 4:8], lg[:, 0:4], AF.Exp, bias=rs[:, 1:2], scale=1.0)
                self.v("tensor_reduce", R, R, rs[:, 2:3], rs[:, 4:8], AX.X, ALU.add)
                self.v("reciprocal", R, R, rs[:, 3:4], rs[:, 2:3])
                self.v("tensor_scalar", R, R, rs[:, 8:12], lg[:, 0:4], rs[:, 0:1], None, ALU.is_equal)
                self.v("tensor_tensor", R, R, rs[:, 16:48].rearrange("p (g e) -> p g e", e=8),
                       lg[:, 4:36].rearrange("p (g e) -> p g e", e=8),
                       rs[:, 8:12].unsqueeze(2).broadcast_to([128, 4, 8]), ALU.mult)
                self.v("tensor_reduce", R, R, rs[:, 48:56], rs[:, 16:48].rearrange("p (g e) -> p e g", e=8), AX.X, ALU.add)
                self.v("max", R, R, rs[:, 56:64], rs[:, 48:56])
                self.v("tensor_scalar", R, R, rs[:, 64:72], rs[:, 48:56], rs[:, 56:57], None, ALU.is_equal)
                self.v("tensor_scalar", R, R, rs[:, 72:80], rs[:, 48:56], rs[:, 57:58], None, ALU.is_equal)
                self.v("tensor_scalar", R, R, rs[:, 80:81], rs[:, 56:57], -1.0, None, ALU.mult)
                self.a(R, R, rs[:, 81:82], rs[:, 57:58], AF.Exp, bias=rs[:, 80:81], scale=1.0)
                self.v("tensor_scalar", R, R, rs[:, 82:83], rs[:, 81:82], 1.0, None, ALU.add)
                self.v("reciprocal", R, R, rs[:, 83:84], rs[:, 82:83])
                self.v("tensor_tensor", R, [self.BWts], self.Wts[:, n, 0:1], rs[:, 3:4], rs[:, 83:84], ALU.mult)
                self.v("tensor_tensor", R + [self.BWts], [self.BWts], self.Wts[:, n, 1:2], self.Wts[:, n, 0:1], rs[:, 81:82], ALU.mult)
                gohb = rs[:, 8:12].unsqueeze(2).broadcast_to([128, 4, 8])
                self.v("tensor_tensor", R, [self.BOH], self.OH1[:, n, :].rearrange("p (g e) -> p g e", e=8), gohb,
                       rs[:, 64:72].unsqueeze(1).broadcast_to([128, 4, 8]), ALU.mult)
                self.v("tensor_tensor", R, [self.BOH], self.OH2[:, n, :].rearrange("p (g e) -> p g e", e=8), gohb,
                       rs[:, 72:80].unsqueeze(1).broadcast_to([128, 4, 8]), ALU.mult)
                self.v("tensor_tensor", [self.BOH], [self.BOH], self.OHb[:, n, :], self.OH1[:, n, :], self.OH2[:, n, :], ALU.add)

    def phase_K(self):
        I, Sc, Bd = self.I, self.Sc, self.Bd
        pes = self.es
        self.slotI = [self.sb(pes, "slotI", [128, NT], I32) for _ in range(2)]
        self.Bslot = Buf()
        with contextlib.ExitStack() as es0:
            idxG = self.sb(es0, "idxG", [128, NBLK, 8], I32)
            idxD = self.sb(es0, "idxD", [128, NBLK, 4], I32)
            Bidx = Buf()
            with contextlib.ExitStack() as es:
                lst = self.sb(es, "lst", [128, 128])
                lsb = self.sb(es, "lsb", [128, 128], BF16)
                onb = self.sb(es, "onb", [128, 128], BF16)
                Bc = Buf()
                self.dma([], [Bc], lst[:], I["lstrict"])
                self.v("tensor_copy", [Bc], [Bc], lsb[:], lst[:])
                self.v("memset", [], [Bc], onb[:], 1.0)
                thr64 = self.sb(es, "thr64", [128, NTHR])
                thr160 = self.sb(es, "thr160", [128, NBLK])
                ipk = self.sb(es, "ipk", [128, 8])
                ipf = self.sb(es, "ipf", [128, 4])
                self.dma([], [Bc], thr64[:], I["thr64"][0].partition_broadcast(128))
                self.dma([], [Bc], thr160[:], I["thr160"][0].partition_broadcast(128))
                self.dma([], [Bc], ipk[:], I["iota_pk"])
                self.dma([], [Bc], ipf[:], I["iota_pf"])
                ones64 = self.sb(es, "ones64", [128, 64])
                self.v("memset", [], [Bc], ones64[:], 1.0)
                base = self.sb(es, "base", [128, NT, 32])
                tot = self.sb(es, "tot", [128, NT, 32])
                incl = self.sb(es, "incl", [128, NT, 32])
                Bb = Buf()
                pp = [self.ps(es, "pk", [128, 512]) for _ in range(2)]
                Bpp = [Buf(), Buf()]
                OHf = self.OHb[:].rearrange("p n e -> p (n e)")
                for c in range(4):
                    self.mm([self.BOH, Bc], [Bpp[0]], pp[0][:, :], lsb[:], OHf[:, c * 512:(c + 1) * 512])
                    self.a([Bpp[0]], [Bb], base[:].rearrange("p n e -> p (n e)")[:, c * 512:(c + 1) * 512], pp[0][:, :], AF.Copy)
                    self.mm([self.BOH, Bc], [Bpp[1]], pp[1][:, :], onb[:], OHf[:, c * 512:(c + 1) * 512])
                    self.a([Bpp[1]], [Bb], tot[:].rearrange("p n e -> p (n e)")[:, c * 512:(c + 1) * 512], pp[1][:, :], AF.Copy)
                for e in range(32):
                    self.v("tensor_tensor_scan", [Bb, Bc], [Bb], incl[:, :, e], ones64[:, 0:NT], tot[:, :, e], 0.0,
                           ALU.mult, ALU.add)
                sm = self.sb(es, "smk", [128, 8, 32])
                Bsm = Buf()
                cmp = self.sb(es, "cmp", [128, 32, NTHR])
                self.v("tensor_copy", [Bb], [Bsm], sm[:, 0, :], incl[:, NT - 1, :])
                self.v("tensor_tensor", [Bsm, Bc], [Bsm], cmp[:], sm[:, 0, :].unsqueeze(2).broadcast_to([128, 32, NTHR]),
                       thr64[:].unsqueeze(1).broadcast_to([128, 32, NTHR]), ALU.is_gt)
                self.v("tensor_reduce", [Bsm], [Bsm], sm[:, 1, :], cmp[:], AX.X, ALU.add)
                self.v("tensor_scalar", [Bsm], [Bsm], sm[:, 2, :], sm[:, 1, :], float(BSZ), None, ALU.mult)
                self.v("tensor_tensor_scan", [Bsm, Bc], [Bsm], sm[:, 3, :], ones64[:, 0:32], sm[:, 2, :], 0.0,
                       ALU.mult, ALU.add)
                self.v("tensor_tensor", [Bsm], [Bsm], sm[:, 4, :], sm[:, 3, :], sm[:, 2, :], ALU.subtract)
                self.v("tensor_tensor", [Bb], [Bb], incl[:], incl[:], tot[:], ALU.subtract)
                self.v("tensor_tensor", [Bb], [Bb], base[:], base[:], incl[:], ALU.add)
                self.v("tensor_tensor", [Bb, Bsm], [Bb], base[:], base[:],
                       sm[:, 4, :].unsqueeze(1).broadcast_to([128, NT, 32]), ALU.add)
                slf = self.sb(es, "slf", [128, 2, NT])
                for kx, OHk in enumerate((self.OH1, self.OH2)):
                    self.v("tensor_tensor", [Bb, self.BOH], [Bb], tot[:], base[:], OHk[:], ALU.mult)
                    self.v("tensor_reduce", [Bb], [Bsm], slf[:, kx, :], tot[:], AX.X, ALU.add)
                    self.v("tensor_copy", [Bsm], [self.Bslot], self.slotI[kx][:], slf[:, kx, :])
                cmp2 = self.sb(es, "cmp2", [128, NBLK, 32])
                be = self.sb(es, "be", [128, NBLK])
                self.v("tensor_tensor", [Bsm, Bc], [Bsm], cmp2[:], sm[:, 3, :].unsqueeze(1).broadcast_to([128, NBLK, 32]),
                       thr160[:].unsqueeze(2).broadcast_to([128, NBLK, 32]), ALU.is_le)
                self.v("tensor_reduce", [Bsm], [Bsm], be[:], cmp2[:], AX.X, ALU.add)
                self.v("tensor_scalar", [Bsm], [Bsm], be[:], be[:], 31.0, None, ALU.min)
                fi = self.sb(es, "fi", [128, NBLK])
                self.v("tensor_scalar", [Bsm, Bc], [Bsm], fi[:], be[:], 128.0, ipk[:, 0:1], ALU.mult, ALU.add)
                self.v("tensor_copy", [Bsm], [Bidx], idxG[:, :, 0], fi[:])
                ht = [self.sb(es, "ht", [128, D], BF16) for _ in range(2)]
                Bht = [Buf(), Buf()]
                for n in range(NT):
                    k = n % 2
                    self.dma([Bd["s_h2"]], [Bht[k]], ht[k][:], Sc["s_h2"][n * 128:(n + 1) * 128, :])
                    for kx in range(2):
                        self.idma([Bht[k], self.Bslot], [Bd["s_xs"]], Sc["s_xs"],
                                  bass.IndirectOffsetOnAxis(ap=self.slotI[kx][:, n:n + 1], axis=0), ht[k][:], None)
            self.s.barrier()
            with contextlib.ExitStack() as es:
                wgS = self.sb(es, "wgS", [128, 8, 512])
                wuS = self.sb(es, "wuS", [128, 8, 512])
                wdS = self.sb(es, "wdS", [128, 4, D])
                BwgS, BwuS, BwdS = Buf(multi=True), Buf(multi=True), Buf(multi=True)
                wgB = self.sb(es, "wgB", [128, 8, 512], BF16)
                wuB = self.sb(es, "wuB", [128, 8, 512], BF16)
                wdB = self.sb(es, "wdB", [128, 4, D], BF16)
                BwgB, BwuB, BwdB = Buf(), Buf(), Buf()
                xb = [self.sb(es, "xb", [128, 4, D], BF16) for _ in range(2)]
                Bxb = [Buf(), Buf()]
                XT = self.sb(es, "XT", [128, 8, BSZ], BF16)
                BXT = Buf()
                sg = [self.sb(es, "sg", [128, 512]) for _ in range(2)]
                Bsg = [Buf(), Buf()]
                hidT = self.sb(es, "hidT", [128, 4, BSZ], BF16)
                BhidT = Buf()
                yb = [self.sb(es, "yb", [128, D]) for _ in range(2)]
                Byb = [Buf(), Buf()]
                pX = [self.ps(es, "pX", [128, 8, 128], BF16) for _ in range(2)]
                BpX = [Buf(), Buf()]
                pG = [self.ps(es, "pG", [128, 512]) for _ in range(2)]
                pU = [self.ps(es, "pU", [128, 512]) for _ in range(2)]
                pY = [self.ps(es, "pY", [128, 512]) for _ in range(2)]
                BpG, BpU, BpY = [Buf(), Buf()], [Buf(), Buf()], [Buf(), Buf()]

                def load(i):
                    g1_, g2_, g3_ = Grp(), Grp(), Grp()
                    off = bass.IndirectOffsetOnAxis(ap=idxG[:, i, 0:1], axis=0)
                    self.idma([Bidx], [BwgS], wgS[:].rearrange("p a f -> p (a f)"), None, I["wg"], off)
                    self.idma([Bidx], [BwuS], wuS[:].rearrange("p a f -> p (a f)"), None, I["wu"], off)
                    self.idma([Bidx], [BwdS], wdS[:].rearrange("p a f -> p (a f)"), None, I["wd"], off)
                    self.s.join([BwgS])
                    self.s.join([BwuS])
                    self.s.join([BwdS])
                    src = Sc["s_xs"][i * BSZ:(i + 1) * BSZ, :].rearrange("(s p) d -> p s d", p=128)
                    self.dma([Bd["s_xs"]], [Bxb[i % 2]], xb[i % 2][:], src)

                def cast(i):
                    self.v("tensor_copy", [BwgS], [BwgB], wgB[:], wgS[:])
                    self.a([BwuS], [BwuB], wuB[:], wuS[:], AF.Copy)
                    self.g("tensor_copy", [BwdS], [BwdB], wdB[:], wdS[:])

                load(0)
                cnt = 0
                for i in range(NBLK):
                    k = i % 2
                    cast(i)
                    if i + 1 < NBLK:
                        load(i + 1)
                    for sub in range(4):
                        px, Bpx = pX[sub % 2], BpX[sub % 2]
                        for kc in range(8):
                            self.tr([Bxb[k], self.Bconst], [Bpx], px[:, kc, :], xb[k][:, sub, kc * 128:(kc + 1) * 128],
                                    self.ident_b[:])
                        if sub % 2 == 0:
                            self.a([Bpx], [BXT], XT[:, :, sub * 128:(sub + 1) * 128], px[:], AF.Copy)
                        else:
                            self.v("tensor_copy", [Bpx], [BXT], XT[:, :, sub * 128:(sub + 1) * 128], px[:])
                    for fc in range(4):
                        j = fc % 2
                        for kc in range(8):
                            self.mm([BXT, BwgB], [BpG[j]], pG[j][:, :], wgB[:, kc, fc * 128:(fc + 1) * 128], XT[:, kc, :],
                                    start=(kc == 0), stop=(kc == 7))
                        for kc in range(8):
                            self.mm([BXT, BwuB], [BpU[j]], pU[j][:, :], wuB[:, kc, fc * 128:(fc + 1) * 128], XT[:, kc, :],
                                    start=(kc == 0), stop=(kc == 7))
                        self.a([BpG[j]], [Bsg[j]], sg[j][:], pG[j][:, :], AF.Silu)
                        self.v("tensor_tensor", [Bsg[j], BpU[j]], [BhidT], hidT[:, fc, :], sg[j][:], pU[j][:, :], ALU.mult)
                    for sub in range(4):
                        y_, By_ = yb[sub % 2], Byb[sub % 2]
                        for half in range(2):
                            for fc in range(4):
                                self.mm([BhidT, BwdB], [BpY[half]], pY[half][:, :], hidT[:, fc, sub * 128:(sub + 1) * 128],
                                        wdB[:, fc, half * 512:(half + 1) * 512], start=(fc == 0), stop=(fc == 3))
                            if half == 0:
                                self.a([BpY[half]], [By_], y_[:, 0:512], pY[half][:, :], AF.Copy)
                            else:
                                self.v("tensor_copy", [BpY[half]], [By_], y_[:, 512:1024], pY[half][:, :])
                        r0 = i * BSZ + sub * 128
                        self.dma([By_], [Bd["s_ys"]], Sc["s_ys"][r0:r0 + 128, :], y_[:])

    def phase_L(self):
        I, Sc, Bd = self.I, self.Sc, self.Bd
        with contextlib.ExitStack() as es:
            G2 = self.sb(es, "G2", [128, D])
            BG2 = Buf()
            gfb = self.sb(es, "gfb", [128, D])
            Bgf = Buf()
            self.dma([], [Bgf], gfb[:], I["gf"][0].partition_broadcast(128))
            pp = [self.ps(es, "pL", [128, 512]) for _ in range(2)]
            Bpp = [Buf(), Buf()]
            Y1 = [self.sb(es, "Y1", [128, D]) for _ in range(2)]
            Y2 = [self.sb(es, "Y2", [128, D]) for _ in range(2)]
            x1 = [self.sb(es, "x1", [128, D]) for _ in range(2)]
            Bin = [Buf(multi=True), Buf(multi=True)]
            ff = self.sb(es, "ff", [128, D])
            Bff = Buf()
            junk = self.sb(es, "junk", [128, D])
            Bjunk = Buf()
            ss = self.sb(es, "ss", [128, 4])
            Bss = Buf()
            ot = [self.sb(es, "ot", [128, D]) for _ in range(2)]
            Bot = [Buf(), Buf()]

            def load(n):
                b, j = n // 32, n % 32
                k = n % 2
                gr = Grp()
                self.idma([self.Bslot, Bd["s_ys"]], [Bin[k]], Y1[k][:], None, Sc["s_ys"],
                          bass.IndirectOffsetOnAxis(ap=self.slotI[0][:, n:n + 1], axis=0), grp=gr)
                self.idma([self.Bslot, Bd["s_ys"]], [Bin[k]], Y2[k][:], None, Sc["s_ys"],
                          bass.IndirectOffsetOnAxis(ap=self.slotI[1][:, n:n + 1], axis=0), grp=gr)
                self.dma([Bd["s_x1"]], [Bin[k]], x1[k][:], Sc["s_x1"][b, j * 128:(j + 1) * 128, :])
                self.s.join([Bin[k]])

            ffL = [ff, self.sb(es, "ff2", [128, D])]
            junkL = [junk, self.sb(es, "junkL2", [128, D])]
            ssL = [ss, self.sb(es, "ssL2", [128, 4])]
            BL_ = {nm: [Buf(), Buf()] for nm in ("Bff", "Bjunk", "Bss")}
            load(0)
            for n in range(NT):
                b, j = n // 32, n % 32
                k = n % 2
                ff, junk, ss = ffL[k], junkL[k], ssL[k]
                Bff, Bjunk, Bss = BL_["Bff"][k], BL_["Bjunk"][k], BL_["Bss"][k]
                if j == 0:
                    self.bcast_row(G2, b, 5, BG2, pp, Bpp, "copy")
                if n + 1 < NT:
                    load(n + 1)
                self.v("tensor_scalar", [Bin[k], self.BWts], [Bff], ff[:], Y1[k][:], self.Wts[:, n, 0:1], None, ALU.mult)
                self.v("scalar_tensor_tensor", [Bin[k], self.BWts, Bff], [Bff], ff[:], Y2[k][:], self.Wts[:, n, 1:2], ff[:],
                       ALU.mult, ALU.add)
                self.v("tensor_tensor", [Bff, BG2], [Bff], ff[:], ff[:], G2[:], ALU.mult)
                self.v("tensor_tensor", [Bff, Bin[k]], [Bff], ff[:], ff[:], x1[k][:], ALU.add)
                rstd = self.rms_rstd(ff, Bff, junk, Bjunk, ss, Bss)
                self.v("scalar_tensor_tensor", [Bff, Bss, Bgf], [Bot[k]], ot[k][:], ff[:], rstd, gfb[:], ALU.mult, ALU.mult)
                self.dma([Bot[k]], [self.Bout], self.out[b, j * 128:(j + 1) * 128, :], ot[k][:])


def _host_inputs(inp):
    f = np.float32
    c = np.ascontiguousarray
    p = np.arange(128)
    ident = np.eye(128, dtype=f)
    maskneg = np.where(p[:, None] > p[None, :], f(-30000.0), f(0.0)).astype(f)
    lstrict = (p[:, None] < p[None, :]).astype(f)
    sel2 = np.zeros((2, 256), f)
    sel2[0, 0:128] = 1.0
    sel2[1, 128:256] = 1.0
    thr64 = (float(BSZ) * np.arange(NTHR, dtype=f))[None, :]
    thr160 = (float(BSZ) * np.arange(NBLK, dtype=f))[None, :]
    iota_pk = (p[:, None] + 128 * np.arange(8)[None, :]).astype(f)
    iota_pf = (p[:, None] + 128 * np.arange(4)[None, :]).astype(f)
    rwv = np.stack([inp["rwkv_w0"][0], inp["rwkv_a0"][0], inp["rwkv_k_k"][0], inp["rwkv_k_a"][0],
                    inp["rwkv_r_k"][0].reshape(512), inp["rwkv_lnx_g"][0], inp["rwkv_lnx_b"][0]]).astype(f)
    shared = {
        "w_ada": c(inp["w_ada"][0]), "b_ada": c(inp["b_ada"]), "g1": c(inp["norm1_g"]), "g2": c(inp["norm2_g"]),
        "gf": c(inp["norm_f_g"][None, :]), "w_in": c(inp["w_in"][0]), "mu": c(inp["rwkv_mu"]), "rwv": c(rwv),
        "w_up": c(inp["rwkv_w_up"][0]), "a_up": c(inp["rwkv_a_up"][0]), "g_up": c(inp["rwkv_g_up"][0]),
        "fbias": c(inp["fox_f_bias"][0][:, None]), "w_out": c(inp["w_out"][0]),
        "w_r": c(np.concatenate([inp["moe_w_grp"][0], inp["moe_w_rt"][0]], axis=1)),
        "b_r": c(np.concatenate([inp["moe_b_grp"][0], inp["moe_b_rt"][0]])[None, :]),
        "wg": c(inp["moe_w_gate"][0].reshape(32, 8, 128, 512).transpose(0, 2, 1, 3).reshape(32 * 128, 8 * 512)),
        "wu": c(inp["moe_w_up"][0].reshape(32, 8, 128, 512).transpose(0, 2, 1, 3).reshape(32 * 128, 8 * 512)),
        "wd": c(inp["moe_w_down"][0].reshape(32, 4, 128, 1024).transpose(0, 2, 1, 3).reshape(32 * 128, 4 * 1024)),
        "ident": ident, "maskneg": maskneg, "lstrict": lstrict, "sel2": sel2, "thr64": thr64, "thr160": thr160,
        "iota_pk": iota_pk, "iota_pf": iota_pf,
    }
    maps = []
    for i in range(NCORES):
        m = dict(shared)
        m["x"] = c(inp["x"][NB * i:NB * (i + 1)])
        cc = inp["c"][NB * i:NB * (i + 1)]
        m["cT"] = c(cc.reshape(NB, 8, 128).transpose(2, 1, 0))
        maps.append(m)
    return maps


def kernel(**inputs):
    inp = {k: np.asarray(v, dtype=np.float32) for k, v in inputs.items()}
    maps = _host_inputs(inp)
    nc = K().build()
    res = run_bass_kernel_spmd(nc, maps, core_ids=list(range(NCORES)))
    return np.concatenate([np.asarray(r["out"]) for r in res.results], axis=0).astype(np.float32)
```

```python
import contextlib
import itertools
import numpy as np
import concourse.bass as bass
import concourse.mybir as mybir
from concourse.bass_utils import run_bass_kernel_spmd

F32 = mybir.dt.float32
BF16 = mybir.dt.bfloat16
I32 = mybir.dt.uint32
AF = mybir.ActivationFunctionType
ALU = mybir.AluOpType
AX = mybir.AxisListType

NCORES = 8
S = 4096
D = 1024
NB = 2
T = NB * S
NT = T // 128
NBLK = 64
BSZ = 512
NTHR = T // BSZ
CH = 32

STOP_AFTER = None
DEBUG = False


MULTI = []


class Buf:
    __slots__ = ("name", "w", "r", "const", "multi", "ws")

    def __init__(self, name="", const=False, multi=False):
        self.name = name
        self.w = None
        self.r = []
        self.const = const
        self.multi = multi
        self.ws = []
        if multi:
            MULTI.append(self)


class Op:
    __slots__ = ("eng", "fn", "deps", "needs", "ev", "dma", "grp")


class Grp:
    __slots__ = ("key", "last")

    def __init__(self):
        self.key = None
        self.last = None


JOIN = "join"


class Sched:
    COMPUTE = ("pe", "dve", "act", "pool")

    def __init__(self, nc, es):
        self.nc = nc
        self.engobj = dict(pe=nc.tensor, dve=nc.vector, act=nc.scalar, pool=nc.gpsimd, sp=nc.sync)
        self.sems = []
        self.semid = {}
        for e in self.COMPUTE + ("sp",):
            self.semid[e] = len(self.sems)
            self.sems.append(es.enter_context(nc.semaphore("s_" + e)))
        self.dsem = {}
        for q, k in (("sp", 32), ("pool", 16)):
            ids = []
            for i in range(k):
                ids.append(len(self.sems))
                self.sems.append(es.enter_context(nc.semaphore(f"d_{q}{i}")))
            self.dsem[q] = ids
        self.ops = []
        self.dcount = {"sp": 0, "pool": 0}
        self.dlast = {}
        self.last = {}

    def _mk(self, eng, fn, reads, writes, dma, grp=None, first=True):
        o = Op()
        o.eng = eng
        o.fn = fn
        o.needs = False
        o.ev = None
        o.dma = dma
        o.grp = grp
        deps = {}
        for b in reads:
            if b.multi:
                for p in b.ws:
                    deps[id(p)] = (p, True)
            elif b.w is not None:
                deps[id(b.w)] = (b.w, True)
        for b in writes:
            if not b.multi and b.w is not None and id(b.w) not in deps:
                deps[id(b.w)] = (b.w, False)
            for r in b.r:
                if id(r) not in deps:
                    deps[id(r)] = (r, False)
        out = []
        for p, raw in deps.values():
            if p is o:
                continue
            if dma is None and p.dma is None and p.eng == eng:
                if eng == "pe" or not raw:
                    continue
            out.append(p)
        if dma is not None:
            prev = self.dlast.get(dma)
            if prev is not None and first:
                out.append(prev)
            self.dlast[dma] = o
            if grp is not None:
                grp.last = o
        for p in out:
            p.needs = True
        o.deps = out
        for b in reads:
            if not b.const and not (fn is JOIN):
                b.r.append(o)
        for b in writes:
            if b.multi:
                if b.r or fn is JOIN:
                    b.ws = [o]
                else:
                    b.ws.append(o)
            b.w = o
            b.r = []
        self.ops.append(o)
        if dma is None:
            self.last[eng] = o
        return o

    def op(self, eng, fn, reads=(), writes=()):
        return self._mk(eng, fn, reads, writes, None)

    def dma(self, fn, reads=(), writes=(), q="sp", grp=None):
        if grp is not None and grp.key is not None:
            assert grp.key[0] == q
            return self._mk(q, fn, reads, writes, grp.key, grp, first=False)
        k = self.dcount[q]
        self.dcount[q] = k + 1
        ids = self.dsem[q]
        key = (q, k % len(ids))
        if grp is not None:
            grp.key = key
        return self._mk(q, fn, reads, writes, key, grp)

    def join(self, bufs):
        return self._mk("sp", JOIN, bufs, bufs, None)

    def barrier(self):
        for b in MULTI:
            b.ws = []
            b.r = []
            b.w = None
        lastops = [o for o in self.last.values()] + list(self.dlast.values())
        for e in ("pe", "dve", "act", "pool", "sp"):
            o = Op()
            o.eng = e
            o.fn = None
            o.needs = False
            o.ev = None
            o.dma = None
            o.grp = None
            o.deps = [p for p in lastops if not (p.dma is None and p.eng == e and e == "pe")]
            for p in o.deps:
                p.needs = True
            self.ops.append(o)

    def emit(self):
        cnt = {}
        for o in self.ops:
            if o.fn is None:
                continue
            if o.dma is not None:
                sid = self.dsem[o.dma[0]][o.dma[1]]
                cnt[sid] = cnt.get(sid, 0) + 16
                o.ev = (sid, cnt[sid])
            elif o.needs:
                sid = self.semid[o.eng]
                cnt[sid] = cnt.get(sid, 0) + 1
                o.ev = (sid, cnt[sid])
        waited = {e: {} for e in self.engobj}
        nwait = 0
        for o in self.ops:
            E = self.engobj[o.eng]
            w = waited[o.eng]
            need = {}
            for p in o.deps:
                sid, v = p.ev if p.grp is None else p.grp.last.ev
                if w.get(sid, 0) < v and need.get(sid, 0) < v:
                    need[sid] = v
            for sid, v in need.items():
                E.wait_ge(self.sems[sid], v)
                w[sid] = v
                nwait += 1
            if o.fn is JOIN:
                if o.needs:
                    E.sem_inc(self.sems[o.ev[0]], 1)
            elif o.fn is not None:
                inst = o.fn()
                if o.dma is not None:
                    inst.then_inc(self.sems[o.ev[0]], 16)
                elif o.needs:
                    inst.then_inc(self.sems[o.ev[0]], 1)
        return nwait


class K:
    def __init__(self):
        self.nc = bass.Bass("TRN2", target_bir_lowering=False)
        self.es = contextlib.ExitStack()
        self.s = Sched(self.nc, self.es)
        self.nbuf = 0

    def din(self, name, shape, dt=F32):
        return self.nc.dram_tensor(name, list(shape), dt, kind="ExternalInput").ap()

    def dscr(self, name, shape, dt=F32):
        kind = "ExternalOutput" if (DEBUG and name in DEBUG) else "Internal"
        return self.nc.dram_tensor(name, list(shape), dt, kind=kind).ap()

    def sb(self, es, name, shape, dt=F32):
        self.nbuf += 1
        return es.enter_context(self.nc.sbuf_tensor(f"{name}_{self.nbuf}", list(shape), dt))

    def ps(self, es, name, shape, dt=F32):
        self.nbuf += 1
        return es.enter_context(self.nc.psum_tensor(f"{name}_{self.nbuf}", list(shape), dt))

    def v(self, name, R, W, *a, **kw):
        f = getattr(self.nc.vector, name)
        return self.s.op("dve", lambda: f(*a, **kw), R, W)

    def g(self, name, R, W, *a, **kw):
        f = getattr(self.nc.gpsimd, name)
        return self.s.op("pool", lambda: f(*a, **kw), R, W)

    def a(self, R, W, out, in_, func, bias=None, scale=None):
        kw = {}
        if bias is not None:
            kw["bias"] = bias
        if scale is not None:
            kw["scale"] = scale
        f = self.nc.scalar.activation
        return self.s.op("act", lambda: f(out, in_, func, **kw), R, W)

    def mm(self, R, W, out, lhsT, rhs, start=True, stop=True):
        f = self.nc.tensor.matmul
        return self.s.op("pe", lambda: f(out, lhsT, rhs, start=start, stop=stop), R, W)

    def tr(self, R, W, out, in_, ident):
        f = self.nc.tensor.transpose
        return self.s.op("pe", lambda: f(out, in_, ident), R, W)

    def dma(self, R, W, out, in_, q="sp", grp=None, **kw):
        f = self.engobj(q).dma_start
        return self.s.dma(lambda: f(out=out, in_=in_, **kw), R, W, q=q)

    def engobj(self, q):
        return self.s.engobj[q]

    def idma(self, R, W, out, out_off, in_, in_off, grp=None):
        f = self.nc.gpsimd.indirect_dma_start
        return self.s.dma(lambda: f(out, out_off, in_, in_off), R, W, q="pool")

    def build(self):
        nc = self.nc
        es = self.es
        I = {}
        I["x"] = self.din("x", [NB, S, D])
        I["cT"] = self.din("cT", [128, 8, NB])
        I["w_ada"] = self.din("w_ada", [D, 6 * D])
        I["b_ada"] = self.din("b_ada", [1, 6 * D])
        I["g1"] = self.din("g1", [1, D])
        I["g2"] = self.din("g2", [1, D])
        I["gf"] = self.din("gf", [1, D])
        I["w_in"] = self.din("w_in", [D, 3336])
        I["mu"] = self.din("mu", [1, 1792])
        I["rwv"] = self.din("rwv", [7, 512])
        I["w_up"] = self.din("w_up", [64, 512])
        I["a_up"] = self.din("a_up", [64, 512])
        I["g_up"] = self.din("g_up", [128, 512])
        I["fbias"] = self.din("fbias", [8, 1])
        I["w_out"] = self.din("w_out", [D, D])
        I["w_r"] = self.din("w_r", [D, 36])
        I["b_r"] = self.din("b_r", [1, 36])
        I["wg"] = self.din("wg", [32 * 128, 8 * 512])
        I["wu"] = self.din("wu", [32 * 128, 8 * 512])
        I["wd"] = self.din("wd", [32 * 128, 4 * 1024])
        I["ident"] = self.din("ident", [128, 128])
        I["maskneg"] = self.din("maskneg", [128, 128])
        I["lstrict"] = self.din("lstrict", [128, 128])
        I["sel2"] = self.din("sel2", [2, 256])
        I["thr64"] = self.din("thr64", [1, NTHR])
        I["thr160"] = self.din("thr160", [1, NBLK])
        I["iota_pk"] = self.din("iota_pk", [128, 8])
        I["iota_pf"] = self.din("iota_pf", [128, 4])
        self.I = I
        self.out = nc.dram_tensor("out", [NB, S, D], F32, kind="ExternalOutput").ap()
        Sc = {}
        for n in ("s_r", "s_w", "s_k", "s_kk", "s_nk"):
            Sc[n] = self.dscr(n, [NB, 8, S, 64])
        Sc["s_v"] = self.dscr("s_v", [NB * 64, S, 8])
        Sc["s_y"] = self.dscr("s_y", [NB * 64, S, 8])
        Sc["s_g"] = self.dscr("s_g", [NB, S, 512])
        Sc["s_vt"] = self.dscr("s_vt", [NB, S, 512])
        Sc["s_bs"] = self.dscr("s_bs", [NB, S, 8])
        Sc["s_qa"] = self.dscr("s_qa", [NB, 8, 70, S], BF16)
        Sc["s_ka"] = self.dscr("s_ka", [NB, 8, 70, S], BF16)
        Sc["s_fv"] = self.dscr("s_fv", [NB, S, 512], BF16)
        Sc["s_yf"] = self.dscr("s_yf", [NB, S, 512])
        Sc["s_hT"] = self.dscr("s_hT", [NB, 8, 128, 8, 512], BF16)
        Sc["s_x1"] = self.dscr("s_x1", [NB, S, D])
        Sc["s_h2"] = self.dscr("s_h2", [T, D], BF16)
        Sc["s_xs"] = self.dscr("s_xs", [NBLK * BSZ, D], BF16)
        Sc["s_ys"] = self.dscr("s_ys", [NBLK * BSZ, D])
        self.Sc = Sc
        self.Bd = {n: Buf(n, multi=True) for n in Sc}
        self.Bout = Buf("out", multi=True)

        self.modrow = self.sb(es, "modrow", [2, 6 * D])
        self.Bmod = Buf("modrow")
        self.ident_f = self.sb(es, "ident_f", [128, 128])
        self.ident_b = self.sb(es, "ident_b", [128, 128], BF16)
        self.sel2 = self.sb(es, "sel2", [2, 256])
        self.Bconst = Buf("const")
        self.dma([], [self.Bconst], self.ident_f[:], I["ident"])
        self.dma([], [self.Bconst], self.sel2[:], I["sel2"])
        self.v("tensor_copy", [self.Bconst], [self.Bconst], self.ident_b[:], self.ident_f[:])

        phases = ["A", "BC1", "EH", "I", "K", "L"]
        for ph in phases:
            getattr(self, "phase_" + ph)()
            self.s.barrier()
            if STOP_AFTER == ph:
                break
        nw = self.s.emit()
        self.es.close()
        return nc

    def bcast_row(self, dst, b, j, Bdst, pp, Bpp, mode, gb=None, Bg=None):
        for half in range(2):
            p = pp[half]
            self.mm([self.Bmod, self.Bconst], [Bpp[half]], p[:, :], self.sel2[0:2, b * 128:(b + 1) * 128],
                    self.modrow[0:2, j * D + half * 512: j * D + half * 512 + 512])
            if mode == "copy":
                self.a([Bpp[half]], [Bdst], dst[:, half * 512:(half + 1) * 512], p[:, :], AF.Copy)
            else:
                self.v("scalar_tensor_tensor", [Bpp[half], Bg], [Bdst], dst[:, half * 512:(half + 1) * 512],
                       p[:, :], 1.0, gb[:, half * 512:(half + 1) * 512], ALU.add, ALU.mult)

    def rms_rstd(self, xt, Bx, junk, Bj, ss, Bs):
        self.g("tensor_tensor", [Bx], [Bj], junk[:], xt[:], xt[:], ALU.mult)
        self.v("tensor_reduce", [Bj], [Bs], ss[:, 0:1], junk[:], AX.X, ALU.add)
        self.a([Bs], [Bs], ss[:, 1:2], ss[:, 0:1], AF.Ln, bias=self.eps6[:, 0:1], scale=1.0 / D)
        self.a([Bs], [Bs], ss[:, 2:3], ss[:, 1:2], AF.Exp, scale=-0.5)
        return ss[:, 2:3]

    def phase_A(self):
        I = self.I
        with contextlib.ExitStack() as es:
            condT = self.sb(es, "condT", [128, 8, NB])
            Bc = Buf()
            self.dma([], [Bc], condT[:], I["cT"])
            self.a([Bc], [Bc], condT[:], condT[:], AF.Silu)
            b2 = self.sb(es, "b_ada2", [2, 6 * D])
            Bb2 = Buf()
            self.dma([], [Bb2], b2[:], I["b_ada"][0].partition_broadcast(2))
            wv = I["w_ada"].rearrange("(kc p) n -> p kc n", p=128)
            wt = [self.sb(es, "wada", [128, 8, 512]) for _ in range(2)]
            Bw = [Buf(), Buf()]
            pp = [self.ps(es, "psA", [2, 512]) for _ in range(2)]
            Bp = [Buf(), Buf()]
            for j in range(12):
                k = j % 2
                self.dma([], [Bw[k]], wt[k][:], wv[:, :, j * 512:(j + 1) * 512])
                for kc in range(8):
                    self.mm([Bc, Bw[k]], [Bp[k]], pp[k][:, :], condT[:, kc, :], wt[k][:, kc, :],
                            start=(kc == 0), stop=(kc == 7))
                self.v("tensor_tensor", [Bp[k], Bb2], [self.Bmod], self.modrow[0:2, j * 512:(j + 1) * 512],
                       pp[k][:, :], b2[:, j * 512:(j + 1) * 512], ALU.add)
        self.eps6 = self.sb(self.es, "eps6", [128, 2])
        self.v("memset", [], [self.Bconst], self.eps6[:, 0:1], 1e-6)
        self.v("memset", [], [self.Bconst], self.eps6[:, 1:2], 64e-5)

    def make_hT(self, es_bufs, b, blk):
        Bf = es_bufs
        I = self.I
        k = (b * 8 + blk) % 2
        hT, BhT = Bf["hT"][k], Bf["BhT"][k]
        if blk == 0:
            self.v("memset", [], [BhT], hT[:, :, 0:4], 0.0)
        else:
            hp, Bhp = Bf["hT"][1 - k], Bf["BhT"][1 - k]
            self.v("tensor_copy", [Bhp], [BhT], hT[:, :, 3:4], hp[:, :, 515:516])
        for j in range(4):
            n = blk * 4 + j
            t0 = n * 128
            kk = n % 2
            xt, Bx = Bf["xt"][kk], Bf["Bxt"][kk]
            self.dma([], [Bx], xt[:], I["x"][b, t0:t0 + 128, :])
            rstd = self.rms_rstd(xt, Bx, Bf["junk"], Bf["Bjunk"], Bf["ss"][kk], Bf["Bss"][kk])
            self.v("scalar_tensor_tensor", [Bx, Bf["Bss"][kk], Bf["BA1"]], [Bf["Bjunk"]], Bf["junk"][:], xt[:], rstd,
                   Bf["A1"][:], ALU.mult, ALU.mult)
            hb, Bhb = Bf["hb"][kk], Bf["Bhb"][kk]
            self.v("tensor_tensor", [Bf["Bjunk"], Bf["BB1"]], [Bhb], hb[:], Bf["junk"][:], Bf["B1"][:], ALU.add)
            pT, BpT = Bf["pT"][kk], Bf["BpT"][kk]
            for kc in range(8):
                self.tr([Bhb, self.Bconst], [BpT], pT[:, kc, :], hb[:, kc * 128:(kc + 1) * 128], self.ident_b[:])
            self.a([BpT], [BhT], hT[:, :, 4 + j * 128: 4 + (j + 1) * 128], pT[:, :, :], AF.Copy)
        return hT, BhT

    def alloc_hT_bufs(self, es):
        Bf = {}
        Bf["hT"] = [self.sb(es, "hT", [128, 8, 516], BF16) for _ in range(2)]
        Bf["BhT"] = [Buf(), Buf()]
        Bf["xt"] = [self.sb(es, "xt", [128, D]) for _ in range(2)]
        Bf["Bxt"] = [Buf(), Buf()]
        Bf["junk"] = self.sb(es, "junk", [128, D])
        Bf["Bjunk"] = Buf()
        Bf["ss"] = [self.sb(es, "ss", [128, 4]) for _ in range(2)]
        Bf["Bss"] = [Buf(), Buf()]
        Bf["hb"] = [self.sb(es, "hb", [128, D], BF16) for _ in range(2)]
        Bf["Bhb"] = [Buf(), Buf()]
        Bf["pT"] = [self.ps(es, "pT", [128, 8, 128], BF16) for _ in range(2)]
        Bf["BpT"] = [Buf(), Buf()]
        Bf["A1"] = self.sb(es, "A1", [128, D])
        Bf["B1"] = self.sb(es, "B1", [128, D])
        Bf["BA1"] = Buf()
        Bf["BB1"] = Buf()
        Bf["g1b"] = self.sb(es, "g1b", [128, D])
        Bf["Bg1b"] = Buf()
        self.dma([], [Bf["Bg1b"]], Bf["g1b"][:], self.I["g1"][0].partition_broadcast(128))
        return Bf

    def set_AB1(self, Bf, b, pp, Bpp):
        self.bcast_row(Bf["B1"], b, 0, Bf["BB1"], pp, Bpp, "copy")
        self.bcast_row(Bf["A1"], b, 1, Bf["BA1"], pp, Bpp, "scale", Bf["g1b"], Bf["Bg1b"])

    def phase_BC1(self):
        I, Sc, Bd = self.I, self.Sc, self.Bd
        with contextlib.ExitStack() as es:
            Bf = self.alloc_hT_bufs(es)
            W1 = self.sb(es, "W1", [128, 8, 1792], BF16)
            W2 = self.sb(es, "W2", [128, 8, 1792], BF16)
            BW = Buf()
            P = {}
            BP = Buf()
            for i, n in enumerate(["w0", "a0", "kk", "ka", "rk"]):
                P[n] = self.sb(es, "P" + n, [128, 512])
                self.dma([], [BP], P[n][:], I["rwv"][i].partition_broadcast(128))
            P["omka"] = self.sb(es, "Pomka", [128, 512])
            self.v("tensor_scalar", [BP], [BP], P["omka"][:], P["ka"][:], -1.0, 1.0, ALU.mult, ALU.add)
            lora = self.sb(es, "lora", [128, 3, 512], BF16)
            BL = Buf()
            es1 = contextlib.ExitStack()
            es_main = es
            es = es1
            mub = self.sb(es, "mub", [128, 1792])
            omub = self.sb(es, "omub", [128, 1792])
            Bmu = Buf()
            self.dma([], [Bmu], mub[:], I["mu"][0].partition_broadcast(128))
            self.v("tensor_scalar", [Bmu], [Bmu], omub[:], mub[:], -1.0, 1.0, ALU.mult, ALU.add)
            wv = I["w_in"].rearrange("(kc p) n -> p kc n", p=128)
            wtmp = [self.sb(es, "wtmp", [128, 8, 256]) for _ in range(2)]
            Bwt = [Buf(), Buf()]
            for c in range(7):
                k = c % 2
                c0 = c * 256
                self.dma([], [Bwt[k]], wtmp[k][:], wv[:, :, c0:c0 + 256])
                self.v("tensor_tensor", [Bwt[k], Bmu], [BW], W1[:, :, c0:c0 + 256], wtmp[k][:],
                       omub[:, c0:c0 + 256].unsqueeze(1).broadcast_to([128, 8, 256]), ALU.mult)
                self.g("tensor_tensor", [Bwt[k], Bmu], [BW], W2[:, :, c0:c0 + 256], wtmp[k][:],
                       mub[:, c0:c0 + 256].unsqueeze(1).broadcast_to([128, 8, 256]), ALU.mult)
            stage = self.sb(es, "lstage", [128, 3, 512])
            self.v("memset", [], [BL], stage[:], 0.0)
            self.dma([], [BL], stage[0:64, 0, :], I["w_up"])
            self.dma([], [BL], stage[64:128, 1, :], I["a_up"])
            self.dma([], [BL], stage[:, 2, :], I["g_up"])
            self.v("tensor_copy", [BL], [BL], lora[:], stage[:])
            self.s.barrier()
            es1.close()
            es = es_main
            NR = 5
            rot = [self.ps(es, "rot", [128, 512]) for _ in range(NR)]
            Brot = [Buf() for _ in range(NR)]
            rc = [0]

            def nxt():
                i = rc[0] % NR
                rc[0] += 1
                return rot[i], Brot[i]

            twd = self.sb(es, "twd", [128, 512], BF16)
            sgd = self.sb(es, "sgd", [128, 512], BF16)
            Btwd, Bsgd = Buf(), Buf()
            names = ["r", "w", "k", "v", "kk", "nk", "g"]
            O = {n: [self.sb(es, "o_" + n, [128, 512]) for _ in range(2)] for n in names}
            BO = {n: [Buf(), Buf()] for n in names}
            obs = [self.sb(es, "o_bs", [128, 8]) for _ in range(2)]
            Bobs = [Buf(), Buf()]
            tmpsL = [{n: self.sb(es, "t_" + n, [128, 512]) for n in ["zw", "a", "kku", "sq", "t1", "rk"]} for _ in range(2)]
            BtL = [{n: Buf() for n in tmpsL[0]} for _ in range(2)]
            smL = [self.sb(es, "sm", [128, 32]) for _ in range(2)]
            BsmL = [Buf(), Buf()]

            for b in range(NB):
                self.set_AB1(Bf, b, [rot[0], rot[1]], [Brot[0], Brot[1]])
                for blk in range(8):
                    hT, BhT = self.make_hT(Bf, b, blk)
                    self.dma([BhT], [Bd["s_hT"]], Sc["s_hT"][b, blk], hT[:, :, 4:516])
                    for ch in (12, 13):
                        p, Bp = nxt()
                        for kc in range(8):
                            self.mm([BW, BhT], [Bp], p[:, :], W1[:, kc, ch * 128:(ch + 1) * 128], hT[:, kc, 4:516],
                                    start=(kc == 0), stop=False)
                            self.mm([BW, BhT], [Bp], p[:, :], W2[:, kc, ch * 128:(ch + 1) * 128], hT[:, kc, 3:515],
                                    start=False, stop=(kc == 7))
                        if ch == 12:
                            self.a([Bp], [Btwd], twd[0:64, :], p[0:64, :], AF.Tanh)
                            self.a([Bp], [Btwd], twd[64:128, :], p[64:128, :], AF.Copy)
                        else:
                            self.a([Bp], [Bsgd], sgd[:, :], p[:, :], AF.Sigmoid)
                    for j in range(4):
                        n = blk * 4 + j
                        t0 = n * 128
                        q = n % 2
                        tmps, Bt, sm, Bsm = tmpsL[q], BtL[q], smL[q], BsmL[q]
                        cur = lambda kc: hT[:, kc, 4 + j * 128: 4 + (j + 1) * 128]
                        prv = lambda kc: hT[:, kc, 3 + j * 128: 3 + (j + 1) * 128]

                        def proj(c0):
                            p, Bp = nxt()
                            for kc in range(8):
                                self.mm([BW, BhT], [Bp], p[:, :], cur(kc), W1[:, kc, c0:c0 + 512],
                                        start=(kc == 0), stop=False)
                                self.mm([BW, BhT], [Bp], p[:, :], prv(kc), W2[:, kc, c0:c0 + 512],
                                        start=False, stop=(kc == 7))
                            return p, Bp

                        p_r, Bp_r = proj(0)
                        self.a([Bp_r], [BO["r"][q]], O["r"][q][:], p_r[:, :], AF.Copy)
                        p_v, Bp_v = proj(1024)
                        self.a([Bp_v], [BO["v"][q]], O["v"][q][:], p_v[:, :], AF.Copy)
                        p_w, Bp_w = nxt()
                        self.mm([Btwd, BL], [Bp_w], p_w[:, :], twd[0:64, j * 128:(j + 1) * 128], lora[0:64, 0, :])
                        self.v("tensor_tensor", [Bp_w, BP], [Bt["zw"]], tmps["zw"][:], p_w[:, :], P["w0"][:], ALU.add)
                        self.a([Bt["zw"]], [Bt["zw"]], tmps["zw"][:], tmps["zw"][:], AF.Sigmoid)
                        self.a([Bt["zw"]], [BO["w"][q]], O["w"][q][:], tmps["zw"][:], AF.Exp, scale=-0.6065306597126334)
                        p_a, Bp_a = nxt()
                        self.mm([Btwd, BL], [Bp_a], p_a[:, :], twd[64:128, j * 128:(j + 1) * 128], lora[64:128, 1, :])
                        self.v("tensor_tensor", [Bp_a, BP], [Bt["a"]], tmps["a"][:], p_a[:, :], P["a0"][:], ALU.add)
                        self.a([Bt["a"]], [Bt["a"]], tmps["a"][:], tmps["a"][:], AF.Sigmoid)
                        p_k, Bp_k = proj(512)
                        self.v("tensor_tensor", [Bp_k, BP], [Bt["kku"]], tmps["kku"][:], p_k[:, :], P["kk"][:], ALU.mult)
                        self.g("tensor_tensor", [Bt["kku"]], [Bt["sq"]], tmps["sq"][:], tmps["kku"][:], tmps["kku"][:], ALU.mult)
                        self.v("tensor_reduce", [Bt["sq"]], [Bsm], sm[:, 0:8],
                               tmps["sq"][:].rearrange("p (h k) -> p h k", k=64), AX.X, ALU.add)
                        self.a([Bsm], [Bsm], sm[:, 8:16], sm[:, 0:8], AF.Sqrt)
                        self.v("tensor_scalar", [Bsm], [Bsm], sm[:, 8:16], sm[:, 8:16], 1e-12, None, ALU.max)
                        self.v("reciprocal", [Bsm], [Bsm], sm[:, 16:24], sm[:, 8:16])
                        self.v("tensor_tensor", [Bt["kku"], Bsm], [BO["kk"][q]],
                               O["kk"][q][:].rearrange("p (h k) -> p h k", k=64),
                               tmps["kku"][:].rearrange("p (h k) -> p h k", k=64),
                               sm[:, 16:24].unsqueeze(2).broadcast_to([128, 8, 64]), ALU.mult)
                        self.v("tensor_tensor", [Bt["a"], BP], [Bt["t1"]], tmps["t1"][:], tmps["a"][:], P["ka"][:], ALU.mult)
                        self.g("tensor_tensor", [Bt["t1"], BP], [Bt["t1"]], tmps["t1"][:], tmps["t1"][:], P["omka"][:], ALU.add)
                        self.v("tensor_tensor", [Bp_k, Bt["t1"]], [BO["k"][q]], O["k"][q][:], p_k[:, :], tmps["t1"][:], ALU.mult)
                        self.v("scalar_tensor_tensor", [BO["kk"][q], Bt["a"]], [BO["nk"][q]], O["nk"][q][:],
                               O["kk"][q][:], -1.0, tmps["a"][:], ALU.mult, ALU.mult)
                        self.g("tensor_tensor", [BO["r"][q], BO["k"][q]], [Bt["rk"]], tmps["rk"][:], O["r"][q][:], O["k"][q][:], ALU.mult)
                        self.g("tensor_tensor", [Bt["rk"], BP], [Bt["rk"]], tmps["rk"][:], tmps["rk"][:], P["rk"][:], ALU.mult)
                        self.v("tensor_reduce", [Bt["rk"]], [Bobs[q]], obs[q][:, 0:8],
                               tmps["rk"][:].rearrange("p (h k) -> p h k", k=64), AX.X, ALU.add)
                        p_g, Bp_g = nxt()
                        self.mm([Bsgd, BL], [Bp_g], p_g[:, :], sgd[:, j * 128:(j + 1) * 128], lora[:, 2, :])
                        self.a([Bp_g], [BO["g"][q]], O["g"][q][:], p_g[:, :], AF.Copy)
                        for n_, sname in (("r", "s_r"), ("w", "s_w"), ("k", "s_k"), ("kk", "s_kk"), ("nk", "s_nk")):
                            dst = Sc[sname][b].rearrange("h t k -> t h k")[t0:t0 + 128]
                            self.dma([BO[n_][q]], [Bd[sname]], dst, O[n_][q][:].rearrange("p (h k) -> p h k", k=64))
                        dstv = Sc["s_v"][b * 64:(b + 1) * 64].rearrange("hv t l -> t hv l")[t0:t0 + 128]
                        self.dma([BO["v"][q]], [Bd["s_v"]], dstv, O["v"][q][:].rearrange("p (hv l) -> p hv l", l=8))
                        self.dma([BO["v"][q]], [Bd["s_vt"]], Sc["s_vt"][b, t0:t0 + 128, :], O["v"][q][:])
                        self.dma([BO["g"][q]], [Bd["s_g"]], Sc["s_g"][b, t0:t0 + 128, :], O["g"][q][:])
                        self.dma([Bobs[q]], [Bd["s_bs"]], Sc["s_bs"][b, t0:t0 + 128, :], obs[q][:])

    def phase_BC2(self):
        I, Sc, Bd = self.I, self.Sc, self.Bd
        with contextlib.ExitStack() as es:
            hTs = [self.sb(es, "hTf", [128, 8, 512], BF16) for _ in range(2)]
            BhTs = [Buf(), Buf()]
            Wf = self.sb(es, "Wf", [128, 8, 1544], BF16)
            BW = Buf()
            wv = I["w_in"].rearrange("(kc p) n -> p kc n", p=128)
            wtmp1 = self.sb(es, "wtmp", [128, 8, 256])
            wtmp = [wtmp1, wtmp1]
            Bwt1 = Buf()
            Bwt = [Bwt1, Bwt1]
            for c in range(7):
                k = c % 2
                c0 = c * 256
                w_ = min(256, 1544 - c0)
                self.dma([], [Bwt[k]], wtmp[k][:, :, 0:w_], wv[:, :, 1792 + c0:1792 + c0 + w_])
                self.v("tensor_copy", [Bwt[k]], [BW], Wf[:, :, c0:c0 + w_], wtmp[k][:, :, 0:w_])
            NR = 4
            rot = [self.ps(es, "rot", [128, 512]) for _ in range(NR)]
            Brot = [Buf() for _ in range(NR)]
            rc = [0]

            def nxt():
                i = rc[0] % NR
                rc[0] += 1
                return rot[i], Brot[i]

            qsb = [self.sb(es, "qsb", [128, 512], BF16) for _ in range(3)]
            Bq = [Buf() for _ in range(3)]
            qc_ = [0]
            vsb = [self.sb(es, "vsb", [128, 512], BF16) for _ in range(2)]
            Bv = [Buf(), Buf()]
            nfb = self.sb(es, "nfb", [8, 2])
            Bnfb = Buf()
            self.dma([], [Bnfb], nfb[:, 0:1], I["fbias"])
            self.v("tensor_scalar", [Bnfb], [Bnfb], nfb[:, 1:2], nfb[:, 0:1], -1.0, None, ALU.mult)
            ones8 = self.sb(es, "ones8", [8, 512])
            ones8b = self.sb(es, "ones8b", [8, 512], BF16)
            Bon = Buf()
            self.v("memset", [], [Bon], ones8[:], 1.0)
            self.v("tensor_copy", [Bon], [Bon], ones8b[:], ones8[:])
            fe = self.sb(es, "fe", [8, 512])
            Bfe = Buf()
            cum = [self.sb(es, "cum", [8, 512]) for _ in range(2)]
            Bcum = [Buf(), Buf()]
            r1 = self.sb(es, "r1", [8, 512])
            Br1 = Buf()
            parts = [self.sb(es, "parts", [8, 6, 512], BF16) for _ in range(2)]
            Bparts = [Buf(), Buf()]
            def loadh(nb_):
                self.dma([Bd["s_hT"]], [BhTs[nb_ % 2]], hTs[nb_ % 2][:], Sc["s_hT"][nb_ // 8, nb_ % 8])

            loadh(0)
            for b in range(NB):
                for blk in range(8):
                    t0 = blk * 512
                    nb = b * 8 + blk
                    if nb + 1 < NB * 8:
                        loadh(nb + 1)
                    hT, BhT = hTs[nb % 2], BhTs[nb % 2]
                    for which, c_base, sname in ((0, 0, "s_qa"), (1, 512, "s_ka")):
                        for hp in range(4):
                            p, Bp = nxt()
                            for kc in range(8):
                                self.mm([BW, BhT], [Bp], p[:, :], Wf[:, kc, c_base + hp * 128: c_base + (hp + 1) * 128],
                                        hT[:, kc, :], start=(kc == 0), stop=(kc == 7))
                            qi = qc_[0] % 3
                            qc_[0] += 1
                            self.a([Bp], [Bq[qi]], qsb[qi][:], p[:, :], AF.Copy, scale=(0.125 if which == 0 else 1.0))
                            for jj in range(2):
                                self.dma([Bq[qi]], [Bd[sname]], Sc[sname][b, 2 * hp + jj, 0:64, t0:t0 + 512],
                                         qsb[qi][jj * 64:(jj + 1) * 64, :])
                            yield
                    p, Bp = nxt()
                    for kc in range(8):
                        self.mm([BW, BhT], [Bp], p[0:8, :], Wf[:, kc, 1536:1544], hT[:, kc, :],
                                start=(kc == 0), stop=(kc == 7))
                    self.a([Bp, Bnfb], [Bfe], fe[:], p[0:8, :], AF.Exp, bias=nfb[:, 1:2], scale=-1.0)
                    self.a([Bfe], [Bfe], fe[:], fe[:], AF.Ln, bias=1.0)
                    ck = nb % 2
                    init = 0.0 if blk == 0 else cum[1 - ck][:, 511:512]
                    self.v("tensor_tensor_scan", [Bfe, Bon, Bcum[1 - ck]], [Bcum[ck]], cum[ck][:], ones8[:], fe[:], init,
                           ALU.mult, ALU.subtract)
                    pt, Bpt = parts[ck], Bparts[ck]
                    self.v("tensor_copy", [Bcum[ck]], [Bpt], pt[:, 0, :], cum[ck][:])
                    self.v("tensor_tensor", [Bcum[ck], Bpt], [Br1], r1[:], cum[ck][:], pt[:, 0, :], ALU.subtract)
                    self.v("tensor_copy", [Br1], [Bpt], pt[:, 1, :], r1[:])
                    self.v("tensor_tensor", [Br1, Bpt], [Br1], r1[:], r1[:], pt[:, 1, :], ALU.subtract)
                    self.v("tensor_copy", [Br1], [Bpt], pt[:, 2, :], r1[:])
                    self.v("tensor_scalar", [Bpt], [Bpt], pt[:, 3:6, :], pt[:, 0:3, :], -1.0, None, ALU.mult)
                    for i in range(3):
                        self.dma([Bpt], [Bd["s_qa"]], Sc["s_qa"][b, :, 64 + i, t0:t0 + 512], pt[:, i, :])
                        self.dma([Bon], [Bd["s_qa"]], Sc["s_qa"][b, :, 67 + i, t0:t0 + 512], ones8b[:])
                        self.dma([Bon], [Bd["s_ka"]], Sc["s_ka"][b, :, 64 + i, t0:t0 + 512], ones8b[:])
                        self.dma([Bpt], [Bd["s_ka"]], Sc["s_ka"][b, :, 67 + i, t0:t0 + 512], pt[:, 3 + i, :])
                    yield
                    for j in range(4):
                        n = blk * 4 + j
                        p, Bp = nxt()
                        for kc in range(8):
                            self.mm([BW, BhT], [Bp], p[:, :], hT[:, kc, j * 128:(j + 1) * 128],
                                    Wf[:, kc, 1024:1536], start=(kc == 0), stop=(kc == 7))
                        self.a([Bp], [Bv[n % 2]], vsb[n % 2][:], p[:, :], AF.Copy)
                        self.dma([Bv[n % 2]], [Bd["s_fv"]], Sc["s_fv"][b, n * 128:(n + 1) * 128, :], vsb[n % 2][:])
                        yield

    def phase_EH(self):
        gE = self.phase_E()
        gH = itertools.chain(self.phase_BC2(), self.phase_H())
        doneH = False
        for _ in gE:
            if not doneH:
                try:
                    next(gH)
                except StopIteration:
                    doneH = True
        if not doneH:
            for _ in gH:
                pass

    def phase_E(self):
        Sc, Bd = self.Sc, self.Bd
        with contextlib.ExitStack() as es:
            St = [self.sb(es, "St", [128, 8, 64]) for _ in range(2)]
            BS = [Buf(), Buf()]
            self.v("memset", [], [BS[0]], St[0][:], 0.0)
            self.v("memset", [], [BS[1]], St[1][:], 0.0)
            Sp = self.sb(es, "Sp", [128, 8, 64])
            BSp = Buf()
            tmp = self.sb(es, "tmp", [128, 8, 64])
            tmp2 = self.sb(es, "tmp2", [128, 8, 64])
            Btmp, Btmp2 = Buf(), Buf()
            tmp3 = [self.sb(es, "tmp3", [128, 8, 64]) for _ in range(2)]
            tmp4 = [self.sb(es, "tmp4", [128, 8, 64]) for _ in range(3)]
            Bt3 = [Buf(), Buf()]
            Bt4 = [Buf(), Buf(), Buf()]
            sa = [self.sb(es, "sa", [128, 8]) for _ in range(2)]
            Bsa = [Buf(), Buf()]
            ops = ["s_kk", "s_w", "s_nk", "s_k", "s_r"]
            OB = {n: [self.sb(es, "ob" + n, [128, CH, 64]) for _ in range(2)] for n in ops}
            BOB = {n: [Buf(multi=True), Buf(multi=True)] for n in ops}
            vB = [self.sb(es, "vB", [128, CH, 8]) for _ in range(2)]
            BvB = [Buf(), Buf()]
            yB = [self.sb(es, "yB", [128, CH, 8]) for _ in range(2)]
            ByB = [Buf(), Buf()]
            nch = S // CH

            def load(c):
                k = c % 2
                t0 = c * CH
                gr = Grp()
                for n in ops:
                    for bh in range(16):
                        b, h = bh // 8, bh % 8
                        src = Sc[n][b, h, t0:t0 + CH, :].partition_broadcast(8)
                        self.dma([Bd[n]], [BOB[n][k]], OB[n][k][bh * 8:(bh + 1) * 8, :, :], src, grp=gr)
                self.dma([Bd["s_v"]], [BvB[k]], vB[k][:], Sc["s_v"][:, t0:t0 + CH, :], grp=gr)
                self.s.join([BOB[n][k] for n in ops] + [BvB[k]])

            def bc(ap):
                return ap.unsqueeze(1).broadcast_to([128, 8, 64])

            def poolC(t):
                c, i = t // CH, t % CH
                k = c % 2
                self.g("tensor_tensor", [BS[t % 2], BOB["s_r"][k]], [Bt4[t % 3]], tmp4[t % 3][:], St[t % 2][:],
                       bc(OB["s_r"][k][:, i, :]), ALU.mult)

            def dveY(t):
                c, i = t // CH, t % CH
                k = c % 2
                self.v("tensor_reduce", [Bt4[t % 3]], [ByB[k]], yB[k][:, i, :], tmp4[t % 3][:], AX.X, ALU.add)
                if i == CH - 1:
                    self.dma([ByB[k]], [Bd["s_y"]], Sc["s_y"][:, c * CH:(c + 1) * CH, :], yB[k][:])

            load(0)
            for c in range(nch):
                k = c % 2
                for i in range(CH):
                    if i == 1 and c + 1 < nch:
                        load(c + 1)
                    t = c * CH + i
                    q = t % 2
                    So, Sn = St[(t + 1) % 2], St[t % 2]
                    BSo, BSn = BS[(t + 1) % 2], BS[t % 2]
                    for vl in range(8):
                        self.a([BOB["s_k"][k], BvB[k]], [Bt3[q]], tmp3[q][:, vl, :], OB["s_k"][k][:, i, :], AF.Copy,
                               scale=vB[k][:, i, vl:vl + 1])
                    self.g("tensor_tensor", [BSo, BOB["s_w"][k]], [BSp], Sp[:], So[:], bc(OB["s_w"][k][:, i, :]), ALU.mult)
                    if t >= 1:
                        poolC(t - 1)
                    self.v("tensor_tensor", [BSo, BOB["s_kk"][k]], [Btmp], tmp[:], So[:], bc(OB["s_kk"][k][:, i, :]), ALU.mult)
                    self.v("tensor_reduce", [Btmp], [Bsa[q]], sa[q][:], tmp[:], AX.X, ALU.add)
                    if t >= 2:
                        dveY(t - 2)
                    self.v("tensor_tensor", [BSp, Bt3[q]], [BSp], Sp[:], Sp[:], tmp3[q][:], ALU.add)
                    self.v("tensor_tensor", [Bsa[q], BOB["s_nk"][k]], [Btmp2], tmp2[:],
                           sa[q][:].unsqueeze(2).broadcast_to([128, 8, 64]), bc(OB["s_nk"][k][:, i, :]), ALU.mult)
                    self.v("tensor_tensor", [BSp, Btmp2], [BSn], Sn[:], Sp[:], tmp2[:], ALU.add)
                    yield
            poolC(S - 1)
            dveY(S - 2)
            dveY(S - 1)

    def phase_H(self):
        I, Sc, Bd = self.I, self.Sc, self.Bd
        with contextlib.ExitStack() as es:
            mstage = self.sb(es, "mstage", [128, 128])
            maskb = self.sb(es, "maskb", [128, 128], BF16)
            Bm = Buf()
            self.dma([], [Bm], mstage[:], I["maskneg"])
            self.v("tensor_copy", [Bm], [Bm], maskb[:], mstage[:])
            qa = [self.sb(es, "qa", [70, S], BF16) for _ in range(2)]
            ka = [self.sb(es, "ka", [70, S], BF16) for _ in range(2)]
            vt = [self.sb(es, "vt", [128, 32, 65], BF16) for _ in range(2)]
            Bqa, Bka, Bvt = [Buf(), Buf()], [Buf(), Buf()], [Buf(), Buf()]
            for k in range(2):
                self.v("memset", [], [Bvt[k]], vt[k][:], 1.0)
            yf = [self.sb(es, "yf", [128, 32, 64]) for _ in range(2)]
            Byf = [Buf(), Buf()]
            NS = 3
            sps = [self.ps(es, "sps", [128, 512]) for _ in range(NS)]
            Bsps = [Buf() for _ in range(NS)]
            acc = [self.ps(es, "acc", [128, 512]) for _ in range(4)]
            Bacc = [Buf() for _ in range(4)]
            NP = 3
            pts = [self.sb(es, "pts", [128, 512], BF16) for _ in range(NP)]
            Bpts = [Buf() for _ in range(NP)]
            rec = self.sb(es, "rec", [128, 8])
            Brec = Buf()
            cnt = 0

            def load(bh):
                b, h = bh // 8, bh % 8
                k = bh % 2
                gr = Grp()
                self.dma([Bd["s_qa"]], [Bqa[k]], qa[k][:], Sc["s_qa"][b, h], grp=gr)
                self.dma([Bd["s_ka"]], [Bka[k]], ka[k][:], Sc["s_ka"][b, h], grp=gr)
                src = Sc["s_fv"][b].rearrange("(n p) c -> p n c", p=128)[:, :, h * 64:(h + 1) * 64]
                self.dma([Bd["s_fv"]], [Bvt[k]], vt[k][:, :, 0:64], src, grp=gr)
                self.s.join([Bqa[k], Bka[k], Bvt[k]])

            load(0)
            for bh in range(16):
                b, h = bh // 8, bh % 8
                k = bh % 2
                if bh + 1 < 16:
                    load(bh + 1)
                for qc in range(8):
                    for kt in range(4 * qc + 4):
                        d = kt - 4 * qc
                        si = cnt % NS
                        pi = cnt % NP
                        cnt += 1
                        sp_, Bsp = sps[si], Bsps[si]
                        lhs = ka[k][:, kt * 128:(kt + 1) * 128]
                        if d < 0:
                            c0 = 0
                            self.mm([Bka[k], Bqa[k]], [Bsp], sp_[:, 0:512], lhs, qa[k][:, qc * 512:(qc + 1) * 512])
                        else:
                            c0 = d * 128
                            q0 = qc * 512 + c0
                            self.mm([Bka[k], Bqa[k]], [Bsp], sp_[:, c0:c0 + 128], lhs, qa[k][:, q0:q0 + 128],
                                    start=True, stop=False)
                            self.mm([Bm, self.Bconst], [Bsp], sp_[:, c0:c0 + 128], self.ident_b[:], maskb[:],
                                    start=False, stop=True)
                            if c0 + 128 < 512:
                                self.mm([Bka[k], Bqa[k]], [Bsp], sp_[:, c0 + 128:512], lhs,
                                        qa[k][:, q0 + 128:qc * 512 + 512])
                        self.a([Bsp], [Bpts[pi]], pts[pi][:, c0:512], sp_[:, c0:512], AF.Exp)
                        for qs in range(max(d, 0), 4):
                            self.mm([Bpts[pi], Bvt[k]], [Bacc[qs]], acc[qs][:, 0:65], pts[pi][:, qs * 128:(qs + 1) * 128],
                                    vt[k][:, kt, :], start=(kt == 0), stop=(kt == 4 * qc + qs))
                        yield
                    for qs in range(4):
                        n = 4 * qc + qs
                        self.v("reciprocal", [Bacc[qs]], [Brec], rec[:, qs:qs + 1], acc[qs][:, 64:65])
                        self.v("tensor_scalar", [Bacc[qs], Brec], [Byf[k]], yf[k][:, n, :], acc[qs][:, 0:64],
                               rec[:, qs:qs + 1], None, ALU.mult)
                    yield
                dst = Sc["s_yf"][b].rearrange("(n p) c -> p n c", p=128)[:, :, h * 64:(h + 1) * 64]
                self.dma([Byf[k]], [Bd["s_yf"]], dst, yf[k][:])

    def phase_I(self):
        I, Sc, Bd = self.I, self.Sc, self.Bd
        es = contextlib.ExitStack()
        with es:
            pes = self.es
            self.Wts = self.sb(pes, "Wts", [128, NT, 2])
            self.OH1 = self.sb(pes, "OH1", [128, NT, 32])
            self.OH2 = self.sb(pes, "OH2", [128, NT, 32])
            self.OHb = self.sb(pes, "OHb", [128, NT, 32], BF16)
            self.BWts, self.BOH = Buf(), Buf()
            wout = self.sb(es, "wout", [128, 8, D], BF16)
            BWo = Buf()
            wv = I["w_out"].rearrange("(kc p) n -> p kc n", p=128)
            wtmp = [self.sb(es, "wtmp", [128, 8, 256]) for _ in range(2)]
            Bwt = [Buf(), Buf()]
            for c in range(4):
                k = c % 2
                self.dma([], [Bwt[k]], wtmp[k][:], wv[:, :, c * 256:(c + 1) * 256])
                self.v("tensor_copy", [Bwt[k]], [BWo], wout[:, :, c * 256:(c + 1) * 256], wtmp[k][:])
            wr = self.sb(es, "wr", [128, 8, 36])
            brb = self.sb(es, "brb", [128, 36])
            BWr = Buf()
            self.dma([], [BWr], wr[:], I["w_r"].rearrange("(kc p) n -> p kc n", p=128))
            self.dma([], [BWr], brb[:], I["b_r"][0].partition_broadcast(128))
            Pl = self.sb(es, "Pl", [128, 2, 512])
            BPl = Buf()
            self.dma([], [BPl], Pl[:, 0, :], I["rwv"][5].partition_broadcast(128))
            self.dma([], [BPl], Pl[:, 1, :], I["rwv"][6].partition_broadcast(128))
            g2b = self.sb(es, "g2b", [128, D])
            Bg2 = Buf()
            self.dma([], [Bg2], g2b[:], I["g2"][0].partition_broadcast(128))
            G1 = self.sb(es, "G1", [128, D])
            A2 = self.sb(es, "A2", [128, D])
            B2 = self.sb(es, "B2", [128, D])
            BG1, BA2, BB2 = Buf(), Buf(), Buf()
            pso = [self.ps(es, "pso", [128, 512]) for _ in range(2)]
            Bpso = [Buf(), Buf()]
            pmT = self.ps(es, "pmT", [128, 8, 128], BF16)
            BpmT = Buf()
            phT = [self.ps(es, "phT", [128, 4, 128]) for _ in range(2)]
            BphT = [Buf(), Buf()]
            pl = self.ps(es, "pl", [128, 512])
            Bpl = Buf()
            yt = [self.sb(es, "yt", [128, 512]) for _ in range(2)]
            gt = [self.sb(es, "gt", [128, 512]) for _ in range(2)]
            vtk = [self.sb(es, "vtk", [128, 512]) for _ in range(2)]
            bst = [self.sb(es, "bst", [128, 8]) for _ in range(2)]
            yft = [self.sb(es, "yft", [128, 512]) for _ in range(2)]
            xt = [self.sb(es, "xt", [128, D]) for _ in range(2)]
            Bin = [Buf(multi=True), Buf(multi=True)]
            ysq = self.sb(es, "ysq", [128, 512])
            yn = self.sb(es, "yn", [128, 512])
            bon = self.sb(es, "bon", [128, 512])
            Bw_ = Buf()
            st = self.sb(es, "st", [128, 64])
            Bst = Buf()
            mix = self.sb(es, "mix", [128, D], BF16)
            Bmix = Buf()
            mixT = self.sb(es, "mixT", [128, 8, 128], BF16)
            BmixT = Buf()
            x1 = [self.sb(es, "x1", [128, D]) for _ in range(2)]
            Bx1 = [Buf(), Buf()]
            junk = self.sb(es, "junk", [128, D])
            Bjunk = Buf()
            ss = self.sb(es, "ss", [128, 4])
            Bss = Buf()
            h2 = [self.sb(es, "h2", [128, D]) for _ in range(2)]
            Bh2 = [Buf(), Buf()]
            h2b = [self.sb(es, "h2b", [128, D], BF16) for _ in range(2)]
            Bh2b = [Buf(), Buf()]
            h2T = self.sb(es, "h2T", [128, 8, 128])
            Bh2T = Buf()
            lg = self.sb(es, "lg", [128, 36])
            rs = self.sb(es, "rs", [128, 96])
            Brs = Buf()

            def v3(ap):
                return ap.rearrange("p (h k) -> p h k", k=64)

            def b8(ap):
                return ap.unsqueeze(2).broadcast_to([128, 8, 64])

            def load(n):
                b, j = n // 32, n % 32
                t0 = j * 128
                k = n % 2
                src = Sc["s_y"][b * 64:(b + 1) * 64].rearrange("hv t l -> t hv l")[t0:t0 + 128]
                gr = Grp()
                self.dma([Bd["s_y"]], [Bin[k]], yt[k][:].rearrange("p (hv l) -> p hv l", l=8), src, grp=gr)
                self.dma([Bd["s_g"]], [Bin[k]], gt[k][:], Sc["s_g"][b, t0:t0 + 128, :], grp=gr)
                self.dma([Bd["s_vt"]], [Bin[k]], vtk[k][:], Sc["s_vt"][b, t0:t0 + 128, :], grp=gr)
                self.dma([Bd["s_bs"]], [Bin[k]], bst[k][:], Sc["s_bs"][b, t0:t0 + 128, :], grp=gr)
                self.dma([Bd["s_yf"]], [Bin[k]], yft[k][:], Sc["s_yf"][b, t0:t0 + 128, :], grp=gr)
                self.dma([], [Bin[k]], xt[k][:], I["x"][b, t0:t0 + 128, :], grp=gr)
                self.s.join([Bin[k]])

            def dup(name, shape, dt=F32):
                return [self.sb(es, name + "2", shape, dt), None]

            L2 = dict(ysq=[ysq, self.sb(es, "ysq2", [128, 512])], yn=[yn, self.sb(es, "yn2", [128, 512])],
                      bon=[bon, self.sb(es, "bon2", [128, 512])], st=[st, self.sb(es, "st2", [128, 64])],
                      mix=[mix, self.sb(es, "mix2", [128, D], BF16)], mixT=[mixT, self.sb(es, "mixT2", [128, 8, 128], BF16)],
                      junk=[junk, self.sb(es, "junk2", [128, D])], ss=[ss, self.sb(es, "ss2", [128, 4])],
                      h2T=[h2T, self.sb(es, "h2T2", [128, 8, 128])], lg=[lg, self.sb(es, "lg2", [128, 36])],
                      rs=[rs, self.sb(es, "rs2", [128, 96])], pmT=[pmT, self.ps(es, "pmT2", [128, 8, 128], BF16)],
                      pl=[pl, self.ps(es, "pl2", [128, 512])])
            B2_ = {nm: [Buf(), Buf()] for nm in ("Bw_", "Bst", "Bmix", "BmixT", "Bjunk", "Bss", "Bh2T", "Brs", "BpmT", "Bpl")}
            load(0)
            for n in range(NT):
                b, j = n // 32, n % 32
                t0 = j * 128
                k = n % 2
                ysq, yn, bon, st, mix, mixT, junk, ss, h2T, lg, rs, pmT, pl = [L2[nm][k] for nm in (
                    "ysq", "yn", "bon", "st", "mix", "mixT", "junk", "ss", "h2T", "lg", "rs", "pmT", "pl")]
                Bw_, Bst, Bmix, BmixT, Bjunk, Bss, Bh2T, Brs, BpmT, Bpl = [B2_[nm][k] for nm in (
                    "Bw_", "Bst", "Bmix", "BmixT", "Bjunk", "Bss", "Bh2T", "Brs", "BpmT", "Bpl")]
                if j == 0:
                    self.bcast_row(G1, b, 2, BG1, pso, Bpso, "copy")
                    self.bcast_row(B2, b, 3, BB2, pso, Bpso, "copy")
                    self.bcast_row(A2, b, 4, BA2, pso, Bpso, "scale", g2b, Bg2)
                if n + 1 < NT:
                    load(n + 1)
                y = yt[k]
                self.v("tensor_reduce", [Bin[k]], [Bst], st[:, 0:8], v3(y[:]), AX.X, ALU.add)
                self.g("tensor_tensor", [Bin[k]], [Bw_], ysq[:], y[:], y[:], ALU.mult)
                self.v("tensor_reduce", [Bw_], [Bst], st[:, 8:16], v3(ysq[:]), AX.X, ALU.add)
                self.v("tensor_scalar", [Bst], [Bst], st[:, 16:24], st[:, 0:8], 1.0 / 64, None, ALU.mult)
                self.v("tensor_tensor", [Bst], [Bst], st[:, 24:32], st[:, 16:24], st[:, 16:24], ALU.mult)
                self.v("scalar_tensor_tensor", [Bst], [Bst], st[:, 32:40], st[:, 8:16], 1.0 / 64, st[:, 24:32],
                       ALU.mult, ALU.subtract)
                self.a([Bst, self.Bconst], [Bst], st[:, 40:48], st[:, 32:40], AF.Ln, bias=self.eps6[:, 1:2], scale=1.0)
                self.a([Bst], [Bst], st[:, 48:56], st[:, 40:48], AF.Exp, scale=-0.5)
                self.v("tensor_tensor", [Bin[k], Bst], [Bw_], v3(yn[:]), v3(y[:]), b8(st[:, 16:24]), ALU.subtract)
                self.v("tensor_tensor", [Bw_, Bst], [Bw_], v3(yn[:]), v3(yn[:]), b8(st[:, 48:56]), ALU.mult)
                self.v("tensor_tensor", [Bw_, BPl], [Bw_], yn[:], yn[:], Pl[:, 0, :], ALU.mult)
                self.g("tensor_tensor", [Bw_, BPl], [Bw_], yn[:], yn[:], Pl[:, 1, :], ALU.add)
                self.g("tensor_tensor", [Bin[k]], [Bw_], v3(bon[:]), v3(vtk[k][:]), b8(bst[k][:, 0:8]), ALU.mult)
                self.v("tensor_tensor", [Bw_], [Bw_], yn[:], yn[:], bon[:], ALU.add)
                self.v("tensor_tensor", [Bw_, Bin[k]], [Bmix], mix[:, 0:512], yn[:], gt[k][:], ALU.mult)
                self.a([Bin[k]], [Bmix], mix[:, 512:1024], yft[k][:], AF.Copy)
                for kc in range(8):
                    self.tr([Bmix, self.Bconst], [BpmT], pmT[:, kc, :], mix[:, kc * 128:(kc + 1) * 128], self.ident_b[:])
                self.a([BpmT], [BmixT], mixT[:], pmT[:], AF.Copy)
                for half in range(2):
                    for kc in range(8):
                        self.mm([BmixT, BWo], [Bpso[half]], pso[half][:, :], mixT[:, kc, :],
                                wout[:, kc, half * 512:(half + 1) * 512], start=(kc == 0), stop=(kc == 7))
                    hs = slice(half * 512, (half + 1) * 512)
                    self.v("tensor_tensor", [Bpso[half], BG1], [Bjunk], junk[:, hs], pso[half][:, :], G1[:, hs], ALU.mult)
                    self.v("tensor_tensor", [Bjunk, Bin[k]], [Bx1[k]], x1[k][:, hs], junk[:, hs], xt[k][:, hs], ALU.add)
                self.dma([Bx1[k]], [Bd["s_x1"]], Sc["s_x1"][b, t0:t0 + 128, :], x1[k][:])
                rstd = self.rms_rstd(x1[k], Bx1[k], junk, Bjunk, ss, Bss)
                self.v("scalar_tensor_tensor", [Bx1[k], Bss, BA2], [Bjunk], junk[:], x1[k][:], rstd, A2[:], ALU.mult, ALU.mult)
                self.v("tensor_tensor", [Bjunk, BB2], [Bh2[k]], h2[k][:], junk[:], B2[:], ALU.add)
                self.a([Bh2[k]], [Bh2b[k]], h2b[k][:], h2[k][:], AF.Copy)
                self.dma([Bh2b[k]], [Bd["s_h2"]], Sc["s_h2"][n * 128:(n + 1) * 128, :], h2b[k][:])
                for hh in range(2):
                    for kc4 in range(4):
                        kc = hh * 4 + kc4
                        self.tr([Bh2[k], self.Bconst], [BphT[hh]], phT[hh][:, kc4, :], h2[k][:, kc * 128:(kc + 1) * 128],
                                self.ident_f[:])
                    self.a([BphT[hh]], [Bh2T], h2T[:, hh * 4:(hh + 1) * 4, :], phT[hh][:], AF.Copy)
                for kc in range(8):
                    self.mm([Bh2T, BWr], [Bpl], pl[:, 0:36], h2T[:, kc, :], wr[:, kc, :], start=(kc == 0), stop=(kc == 7))
                self.v("tensor_tensor", [Bpl, BWr], [Brs], lg[:], pl[:, 0:36], brb[:], ALU.add)
                R = [Brs]
                self.v("tensor_reduce", R, R, rs[:, 0:1], lg[:, 0:4], AX.X, ALU.max)
                self.v("tensor_scalar", R, R, rs[:, 1:2], rs[:, 0:1], -1.0, None, ALU.mult)
                self.a(R, R, rs[:, 4:8], lg[:, 0:4], AF.Exp, bias=rs[:, 1:2], scale=1.0)
                self.v("tensor_reduce", R, R, rs[:, 2:3], rs[:, 4:8], AX.X, ALU.add)
                self.v("reciprocal", R, R, rs[:, 3:4], rs[:, 2:3])
                self.v("tensor_scalar", R, R, rs[:, 8:12], lg[:, 0:4], rs[:, 0:1], None, ALU.is_equal)
                self.v("tensor_tensor", R, R, rs[:, 16:48].rearrange("p (g e) -> p g e", e=8),
                       lg[:, 4:36].rearrange("p (g e) -> p g e", e=8),
                       rs[:, 8:12].unsqueeze(2).broadcast_to([128, 4, 8]), ALU.mult)
                self.v("tensor_reduce", R, R, rs[:, 48:56], rs[:, 16:48].rearrange("p (g e) -> p e g", e=8), AX.X, ALU.add)
                self.v("max", R, R, rs[:, 56:64], rs[:, 48:56])
                self.v("tensor_scalar", R, R, rs[:, 64:72], rs[:, 48:56], rs[:, 56:57], None, ALU.is_equal)
                self.v("tensor_scalar", R, R, rs[:, 72:80], rs[:, 48:56], rs[:, 57:58], None, ALU.is_equal)
                self.v("tensor_scalar", R, R, rs[:, 80:81], rs[:, 56:57], -1.0, None, ALU.mult)
                self.a(R, R, rs[:, 81:82], rs[:, 57:58], AF.Exp, bias=rs[:, 80:81], scale=1.0)
                self.v("tensor_scalar", R, R, rs[:, 82:83], rs[:, 81:82], 1.0, None, ALU.add)
                self.v("reciprocal", R, R, rs[:, 83:84], rs[:, 82:83])
                self.v("tensor_tensor", R, [self.BWts], self.Wts[:, n, 0:1], rs[:, 3:4], rs[:, 83:84], ALU.mult)
                self.v("tensor_tensor", R + [self.BWts], [self.BWts], self.Wts[:, n, 1:2], self.Wts[:, n, 0:1], rs[:, 81:82], ALU.mult)
                gohb = rs[:, 8:12].unsqueeze(2).broadcast_to([128, 4, 8])
                self.v("tensor_tensor", R, [self.BOH], self.OH1[:, n, :].rearrange("p (g e) -> p g e", e=8), gohb,
                       rs[:, 64:72].unsqueeze(1).broadcast_to([128, 4, 8]), ALU.mult)
                self.v("tensor_tensor", R, [self.BOH], self.OH2[:, n, :].rearrange("p (g e) -> p g e", e=8), gohb,
                       rs[:, 72:80].unsqueeze(1).broadcast_to([128, 4, 8]), ALU.mult)
                self.v("tensor_tensor", [self.BOH], [self.BOH], self.OHb[:, n, :], self.OH1[:, n, :], self.OH2[:, n, :], ALU.add)

    def phase_K(self):
        I, Sc, Bd = self.I, self.Sc, self.Bd
        pes = self.es
        self.slotI = [self.sb(pes, "slotI", [128, NT], I32) for _ in range(2)]
        self.Bslot = Buf()
        with contextlib.ExitStack() as es0:
            idxG = self.sb(es0, "idxG", [128, NBLK, 8], I32)
            idxD = self.sb(es0, "idxD", [128, NBLK, 4], I32)
            Bidx = Buf()
            with contextlib.ExitStack() as es:
                lst = self.sb(es, "lst", [128, 128])
                lsb = self.sb(es, "lsb", [128, 128], BF16)
                onb = self.sb(es, "onb", [128, 128], BF16)
                Bc = Buf()
                self.dma([], [Bc], lst[:], I["lstrict"])
                self.v("tensor_copy", [Bc], [Bc], lsb[:], lst[:])
                self.v("memset", [], [Bc], onb[:], 1.0)
                thr64 = self.sb(es, "thr64", [128, NTHR])
                thr160 = self.sb(es, "thr160", [128, NBLK])
                ipk = self.sb(es, "ipk", [128, 8])
                ipf = self.sb(es, "ipf", [128, 4])
                self.dma([], [Bc], thr64[:], I["thr64"][0].partition_broadcast(128))
                self.dma([], [Bc], thr160[:], I["thr160"][0].partition_broadcast(128))
                self.dma([], [Bc], ipk[:], I["iota_pk"])
                self.dma([], [Bc], ipf[:], I["iota_pf"])
                ones64 = self.sb(es, "ones64", [128, 64])
                self.v("memset", [], [Bc], ones64[:], 1.0)
                base = self.sb(es, "base", [128, NT, 32])
                tot = self.sb(es, "tot", [128, NT, 32])
                incl = self.sb(es, "incl", [128, NT, 32])
                Bb = Buf()
                pp = [self.ps(es, "pk", [128, 512]) for _ in range(2)]
                Bpp = [Buf(), Buf()]
                OHf = self.OHb[:].rearrange("p n e -> p (n e)")
                for c in range(4):
                    self.mm([self.BOH, Bc], [Bpp[0]], pp[0][:, :], lsb[:], OHf[:, c * 512:(c + 1) * 512])
                    self.a([Bpp[0]], [Bb], base[:].rearrange("p n e -> p (n e)")[:, c * 512:(c + 1) * 512], pp[0][:, :], AF.Copy)
                    self.mm([self.BOH, Bc], [Bpp[1]], pp[1][:, :], onb[:], OHf[:, c * 512:(c + 1) * 512])
                    self.a([Bpp[1]], [Bb], tot[:].rearrange("p n e -> p (n e)")[:, c * 512:(c + 1) * 512], pp[1][:, :], AF.Copy)
                for e in range(32):
                    self.v("tensor_tensor_scan", [Bb, Bc], [Bb], incl[:, :, e], ones64[:, 0:NT], tot[:, :, e], 0.0,
                           ALU.mult, ALU.add)
                sm = self.sb(es, "smk", [128, 8, 32])
                Bsm = Buf()
                cmp = self.sb(es, "cmp", [128, 32, NTHR])
                self.v("tensor_copy", [Bb], [Bsm], sm[:, 0, :], incl[:, NT - 1, :])
                self.v("tensor_tensor", [Bsm, Bc], [Bsm], cmp[:], sm[:, 0, :].unsqueeze(2).broadcast_to([128, 32, NTHR]),
                       thr64[:].unsqueeze(1).broadcast_to([128, 32, NTHR]), ALU.is_gt)
                self.v("tensor_reduce", [Bsm], [Bsm], sm[:, 1, :], cmp[:], AX.X, ALU.add)
                self.v("tensor_scalar", [Bsm], [Bsm], sm[:, 2, :], sm[:, 1, :], float(BSZ), None, ALU.mult)
                self.v("tensor_tensor_scan", [Bsm, Bc], [Bsm], sm[:, 3, :], ones64[:, 0:32], sm[:, 2, :], 0.0,
                       ALU.mult, ALU.add)
                self.v("tensor_tensor", [Bsm], [Bsm], sm[:, 4, :], sm[:, 3, :], sm[:, 2, :], ALU.subtract)
                self.v("tensor_tensor", [Bb], [Bb], incl[:], incl[:], tot[:], ALU.subtract)
                self.v("tensor_tensor", [Bb], [Bb], base[:], base[:], incl[:], ALU.add)
                self.v("tensor_tensor", [Bb, Bsm], [Bb], base[:], base[:],
                       sm[:, 4, :].unsqueeze(1).broadcast_to([128, NT, 32]), ALU.add)
                slf = self.sb(es, "slf", [128, 2, NT])
                for kx, OHk in enumerate((self.OH1, self.OH2)):
                    self.v("tensor_tensor", [Bb, self.BOH], [Bb], tot[:], base[:], OHk[:], ALU.mult)
                    self.v("tensor_reduce", [Bb], [Bsm], slf[:, kx, :], tot[:], AX.X, ALU.add)
                    self.v("tensor_copy", [Bsm], [self.Bslot], self.slotI[kx][:], slf[:, kx, :])
                cmp2 = self.sb(es, "cmp2", [128, NBLK, 32])
                be = self.sb(es, "be", [128, NBLK])
                self.v("tensor_tensor", [Bsm, Bc], [Bsm], cmp2[:], sm[:, 3, :].unsqueeze(1).broadcast_to([128, NBLK, 32]),
                       thr160[:].unsqueeze(2).broadcast_to([128, NBLK, 32]), ALU.is_le)
                self.v("tensor_reduce", [Bsm], [Bsm], be[:], cmp2[:], AX.X, ALU.add)
                self.v("tensor_scalar", [Bsm], [Bsm], be[:], be[:], 31.0, None, ALU.min)
                fi = self.sb(es, "fi", [128, NBLK])
                self.v("tensor_scalar", [Bsm, Bc], [Bsm], fi[:], be[:], 128.0, ipk[:, 0:1], ALU.mult, ALU.add)
                self.v("tensor_copy", [Bsm], [Bidx], idxG[:, :, 0], fi[:])
                ht = [self.sb(es, "ht", [128, D], BF16) for _ in range(2)]
                Bht = [Buf(), Buf()]
                for n in range(NT):
                    k = n % 2
                    self.dma([Bd["s_h2"]], [Bht[k]], ht[k][:], Sc["s_h2"][n * 128:(n + 1) * 128, :])
                    for kx in range(2):
                        self.idma([Bht[k], self.Bslot], [Bd["s_xs"]], Sc["s_xs"],
                                  bass.IndirectOffsetOnAxis(ap=self.slotI[kx][:, n:n + 1], axis=0), ht[k][:], None)
            self.s.barrier()
            with contextlib.ExitStack() as es:
                wgS = self.sb(es, "wgS", [128, 8, 512])
                wuS = self.sb(es, "wuS", [128, 8, 512])
                wdS = self.sb(es, "wdS", [128, 4, D])
                BwgS, BwuS, BwdS = Buf(multi=True), Buf(multi=True), Buf(multi=True)
                wgB = self.sb(es, "wgB", [128, 8, 512], BF16)
                wuB = self.sb(es, "wuB", [128, 8, 512], BF16)
                wdB = self.sb(es, "wdB", [128, 4, D], BF16)
                BwgB, BwuB, BwdB = Buf(), Buf(), Buf()
                xb = [self.sb(es, "xb", [128, 4, D], BF16) for _ in range(2)]
                Bxb = [Buf(), Buf()]
                XT = self.sb(es, "XT", [128, 8, BSZ], BF16)
                BXT = Buf()
                sg = [self.sb(es, "sg", [128, 512]) for _ in range(2)]
                Bsg = [Buf(), Buf()]
                hidT = self.sb(es, "hidT", [128, 4, BSZ], BF16)
                BhidT = Buf()
                yb = [self.sb(es, "yb", [128, D]) for _ in range(2)]
                Byb = [Buf(), Buf()]
                pX = [self.ps(es, "pX", [128, 8, 128], BF16) for _ in range(2)]
                BpX = [Buf(), Buf()]
                pG = [self.ps(es, "pG", [128, 512]) for _ in range(2)]
                pU = [self.ps(es, "pU", [128, 512]) for _ in range(2)]
                pY = [self.ps(es, "pY", [128, 512]) for _ in range(2)]
                BpG, BpU, BpY = [Buf(), Buf()], [Buf(), Buf()], [Buf(), Buf()]

                def load(i):
                    g1_, g2_, g3_ = Grp(), Grp(), Grp()
                    off = bass.IndirectOffsetOnAxis(ap=idxG[:, i, 0:1], axis=0)
                    self.idma([Bidx], [BwgS], wgS[:].rearrange("p a f -> p (a f)"), None, I["wg"], off)
                    self.idma([Bidx], [BwuS], wuS[:].rearrange("p a f -> p (a f)"), None, I["wu"], off)
                    self.idma([Bidx], [BwdS], wdS[:].rearrange("p a f -> p (a f)"), None, I["wd"], off)
                    self.s.join([BwgS])
                    self.s.join([BwuS])
                    self.s.join([BwdS])
                    src = Sc["s_xs"][i * BSZ:(i + 1) * BSZ, :].rearrange("(s p) d -> p s d", p=128)
                    self.dma([Bd["s_xs"]], [Bxb[i % 2]], xb[i % 2][:], src)

                def cast(i):
                    self.v("tensor_copy", [BwgS], [BwgB], wgB[:], wgS[:])
                    self.a([BwuS], [BwuB], wuB[:], wuS[:], AF.Copy)
                    self.g("tensor_copy", [BwdS], [BwdB], wdB[:], wdS[:])

                load(0)
                cnt = 0
                for i in range(NBLK):
                    k = i % 2
                    cast(i)
                    if i + 1 < NBLK:
                        load(i + 1)
                    for sub in range(4):
                        px, Bpx = pX[sub % 2], BpX[sub % 2]
                        for kc in range(8):
                            self.tr([Bxb[k], self.Bconst], [Bpx], px[:, kc, :], xb[k][:, sub, kc * 128:(kc + 1) * 128],
                                    self.ident_b[:])
                        if sub % 2 == 0:
                            self.a([Bpx], [BXT], XT[:, :, sub * 128:(sub + 1) * 128], px[:], AF.Copy)
                        else:
                            self.v("tensor_copy", [Bpx], [BXT], XT[:, :, sub * 128:(sub + 1) * 128], px[:])
                    for fc in range(4):
                        j = fc % 2
                        for kc in range(8):
                            self.mm([BXT, BwgB], [BpG[j]], pG[j][:, :], wgB[:, kc, fc * 128:(fc + 1) * 128], XT[:, kc, :],
                                    start=(kc == 0), stop=(kc == 7))
                        for kc in range(8):
                            self.mm([BXT, BwuB], [BpU[j]], pU[j][:, :], wuB[:, kc, fc * 128:(fc + 1) * 128], XT[:, kc, :],
                                    start=(kc == 0), stop=(kc == 7))
                        self.a([BpG[j]], [Bsg[j]], sg[j][:], pG[j][:, :], AF.Silu)
                        self.v("tensor_tensor", [Bsg[j], BpU[j]], [BhidT], hidT[:, fc, :], sg[j][:], pU[j][:, :], ALU.mult)
                    for sub in range(4):
                        y_, By_ = yb[sub % 2], Byb[sub % 2]
                        for half in range(2):
                            for fc in range(4):
                                self.mm([BhidT, BwdB], [BpY[half]], pY[half][:, :], hidT[:, fc, sub * 128:(sub + 1) * 128],
                                        wdB[:, fc, half * 512:(half + 1) * 512], start=(fc == 0), stop=(fc == 3))
                            if half == 0:
                                self.a([BpY[half]], [By_], y_[:, 0:512], pY[half][:, :], AF.Copy)
                            else:
                                self.v("tensor_copy", [BpY[half]], [By_], y_[:, 512:1024], pY[half][:, :])
                        r0 = i * BSZ + sub * 128
                        self.dma([By_], [Bd["s_ys"]], Sc["s_ys"][r0:r0 + 128, :], y_[:])

    def phase_L(self):
        I, Sc, Bd = self.I, self.Sc, self.Bd
        with contextlib.ExitStack() as es:
            G2 = self.sb(es, "G2", [128, D])
            BG2 = Buf()
            gfb = self.sb(es, "gfb", [128, D])
            Bgf = Buf()
            self.dma([], [Bgf], gfb[:], I["gf"][0].partition_broadcast(128))
            pp = [self.ps(es, "pL", [128, 512]) for _ in range(2)]
            Bpp = [Buf(), Buf()]
            Y1 = [self.sb(es, "Y1", [128, D]) for _ in range(2)]
            Y2 = [self.sb(es, "Y2", [128, D]) for _ in range(2)]
            x1 = [self.sb(es, "x1", [128, D]) for _ in range(2)]
            Bin = [Buf(multi=True), Buf(multi=True)]
            ff = self.sb(es, "ff", [128, D])
            Bff = Buf()
            junk = self.sb(es, "junk", [128, D])
            Bjunk = Buf()
            ss = self.sb(es, "ss", [128, 4])
            Bss = Buf()
            ot = [self.sb(es, "ot", [128, D]) for _ in range(2)]
            Bot = [Buf(), Buf()]

            def load(n):
                b, j = n // 32, n % 32
                k = n % 2
                gr = Grp()
                self.idma([self.Bslot, Bd["s_ys"]], [Bin[k]], Y1[k][:], None, Sc["s_ys"],
                          bass.IndirectOffsetOnAxis(ap=self.slotI[0][:, n:n + 1], axis=0), grp=gr)
                self.idma([self.Bslot, Bd["s_ys"]], [Bin[k]], Y2[k][:], None, Sc["s_ys"],
                          bass.IndirectOffsetOnAxis(ap=self.slotI[1][:, n:n + 1], axis=0), grp=gr)
                self.dma([Bd["s_x1"]], [Bin[k]], x1[k][:], Sc["s_x1"][b, j * 128:(j + 1) * 128, :])
                self.s.join([Bin[k]])

            ffL = [ff, self.sb(es, "ff2", [128, D])]
            junkL = [junk, self.sb(es, "junkL2", [128, D])]
            ssL = [ss, self.sb(es, "ssL2", [128, 4])]
            BL_ = {nm: [Buf(), Buf()] for nm in ("Bff", "Bjunk", "Bss")}
            load(0)
            for n in range(NT):
                b, j = n // 32, n % 32
                k = n % 2
                ff, junk, ss = ffL[k], junkL[k], ssL[k]
                Bff, Bjunk, Bss = BL_["Bff"][k], BL_["Bjunk"][k], BL_["Bss"][k]
                if j == 0:
                    self.bcast_row(G2, b, 5, BG2, pp, Bpp, "copy")
                if n + 1 < NT:
                    load(n + 1)
                self.v("tensor_scalar", [Bin[k], self.BWts], [Bff], ff[:], Y1[k][:], self.Wts[:, n, 0:1], None, ALU.mult)
                self.v("scalar_tensor_tensor", [Bin[k], self.BWts, Bff], [Bff], ff[:], Y2[k][:], self.Wts[:, n, 1:2], ff[:],
                       ALU.mult, ALU.add)
                self.v("tensor_tensor", [Bff, BG2], [Bff], ff[:], ff[:], G2[:], ALU.mult)
                self.v("tensor_tensor", [Bff, Bin[k]], [Bff], ff[:], ff[:], x1[k][:], ALU.add)
                rstd = self.rms_rstd(ff, Bff, junk, Bjunk, ss, Bss)
                self.v("scalar_tensor_tensor", [Bff, Bss, Bgf], [Bot[k]], ot[k][:], ff[:], rstd, gfb[:], ALU.mult, ALU.mult)
                self.dma([Bot[k]], [self.Bout], self.out[b, j * 128:(j + 1) * 128, :], ot[k][:])


def _host_inputs(inp):
    f = np.float32
    c = np.ascontiguousarray
    p = np.arange(128)
    ident = np.eye(128, dtype=f)
    maskneg = np.where(p[:, None] > p[None, :], f(-30000.0), f(0.0)).astype(f)
    lstrict = (p[:, None] < p[None, :]).astype(f)
    sel2 = np.zeros((2, 256), f)
    sel2[0, 0:128] = 1.0
    sel2[1, 128:256] = 1.0
    thr64 = (float(BSZ) * np.arange(NTHR, dtype=f))[None, :]
    thr160 = (float(BSZ) * np.arange(NBLK, dtype=f))[None, :]
    iota_pk = (p[:, None] + 128 * np.arange(8)[None, :]).astype(f)
    iota_pf = (p[:, None] + 128 * np.arange(4)[None, :]).astype(f)
    rwv = np.stack([inp["rwkv_w0"][0], inp["rwkv_a0"][0], inp["rwkv_k_k"][0], inp["rwkv_k_a"][0],
                    inp["rwkv_r_k"][0].reshape(512), inp["rwkv_lnx_g"][0], inp["rwkv_lnx_b"][0]]).astype(f)
    shared = {
        "w_ada": c(inp["w_ada"][0]), "b_ada": c(inp["b_ada"]), "g1": c(inp["norm1_g"]), "g2": c(inp["norm2_g"]),
        "gf": c(inp["norm_f_g"][None, :]), "w_in": c(inp["w_in"][0]), "mu": c(inp["rwkv_mu"]), "rwv": c(rwv),
        "w_up": c(inp["rwkv_w_up"][0]), "a_up": c(inp["rwkv_a_up"][0]), "g_up": c(inp["rwkv_g_up"][0]),
        "fbias": c(inp["fox_f_bias"][0][:, None]), "w_out": c(inp["w_out"][0]),
        "w_r": c(np.concatenate([inp["moe_w_grp"][0], inp["moe_w_rt"][0]], axis=1)),
        "b_r": c(np.concatenate([inp["moe_b_grp"][0], inp["moe_b_rt"][0]])[None, :]),
        "wg": c(inp["moe_w_gate"][0].reshape(32, 8, 128, 512).transpose(0, 2, 1, 3).reshape(32 * 128, 8 * 512)),
        "wu": c(inp["moe_w_up"][0].reshape(32, 8, 128, 512).transpose(0, 2, 1, 3).reshape(32 * 128, 8 * 512)),
        "wd": c(inp["moe_w_down"][0].reshape(32, 4, 128, 1024).transpose(0, 2, 1, 3).reshape(32 * 128, 4 * 1024)),
        "ident": ident, "maskneg": maskneg, "lstrict": lstrict, "sel2": sel2, "thr64": thr64, "thr160": thr160,
        "iota_pk": iota_pk, "iota_pf": iota_pf,
    }
    maps = []
    for i in range(NCORES):
        m = dict(shared)
        m["x"] = c(inp["x"][NB * i:NB * (i + 1)])
        cc = inp["c"][NB * i:NB * (i + 1)]
        m["cT"] = c(cc.reshape(NB, 8, 128).transpose(2, 1, 0))
        maps.append(m)
    return maps


def kernel(**inputs):
    inp = {k: np.asarray(v, dtype=np.float32) for k, v in inputs.items()}
    maps = _host_inputs(inp)
    nc = K().build()
    res = run_bass_kernel_spmd(nc, maps, core_ids=list(range(NCORES)))
    return np.concatenate([np.asarray(r["out"]) for r in res.results], axis=0).astype(np.float32)
```
